# Optimizing a Trainium2 kernel written in Bass

```python
import math
import jax
import jax.numpy as jnp
from jax import lax
import numpy as np

D_MODEL = 2048
BATCH = 1
SEQ = 16384
DEPTH = 2

F32 = jnp.float32
EPS = 1e-6
MIX_WIDTH = D_MODEL // 2
N_BRANCH = 4

POOL_WINDOWS = (2, 4, 8, 16)
POOL_WIDTH = MIX_WIDTH
POOL_GROUP = POOL_WIDTH // len(POOL_WINDOWS)

SSD_WIDTH = MIX_WIDTH
SSD_HEADDIM = 64
SSD_HEADS = SSD_WIDTH // SSD_HEADDIM
SSD_STATE = 128
SSD_GROUPS = 2
SSD_CONV = 4
SSD_CHUNK = 128
SSD_CONV_DIM = SSD_WIDTH + 2 * SSD_GROUPS * SSD_STATE
DT_MIN = 1e-3
DT_MAX = 1e-1

MLA_HEADS = 8
MLA_NOPE = 128
MLA_ROPE = 64
MLA_QK = MLA_NOPE + MLA_ROPE
MLA_V = MIX_WIDTH // MLA_HEADS
MLA_Q_LORA = 768
MLA_KV_LORA = 512
ROPE_THETA = 10000.0
ATT_BLOCK = 128
MAX_POS_OFFSET = 4096

SGU_WIDTH = MIX_WIDTH
SGU_CHUNK = 128
SGU_GROUPS = 8
SGU_GROUP_CH = SGU_WIDTH // SGU_GROUPS

IN_SPLITS = (POOL_WIDTH, SSD_WIDTH, SSD_CONV_DIM, SSD_HEADS, MLA_Q_LORA, MLA_KV_LORA, MLA_ROPE, 2 * SGU_WIDTH, N_BRANCH * D_MODEL)
D_IN = sum(IN_SPLITS)

D_FF = 5632
N_EXPERTS = 8
TOP_K = 2
D_FF_EXPERT = 7168
MOE_BLOCK = 512
N_DENSE = (DEPTH + 1) // 2
N_MOE = DEPTH // 2

kernel_name = 'hybrid_parallel_gated_trunk'


def rms_norm(x, gain=None):
    xf = x.astype(F32)
    y = xf * lax.rsqrt(jnp.mean(xf * xf, axis=-1, keepdims=True) + EPS)
    if gain is not None:
        y = y * gain.astype(F32)
    return y.astype(x.dtype)


def layer_norm(x, gain, bias):
    xf = x.astype(F32)
    xc = xf - jnp.mean(xf, axis=-1, keepdims=True)
    y = xc * lax.rsqrt(jnp.mean(xc * xc, axis=-1, keepdims=True) + EPS)
    return (y * gain.astype(F32) + bias.astype(F32)).astype(x.dtype)


def swiglu(h, w1, w3, w2):
    return (jax.nn.silu(h @ w1) * (h @ w3)) @ w2


def pool_mixer(a, w_pool, pool_scale):
    b, s, _ = a.shape
    af = a.astype(F32)
    cs = jnp.concatenate([jnp.zeros((b, 1, POOL_WIDTH), F32), jnp.cumsum(af, axis=1)], axis=1)
    t = jnp.arange(s)
    groups = []
    for g, win in enumerate(POOL_WINDOWS):
        ch = slice(g * POOL_GROUP, (g + 1) * POOL_GROUP)
        start = jnp.maximum(t + 1 - win, 0)
        count = (t + 1 - start).astype(F32)[None, :, None]
        mean = (cs[:, 1:, ch] - cs[:, start, ch]) / count
        groups.append(mean - af[:, :, ch])
    pooled = jnp.stack(groups, axis=2).astype(a.dtype)
    mixed = jnp.einsum('bsgc,gcd->bsgd', pooled, w_pool)
    return mixed.reshape(b, s, POOL_WIDTH) * pool_scale


def causal_dwconv(x, w, bias):
    y = lax.conv_general_dilated(x, w[:, None, :], window_strides=(1,), padding=[(SSD_CONV - 1, 0)],
                                 dimension_numbers=('NWC', 'WIO', 'NWC'), feature_group_count=x.shape[-1])
    return y + bias


def segsum(a):
    cs = jnp.cumsum(a, axis=-1)
    t = a.shape[-1]
    diff = cs[..., :, None] - cs[..., None, :]
    return jnp.where(jnp.tril(jnp.ones((t, t), bool)), diff, -jnp.inf)


def ssd_mixer(z, xbc, dt_raw, conv_w, conv_b, dt_bias, a_log, d_skip, norm_gain):
    b, s, _ = z.shape
    nc, ln = s // SSD_CHUNK, SSD_CHUNK
    hpg = SSD_HEADS // SSD_GROUPS
    xbc = jax.nn.silu(causal_dwconv(xbc, conv_w, conv_b))
    xs, bm, cm = jnp.split(xbc, [SSD_WIDTH, SSD_WIDTH + SSD_GROUPS * SSD_STATE], axis=-1)
    xs = xs.astype(F32).reshape(b, nc, ln, SSD_HEADS, SSD_HEADDIM)
    bm = bm.astype(F32).reshape(b, nc, ln, SSD_GROUPS, SSD_STATE)
    cm = cm.astype(F32).reshape(b, nc, ln, SSD_GROUPS, SSD_STATE)
    dt = jax.nn.softplus(dt_raw.astype(F32) + dt_bias.astype(F32)).reshape(b, nc, ln, SSD_HEADS)
    a = -jnp.exp(a_log.astype(F32))
    da = (dt * a).transpose(0, 3, 1, 2)
    xdt = xs * dt[..., None]
    a_cs = jnp.cumsum(da, axis=-1)
    cb = jnp.repeat(jnp.einsum('bclgn,bcsgn->bgcls', cm, bm), hpg, axis=1)
    y_diag = jnp.einsum('bhcls,bcshp->bclhp', cb * jnp.exp(segsum(da)), xdt)
    bh = jnp.repeat(bm, hpg, axis=3)
    ch = jnp.repeat(cm, hpg, axis=3)
    states = jnp.einsum('bclhn,bhcl,bclhp->bchpn', bh, jnp.exp(a_cs[..., -1:] - a_cs), xdt)
    def step(hstate, inp):
        decay, st = inp
        return hstate * decay[..., None, None] + st, hstate
    h0 = jnp.zeros((b, SSD_HEADS, SSD_HEADDIM, SSD_STATE), F32)
    _, prev = lax.scan(step, h0, (jnp.exp(a_cs[..., -1]).transpose(2, 0, 1), states.transpose(1, 0, 2, 3, 4)))
    y_off = jnp.einsum('bclhn,cbhpn,bhcl->bclhp', ch, prev, jnp.exp(a_cs))
    y = (y_diag + y_off + xs * d_skip.astype(F32)[:, None]).reshape(b, s, SSD_WIDTH)
    y = rms_norm(y * jax.nn.silu(z.astype(F32)), norm_gain)
    return y.astype(z.dtype)


def apply_rope(x, positions):
    half = MLA_ROPE // 2
    inv_freq = ROPE_THETA ** (-jnp.arange(half, dtype=F32) * 2.0 / MLA_ROPE)
    ang = positions.astype(F32)[:, :, None, None] * inv_freq
    cos, sin = jnp.cos(ang), jnp.sin(ang)
    xf = x.astype(F32)
    x1, x2 = xf[..., :half], xf[..., half:]
    return jnp.concatenate([x1 * cos - x2 * sin, x1 * sin + x2 * cos], axis=-1).astype(x.dtype)


def mla_mixer(cq, ckv, k_rope, positions, q_norm, w_uq, kv_norm, w_ukv, q_gain, k_gain):
    b, s, _ = cq.shape
    q = (rms_norm(cq, q_norm) @ w_uq).reshape(b, s, MLA_HEADS, MLA_QK)
    kv = (rms_norm(ckv, kv_norm) @ w_ukv).reshape(b, s, MLA_HEADS, MLA_NOPE + MLA_V)
    k_nope, v = kv[..., :MLA_NOPE], kv[..., MLA_NOPE:]
    k = jnp.concatenate([k_nope, jnp.broadcast_to(k_rope[:, :, None, :], (b, s, MLA_HEADS, MLA_ROPE))], axis=-1)
    q = rms_norm(q, q_gain)
    k = rms_norm(k, k_gain)
    q = jnp.concatenate([q[..., :MLA_NOPE], apply_rope(q[..., MLA_NOPE:], positions)], axis=-1)
    k = jnp.concatenate([k[..., :MLA_NOPE], apply_rope(k[..., MLA_NOPE:], positions)], axis=-1)
    q, k, v = q.transpose(0, 2, 1, 3), k.transpose(0, 2, 1, 3), v.transpose(0, 2, 1, 3)
    scale = MLA_QK ** -0.5
    kpos = jnp.arange(s)
    def query_block(i):
        qb = lax.dynamic_slice_in_dim(q, i * ATT_BLOCK, ATT_BLOCK, axis=2)
        sc = jnp.einsum('bhqd,bhkd->bhqk', qb, k).astype(F32) * scale
        qpos = i * ATT_BLOCK + jnp.arange(ATT_BLOCK)
        sc = jnp.where(kpos[None, :] <= qpos[:, None], sc, -jnp.inf)
        p = jax.nn.softmax(sc, axis=-1)
        return jnp.einsum('bhqk,bhkd->bhqd', p.astype(v.dtype), v)
    o = lax.map(query_block, jnp.arange(s // ATT_BLOCK))
    return o.transpose(1, 0, 3, 2, 4).reshape(b, s, MLA_HEADS * MLA_V)


def sgu_mixer(uv, ln_gain, ln_bias, w_s, b_s):
    b, s, _ = uv.shape
    uv = jax.nn.gelu(uv, approximate=False)
    u, v = jnp.split(uv, 2, axis=-1)
    v = layer_norm(v, ln_gain, ln_bias)
    v = v.reshape(b, s // SGU_CHUNK, SGU_CHUNK, SGU_GROUPS, SGU_GROUP_CH)
    w = w_s * jnp.tril(jnp.ones((SGU_CHUNK, SGU_CHUNK), w_s.dtype))
    mixed = jnp.einsum('gts,bnsgc->bntgc', w, v) + b_s.T[None, None, :, :, None]
    return u * mixed.reshape(b, s, SGU_WIDTH)


def moe_ffn(h, w_router, w1, w3, w2):
    b, s, d = h.shape
    n = b * s
    hf = h.reshape(n, d)
    logits = (hf @ w_router).astype(F32)
    top_logit, top_idx = lax.top_k(logits, TOP_K)
    top_w = jax.nn.softmax(top_logit, axis=-1)
    e_flat = top_idx.reshape(-1)
    tok_flat = jnp.arange(n * TOP_K, dtype=jnp.int32) // TOP_K
    order = jnp.argsort(e_flat)
    e_sorted = e_flat[order]
    counts = jnp.bincount(e_flat, length=N_EXPERTS)
    padded = (counts + MOE_BLOCK - 1) // MOE_BLOCK * MOE_BLOCK
    start = jnp.cumsum(counts) - counts
    pend = jnp.cumsum(padded)
    pstart = pend - padded
    slot = pstart[e_sorted] + jnp.arange(n * TOP_K) - start[e_sorted]
    n_slots = -(-(n * TOP_K) // MOE_BLOCK) * MOE_BLOCK + N_EXPERTS * MOE_BLOCK
    n_blocks = n_slots // MOE_BLOCK
    slot_tok = jnp.full((n_slots,), n, jnp.int32).at[slot].set(tok_flat[order])
    slot_w = jnp.zeros((n_slots,), F32).at[slot].set(top_w.reshape(-1)[order])
    block_e = jnp.minimum(jnp.searchsorted(pend, jnp.arange(n_blocks) * MOE_BLOCK, side='right'), N_EXPERTS - 1)
    h_pad = jnp.concatenate([hf, jnp.zeros((1, d), hf.dtype)], axis=0)
    xb = h_pad[slot_tok].reshape(n_blocks, MOE_BLOCK, d)
    yb = lax.map(lambda args: swiglu(args[0], w1[args[1]], w3[args[1]], w2[args[1]]), (xb, block_e))
    out = jnp.zeros((n + 1, d), F32).at[slot_tok].add(yb.reshape(n_slots, d).astype(F32) * slot_w[:, None])
    return out[:n].reshape(b, s, d).astype(h.dtype)


def setup_inputs(seed: int = 0) -> dict:
    key = jax.random.key(seed)
    keys = iter(jax.random.split(key, 48))
    def normal(shape, scale):
        return jax.random.normal(next(keys), shape, F32) * scale
    def near_one(shape):
        return 1.0 + normal(shape, 0.02)
    L = DEPTH
    x = normal((BATCH, SEQ, D_MODEL), 1.0)
    c = normal((BATCH, D_MODEL), 1.0)
    offset = jax.random.randint(next(keys), (BATCH, 1), 0, MAX_POS_OFFSET, jnp.int32)
    positions = offset + jnp.arange(SEQ, dtype=jnp.int32)[None, :]
    w_ada = normal((L, D_MODEL, 6 * D_MODEL), 0.5 * D_MODEL ** -0.5)
    b_ada = normal((L, 6 * D_MODEL), 0.02)
    w_in = normal((L, D_MODEL, D_IN), D_MODEL ** -0.5)
    w_pool = normal((L, len(POOL_WINDOWS), POOL_GROUP, POOL_GROUP), POOL_GROUP ** -0.5)
    pool_scale = near_one((L, POOL_WIDTH))
    ssd_conv_w = normal((L, SSD_CONV, SSD_CONV_DIM), SSD_CONV ** -0.5)
    ssd_conv_b = normal((L, SSD_CONV_DIM), 0.02)
    dt0 = jnp.exp(jax.random.uniform(next(keys), (L, SSD_HEADS), F32, math.log(DT_MIN), math.log(DT_MAX)))
    ssd_dt_bias = dt0 + jnp.log(-jnp.expm1(-dt0))
    ssd_a_log = jnp.log(jax.random.uniform(next(keys), (L, SSD_HEADS), F32, 1.0, 16.0))
    ssd_d = near_one((L, SSD_HEADS))
    ssd_norm = near_one((L, SSD_WIDTH))
    mla_q_norm = near_one((L, MLA_Q_LORA))
    mla_w_uq = normal((L, MLA_Q_LORA, MLA_HEADS * MLA_QK), MLA_Q_LORA ** -0.5)
    mla_kv_norm = near_one((L, MLA_KV_LORA))
    mla_w_ukv = normal((L, MLA_KV_LORA, MLA_HEADS * (MLA_NOPE + MLA_V)), MLA_KV_LORA ** -0.5)
    mla_q_gain = near_one((L, MLA_QK))
    mla_k_gain = near_one((L, MLA_QK))
    sgu_ln_gain = near_one((L, SGU_WIDTH))
    sgu_ln_bias = normal((L, SGU_WIDTH), 0.02)
    sgu_w_s = normal((L, SGU_GROUPS, SGU_CHUNK, SGU_CHUNK), SGU_CHUNK ** -0.5)
    sgu_b_s = near_one((L, SGU_GROUPS, SGU_CHUNK))
    w_branch = normal((L, N_BRANCH, MIX_WIDTH, D_MODEL), MIX_WIDTH ** -0.5)
    w_out = normal((L, D_MODEL, D_MODEL), D_MODEL ** -0.5)
    ffn_w1 = normal((N_DENSE, D_MODEL, D_FF), D_MODEL ** -0.5)
    ffn_w3 = normal((N_DENSE, D_MODEL, D_FF), D_MODEL ** -0.5)
    ffn_w2 = normal((N_DENSE, D_FF, D_MODEL), D_FF ** -0.5)
    moe_router = normal((N_MOE, D_MODEL, N_EXPERTS), D_MODEL ** -0.5)
    moe_w1 = normal((N_MOE, N_EXPERTS, D_MODEL, D_FF_EXPERT), D_MODEL ** -0.5)
    moe_w3 = normal((N_MOE, N_EXPERTS, D_MODEL, D_FF_EXPERT), D_MODEL ** -0.5)
    moe_w2 = normal((N_MOE, N_EXPERTS, D_FF_EXPERT, D_MODEL), D_FF_EXPERT ** -0.5)
    return {'x': x, 'c': c, 'positions': positions, 'w_ada': w_ada, 'b_ada': b_ada, 'w_in': w_in,
            'w_pool': w_pool, 'pool_scale': pool_scale, 'ssd_conv_w': ssd_conv_w, 'ssd_conv_b': ssd_conv_b,
            'ssd_dt_bias': ssd_dt_bias, 'ssd_a_log': ssd_a_log, 'ssd_d': ssd_d, 'ssd_norm': ssd_norm,
            'mla_q_norm': mla_q_norm, 'mla_w_uq': mla_w_uq, 'mla_kv_norm': mla_kv_norm, 'mla_w_ukv': mla_w_ukv,
            'mla_q_gain': mla_q_gain, 'mla_k_gain': mla_k_gain, 'sgu_ln_gain': sgu_ln_gain,
            'sgu_ln_bias': sgu_ln_bias, 'sgu_w_s': sgu_w_s, 'sgu_b_s': sgu_b_s, 'w_branch': w_branch,
            'w_out': w_out, 'ffn_w1': ffn_w1, 'ffn_w3': ffn_w3, 'ffn_w2': ffn_w2, 'moe_router': moe_router,
            'moe_w1': moe_w1, 'moe_w3': moe_w3, 'moe_w2': moe_w2}


def reference(x, c, positions, w_ada, b_ada, w_in, w_pool, pool_scale, ssd_conv_w, ssd_conv_b, ssd_dt_bias,
              ssd_a_log, ssd_d, ssd_norm, mla_q_norm, mla_w_uq, mla_kv_norm, mla_w_ukv, mla_q_gain, mla_k_gain,
              sgu_ln_gain, sgu_ln_bias, sgu_w_s, sgu_b_s, w_branch, w_out, ffn_w1, ffn_w3, ffn_w2, moe_router,
              moe_w1, moe_w3, moe_w2):
    b, s, _ = x.shape
    offsets = np.cumsum(IN_SPLITS)[:-1].tolist()
    c_act = jax.nn.silu(c)
    for layer in range(DEPTH):
        mod = (c_act @ w_ada[layer] + b_ada[layer])[:, None, :]
        shift1, scale1, gate1, shift2, scale2, gate2 = jnp.split(mod, 6, axis=-1)
        h = rms_norm(x) * (1 + scale1) + shift1
        proj = h @ w_in[layer]
        a_in, z, xbc, dt_raw, cq, ckv, k_rope, uv, gate_logits = jnp.split(proj, offsets, axis=-1)
        y_pool = pool_mixer(a_in, w_pool[layer], pool_scale[layer])
        y_ssd = ssd_mixer(z, xbc, dt_raw, ssd_conv_w[layer], ssd_conv_b[layer], ssd_dt_bias[layer],
                          ssd_a_log[layer], ssd_d[layer], ssd_norm[layer])
        y_att = mla_mixer(cq, ckv, k_rope, positions, mla_q_norm[layer], mla_w_uq[layer], mla_kv_norm[layer],
                          mla_w_ukv[layer], mla_q_gain[layer], mla_k_gain[layer])
        y_sgu = sgu_mixer(uv, sgu_ln_gain[layer], sgu_ln_bias[layer], sgu_w_s[layer], sgu_b_s[layer])
        gates = jax.nn.sigmoid(gate_logits).reshape(b, s, N_BRANCH, D_MODEL)
        merged = sum(gates[:, :, i] * (y @ w_branch[layer, i]) for i, y in enumerate((y_pool, y_ssd, y_att, y_sgu)))
        x = x + gate1 * (merged @ w_out[layer])
        h2 = rms_norm(x) * (1 + scale2) + shift2
        idx = layer // 2
        if layer % 2 == 0:
            f = swiglu(h2, ffn_w1[idx], ffn_w3[idx], ffn_w2[idx])
        else:
            f = moe_ffn(h2, moe_router[idx], moe_w1[idx], moe_w3[idx], moe_w2[idx])
        x = x + gate2 * f
    return x
```

```python
import math
import numpy as np
import ml_dtypes
import concourse.bass as bass
import concourse.mybir as mybir
from concourse.bass_utils import run_bass_kernel_spmd

F32 = mybir.dt.float32
BF16 = mybir.dt.bfloat16
I32 = mybir.dt.int32
AF = mybir.ActivationFunctionType
ALU = mybir.AluOpType
AX = mybir.AxisListType

D = 2048
KD = 16
EPS = 1e-6
OFF = dict(a_in=0, z=1024, xbc=2048, dt=3584, cq=3600, ckv=4368, kr=4880, uv=4944, gates=6992)


class Tl:
    __slots__ = ("t", "name", "last_w", "readers", "root")

    def __init__(self, t, name):
        self.t = t
        self.name = name
        self.last_w = None
        self.readers = {}
        self.root = self

    def __getitem__(self, idx):
        return self.t[idx]


class View:
    __slots__ = ("t", "name", "root")

    def __init__(self, parent, ap, name):
        self.t = ap
        self.name = name
        self.root = parent.root

    def __getitem__(self, idx):
        return self.t[idx]


class Ring:
    def __init__(self, tiles):
        self.tiles = tiles
        self.i = 0

    def next(self):
        t = self.tiles[self.i]
        self.i = (self.i + 1) % len(self.tiles)
        return t


class KB:
    NDMA = 6

    def __init__(self, nc):
        self.nc = nc
        self.E = {"pe": nc.tensor, "act": nc.scalar, "dve": nc.vector,
                  "pool": nc.gpsimd, "sp": nc.sync}
        self.sems = {}
        self.cnt = {}
        for k in self.E:
            self.sems[k] = nc.alloc_semaphore("c_" + k)
            self.cnt[k] = 0
        self.dma_rr = {}
        for q in ("sp", "pool", "act"):
            self.dma_rr[q] = 0
            for i in range(self.NDMA):
                key = ("d", q, i)
                self.sems[key] = nc.alloc_semaphore("d_%s_%d" % (q, i))
                self.cnt[key] = 0
        self.seen = {k: {} for k in self.E}
        self.ntile = 0
        self.pending_out = []

    def sb(self, shape, dt, name="t"):
        self.ntile += 1
        return Tl(self.nc.alloc_sbuf_tensor("%s_%d" % (name, self.ntile), list(shape), dt), name)

    def ps(self, shape, dt=F32, name="p"):
        self.ntile += 1
        return Tl(self.nc.alloc_psum_tensor("%s_%d" % (name, self.ntile), list(shape), dt), name)

    def ring(self, n, shape, dt, name="r", psum=False):
        f = self.ps if psum else self.sb
        return Ring([f(shape, dt, name) for _ in range(n)])

    def _wait(self, e, deps):
        need = {}
        for d in deps:
            if d is None:
                continue
            k, v = d
            if k == e and e == "pe":
                continue
            if need.get(k, 0) < v:
                need[k] = v
        seen = self.seen[e]
        for k, v in need.items():
            if seen.get(k, 0) >= v:
                continue
            self.E[e].wait_ge(self.sems[k], v)
            seen[k] = v

    def _deps(self, reads, writes):
        deps = []
        for r in reads:
            deps.append(r.root.last_w)
        for w in writes:
            deps.append(w.root.last_w)
            deps.extend(w.root.readers.items())
        return deps

    def _commit(self, tok, reads, writes):
        writes = [w.root for w in writes]
        reads = [r.root for r in reads]
        for w in writes:
            w.last_w = tok
            w.readers = {}
        k, v = tok
        for r in reads:
            if r in writes:
                continue
            if r.readers.get(k, 0) < v:
                r.readers[k] = v

    def op(self, e, fn, reads=(), writes=(), inc=True):
        self._wait(e, self._deps(reads, writes))
        ins = fn()
        if inc:
            self.cnt[e] += 1
            ins.then_inc(self.sems[e], 1)
            tok = (e, self.cnt[e])
        else:
            tok = (e, self.cnt[e] + 1)
        self._commit(tok, reads, writes)
        return ins

    def dma(self, q, out, in_, reads=(), writes=(), **kw):
        i = self.dma_rr[q]
        self.dma_rr[q] = (i + 1) % self.NDMA
        key = ("d", q, i)
        deps = self._deps(reads, writes)
        deps.append((key, self.cnt[key]))
        self._wait(q, deps)
        ins = self.E[q].dma_start(out=out, in_=in_, **kw)
        self.cnt[key] += 16
        ins.then_inc(self.sems[key], 16)
        tok = (key, self.cnt[key])
        self._commit(tok, reads, writes)
        if not writes:
            self.pending_out.append(tok)
        return ins

    def finish(self):
        last = {}
        for k, v in self.pending_out:
            last[k] = max(last.get(k, 0), v)
        self._wait("sp", list(last.items()))
        self._wait("sp", [(k, self.cnt[k]) for k in ("pe", "act", "dve", "pool") if self.cnt[k]])

    def mm(self, ps, out_ap, pairs):
        nc = self.nc
        n = len(pairs)
        for i, (l, r, rd) in enumerate(pairs):
            self.op("pe", lambda: nc.tensor.matmul(out_ap, l, r, start=(i == 0), stop=(i == n - 1)),
                    reads=rd, writes=[ps], inc=(i == n - 1))


def _din(nc, name, shape, dt=F32):
    return nc.dram_tensor(name, list(shape), dt, kind="ExternalInput").ap()


def _dout(nc, name, shape, dt=F32):
    return nc.dram_tensor(name, list(shape), dt, kind="ExternalOutput").ap()


def _rstd(kb, ps_ss, out, n, epsb):
    nc = kb.nc
    kb.op("act", lambda: nc.scalar.activation(out[:], ps_ss[:], AF.Sqrt, bias=epsb[:, 0:1], scale=1.0 / n),
          reads=[ps_ss, epsb], writes=[out])
    kb.op("dve", lambda: nc.vector.reciprocal(out[:], out[:]), reads=[out], writes=[out])


def _emit_mod(kb, c_pk, wada, bada, ncol, psr):
    nc = kb.nc
    nj = ncol // 128
    cs = kb.sb([128, 16], F32, "cs")
    cact = kb.sb([128, 16], F32, "cact")
    bsb = kb.sb([128, nj], F32, "bada")
    modT = kb.sb([128, nj], F32, "modT")
    kb.dma("sp", cs[:], c_pk, writes=[cs])
    kb.dma("sp", bsb[:], bada, writes=[bsb])
    kb.op("act", lambda: nc.scalar.activation(cact[:], cs[:], AF.Silu), reads=[cs], writes=[cact])
    war = kb.ring(2, [128, 16, 128], F32, "wada")
    ps = psr.next()
    for j in range(nj):
        wa = war.next()
        kb.dma("sp", wa[:], wada[j].rearrange("p (k c) -> p k c", k=16), writes=[wa])
        kb.mm(ps, ps[:, j:j + 1], [(wa[:, k, :], cact[:, k:k + 1], [wa, cact]) for k in range(16)])
    kb.op("dve", lambda: nc.vector.tensor_tensor(modT[:], ps[:, 0:nj], bsb[:], ALU.add),
          reads=[ps, bsb], writes=[modT])
    return modT


def _load_mod(kb, mod_d, nj):
    t = kb.sb([128, nj], F32, "modT")
    kb.dma("sp", t[:], mod_d, writes=[t])
    return t


def build_M():
    nc = bass.Bass("TRN2", target_bir_lowering=False)
    c_pk = _din(nc, "c_pk", [128, 16])
    wada = _din(nc, "wada", [24, 128, KD * 128])
    bada = _din(nc, "bada_pk", [128, 24])
    o = _dout(nc, "modT", [128, 24])
    kb = KB(nc)
    psr = kb.ring(2, [128, 512], F32, "ps", psum=True)
    modT = _emit_mod(kb, c_pk, wada, bada, 24 * 128, psr)
    kb.dma("sp", o, modT[:], reads=[modT])
    kb.finish()
    return nc


def run_M(inp):
    maps = []
    for i in range(8):
        w = np.concatenate([inp["w_ada"][l][:, i * 1536:(i + 1) * 1536] for l in range(2)], axis=1)
        b = np.concatenate([inp["b_ada"][l][i * 1536:(i + 1) * 1536] for l in range(2)])
        maps.append(dict(c_pk=_pk(inp["c"][0], 16), wada=_blockify(np.ascontiguousarray(w), 24), bada_pk=_pk(b, 24)))
    res = run_bass_kernel_spmd(build_M(), maps, core_ids=list(range(8)))
    out = []
    for l in range(2):
        out.append(np.ascontiguousarray(np.concatenate([r["modT"][:, l * 12:(l + 1) * 12] for r in res.results], axis=1)))
    return out


def _emit_norm_mod(kb, xk, hT, shiftT, onepT, ones, epsb, psr, sqr, tmpr, TG, rstd_t):
    nc = kb.nc
    ps = psr.next()
    for k in range(KD):
        sq = sqr.next()
        kb.op("act", lambda: nc.scalar.activation(sq[:], xk[k][:], AF.Square), reads=[xk[k]], writes=[sq])
        kb.op("pe", lambda: nc.tensor.matmul(ps[:, 0:TG], ones[:], sq[:], start=(k == 0), stop=(k == KD - 1)),
              reads=[ones, sq], writes=[ps], inc=True)
    rstd = rstd_t
    _rstd(kb, ps, rstd, D, epsb)
    for k in range(KD):
        tmp = tmpr.next()
        kb.op("dve", lambda: nc.vector.tensor_tensor(tmp[:], xk[k][:], rstd[:], ALU.mult),
              reads=[xk[k], rstd], writes=[tmp])
        kb.op("act", lambda: nc.scalar.activation(hT[k][:], tmp[:], AF.Identity,
                                                  bias=shiftT[:, k:k + 1], scale=onepT[:, k:k + 1]),
              reads=[tmp, shiftT, onepT], writes=[hT[k]])


PI_LO = 3.1415925
C1_2PI = 6.28125
C2_2PI = 2.0 * math.pi - 6.28125


def _emit_rope_tables(kb, posi, ang, angk, angi, sin2, cos2, rc_s):
    nc = kb.nc
    V = nc.vector
    kb.op("dve", lambda: V.tensor_copy(ang[:], posi[:]), reads=[posi], writes=[ang])
    kb.op("dve", lambda: V.tensor_scalar_mul(ang[:], ang[:], rc_s[:, 0:1]), reads=[ang, rc_s], writes=[ang])
    kb.op("dve", lambda: V.tensor_scalar_mul(angk[:], ang[:], 1.0 / (2.0 * math.pi)), reads=[ang], writes=[angk])
    kb.op("dve", lambda: V.tensor_copy(angi[:], angk[:]), reads=[angk], writes=[angi])
    kb.op("dve", lambda: V.tensor_copy(angk[:], angi[:]), reads=[angi], writes=[angk])
    kb.op("dve", lambda: V.scalar_tensor_tensor(ang[:], angk[:], -C1_2PI, ang[:], ALU.mult, ALU.add),
          reads=[angk, ang], writes=[ang])
    kb.op("dve", lambda: V.scalar_tensor_tensor(ang[:], angk[:], -C2_2PI, ang[:], ALU.mult, ALU.add),
          reads=[angk, ang], writes=[ang])

    def wrap(t):
        kb.op("dve", lambda: V.tensor_scalar(angk[:], t[:], math.pi, -2.0 * math.pi, ALU.is_gt, ALU.mult),
              reads=[t], writes=[angk])
        kb.op("dve", lambda: V.tensor_tensor(t[:], t[:], angk[:], ALU.add), reads=[t, angk], writes=[t])
        kb.op("dve", lambda: V.tensor_scalar(angk[:], t[:], -math.pi, 2.0 * math.pi, ALU.is_lt, ALU.mult),
              reads=[t], writes=[angk])
        kb.op("dve", lambda: V.tensor_tensor(t[:], t[:], angk[:], ALU.add), reads=[t, angk], writes=[t])
        kb.op("dve", lambda: V.tensor_scalar(t[:], t[:], PI_LO, -PI_LO, ALU.min, ALU.max), reads=[t], writes=[t])
    wrap(ang)
    kb.op("dve", lambda: V.tensor_scalar_add(cos2[:], ang[:], 0.5 * math.pi), reads=[ang], writes=[cos2])
    wrap(cos2)
    kb.op("act", lambda: nc.scalar.activation(sin2[:], ang[:], AF.Sin, scale=rc_s[:, 1:2]),
          reads=[ang, rc_s], writes=[sin2])
    kb.op("act", lambda: nc.scalar.activation(cos2[:], cos2[:], AF.Sin), reads=[cos2], writes=[cos2])

def _emit_norm_mod2(kb, xload, hT, shiftT, onepT, ones, epsb, psr, sqr, tmpr, TG, rstd_t, after=None):
    nc = kb.nc
    ps = psr.next()
    for k in range(KD):
        xt = xload(k)
        sq = sqr.next()
        kb.op("act", lambda: nc.scalar.activation(sq[:], xt[:], AF.Square), reads=[xt], writes=[sq])
        kb.op("pe", lambda: nc.tensor.matmul(ps[:, 0:TG], ones[:], sq[:], start=(k == 0), stop=(k == KD - 1)),
              reads=[ones, sq], writes=[ps], inc=True)
    _rstd(kb, ps, rstd_t, D, epsb)
    for k in range(KD):
        xt = xload(k)
        tmp = tmpr.next()
        kb.op("dve", lambda: nc.vector.tensor_tensor(tmp[:], xt[:], rstd_t[:], ALU.mult),
              reads=[xt, rstd_t], writes=[tmp])
        kb.op("act", lambda: nc.scalar.activation(hT[k][:], tmp[:], AF.Identity,
                                                  bias=shiftT[:, k:k + 1], scale=onepT[:, k:k + 1]),
              reads=[tmp, shiftT, onepT], writes=[hT[k]])
        if after is not None:
            after(k, tmp)


NBA = 24


def build_A(NTOK, dbg=False):
    TG = 512
    NG = NTOK // TG
    nc = bass.Bass("TRN2", target_bir_lowering=False)
    xT = _din(nc, "xT", [D, NTOK])
    mod_d = _din(nc, "modT", [128, 32])
    winb = _din(nc, "winb", [NBA, 128, KD * 128])
    dtb = _din(nc, "dtb_bc", [128, 16])
    qn = _din(nc, "qnorm_pk", [128, 6])
    kvn = _din(nc, "kvnorm_pk", [128, 4])
    wuq = _din(nc, "wuq", [768, 2048])
    wukk = _din(nc, "wukv_k", [512, 1024])
    wukv = _din(nc, "wukv_v", [512, 1024])
    qg = _din(nc, "qgain", [128, 3])
    kg = _din(nc, "kgain", [128, 3])
    pos = _din(nc, "pos_bc", [64, NTOK], I32)
    rc = _din(nc, "ropec", [64, 4])
    o_xbc = _dout(nc, "xbcT", [1536, NTOK])
    o_dt = _dout(nc, "dt", [NTOK, 16])
    o_q = _dout(nc, "qT", [8, 192, NTOK], BF16)
    o_k = _dout(nc, "kT", [8, 192, NTOK], BF16)
    o_v = _dout(nc, "v", [NTOK, 1024], BF16)
    if dbg:
        o_dh = _dout(nc, "dbg_h", [D, TG], BF16)
        o_dm = _dout(nc, "dbg_mod", [128, 32])

    kb = KB(nc)
    psr = kb.ring(8, [128, 512], F32, "ps", psum=True)
    ones = kb.sb([128, 128], BF16, "ones")
    epsb = kb.sb([128, 1], F32, "eps")
    kb.op("pool", lambda: nc.gpsimd.memset(ones[:], 1.0), writes=[ones])
    kb.op("pool", lambda: nc.gpsimd.memset(epsb[:], EPS), writes=[epsb])

    modT = _load_mod(kb, mod_d, 32)
    onepT = kb.sb([128, 16], F32, "onep")
    kb.op("dve", lambda: nc.vector.tensor_scalar_add(onepT[:], modT[:, 16:32], 1.0), reads=[modT], writes=[onepT])

    def load_small(ap, shape, dt=F32, name="c"):
        t = kb.sb(shape, dt, name)
        kb.dma("sp", t[:], ap, writes=[t])
        return t
    dtb_s = load_small(dtb, [128, 16])
    qn_s = load_small(qn, [128, 6])
    kvn_s = load_small(kvn, [128, 4])
    qg_s = load_small(qg, [128, 3])
    kg_s = load_small(kg, [128, 3])
    rc_s = load_small(rc, [64, 4])
    wuq_s = kb.sb([128, 6, 2048], BF16, "wuq")
    wukk_s = kb.sb([128, 4, 1024], BF16, "wukk")
    wukv_s = kb.sb([128, 4, 1024], BF16, "wukv")
    wuq_v = wuq.rearrange("(k p) c -> p k c", p=128)
    for j in range(6):
        kb.dma("pool", wuq_s[:, j, :], wuq_v[:, j, :], writes=[wuq_s])
    kb.dma("pool", wukk_s[:], wukk.rearrange("(k p) c -> p k c", p=128), writes=[wukk_s])
    kb.dma("pool", wukv_s[:], wukv.rearrange("(k p) c -> p k c", p=128), writes=[wukv_s])

    xk = [kb.sb([128, TG], F32, "x%d" % k) for k in range(KD)]
    hT = [kb.sb([128, TG], BF16, "h%d" % k) for k in range(KD)]
    sqr = kb.ring(3, [128, TG], BF16, "sq")
    tmpr = kb.ring(6, [128, TG], F32, "tmp")
    rstd_x = kb.sb([128, TG], F32, "rstdx")
    wbr = kb.ring(3, [128, KD, 128], BF16, "wb")
    stg = kb.ring(3, [128, TG], F32, "stg")
    stgb = kb.ring(4, [128, TG], BF16, "stgb")
    cq_s = [kb.sb([128, TG], F32, "cq%d" % j) for j in range(6)]
    cqn = [kb.sb([128, TG], BF16, "cqn%d" % j) for j in range(6)]
    ckv_s = [kb.sb([128, TG], F32, "ckv%d" % j) for j in range(4)]
    ckvn = [kb.sb([128, TG], BF16, "ckvn%d" % j) for j in range(4)]
    kr_s = kb.sb([64, TG], F32, "kr")
    krs_s = kb.sb([64, TG], F32, "krs")
    krsq = kb.sb([64, TG], BF16, "krsq")
    posi = kb.sb([64, TG], I32, "posi")
    ang = kb.sb([64, TG], F32, "ang")
    angk = kb.sb([64, TG], F32, "angk")
    angi = kb.sb([64, TG], I32, "angi")
    cos2 = kb.sb([64, TG], F32, "cos2")
    sin2 = kb.sb([64, TG], F32, "sin2")
    dts = kb.sb([128, 4, 16], F32, "dts")
    xTv = xT.rearrange("(k p) t -> p k t", p=128)

    def load_wblock(b):
        wb = wbr.next()
        kb.dma("pool", wb[:], winb[b].rearrange("p (k c) -> p k c", k=KD), writes=[wb])
        return wb

    for g in range(NG):
        t0 = g * TG
        tsl = slice(t0, t0 + TG)
        for k in range(KD):
            kb.dma("sp", xk[k][:], xTv[:, k, tsl], writes=[xk[k]])
        kb.dma("sp", posi[:], pos[:, tsl], writes=[posi])
        _emit_norm_mod(kb, xk, hT, modT, onepT, ones, epsb, psr, sqr, tmpr, TG, rstd_x)
        _emit_rope_tables(kb, posi, ang, angk, angi, sin2, cos2, rc_s)
        if dbg and g == 0:
            for k in range(KD):
                kb.dma("act", o_dh[k * 128:(k + 1) * 128, :], hT[k][:], reads=[hT[k]])
            kb.dma("act", o_dm, modT[:], reads=[modT])

        def hp(wb, c0, c1):
            return [(wb[:, k, c0:c1], hT[k][:], [wb, hT[k]]) for k in range(KD)]

        for b in range(12):
            wb = load_wblock(b)
            ps = psr.next()
            kb.mm(ps, ps[:, 0:TG], hp(wb, 0, 128))
            st = stg.next()
            if b % 2 == 0:
                kb.op("act", lambda: nc.scalar.copy(st[:], ps[:, 0:TG]), reads=[ps], writes=[st])
            else:
                kb.op("dve", lambda: nc.vector.tensor_copy(st[:], ps[:, 0:TG]), reads=[ps], writes=[st])
            kb.dma("act", o_xbc[b * 128:(b + 1) * 128, tsl], st[:], reads=[st])

        def lat(b0, nb, dst, dstn, gains, nfeat):
            pss = psr.next()
            for j in range(nb):
                wb = load_wblock(b0 + j)
                ps = psr.next()
                kb.mm(ps, ps[:, 0:TG], hp(wb, 0, 128))
                kb.op("act", lambda: nc.scalar.copy(dst[j][:], ps[:, 0:TG]), reads=[ps], writes=[dst[j]])
                sq = sqr.next()
                kb.op("act", lambda: nc.scalar.activation(sq[:], ps[:, 0:TG], AF.Square), reads=[ps], writes=[sq])
                kb.op("pe", lambda: nc.tensor.matmul(pss[:, 0:TG], ones[:], sq[:], start=(j == 0), stop=(j == nb - 1)),
                      reads=[ones, sq], writes=[pss])
            r = tmpr.next()
            _rstd(kb, pss, r, nfeat, epsb)
            for j in range(nb):
                kb.op("dve", lambda: nc.vector.scalar_tensor_tensor(dstn[j][:], dst[j][:], gains[:, j:j + 1], r[:],
                                                                    ALU.mult, ALU.mult),
                      reads=[dst[j], gains, r], writes=[dstn[j]])
        lat(12, 6, cq_s, cqn, qn_s, 768)
        lat(18, 4, ckv_s, ckvn, kvn_s, 512)

        wb = load_wblock(22)
        ps = psr.next()
        kb.mm(ps, ps[0:64, 0:TG], hp(wb, 0, 64))
        kb.op("act", lambda: nc.scalar.copy(kr_s[:], ps[0:64, 0:TG]), reads=[ps], writes=[kr_s])
        kb.op("act", lambda: nc.scalar.activation(krsq[:], ps[0:64, 0:TG], AF.Square), reads=[ps], writes=[krsq])
        ps = psr.next()
        kb.mm(ps, ps[0:64, 0:TG], hp(wb, 64, 128))
        kb.op("act", lambda: nc.scalar.copy(krs_s[:], ps[0:64, 0:TG]), reads=[ps], writes=[krs_s])

        wb = load_wblock(23)
        ps = psr.next()
        for tt in range(TG // 128):
            kb.mm(ps, ps[:, tt * 16:(tt + 1) * 16],
                  [(hT[k][:, tt * 128:(tt + 1) * 128], wb[:, k, 0:16], [wb, hT[k]]) for k in range(KD)])
        for tt in range(TG // 128):
            kb.op("dve", lambda: nc.vector.tensor_tensor(dts[:, tt, :], ps[:, tt * 16:(tt + 1) * 16], dtb_s[:], ALU.add),
                  reads=[ps, dtb_s], writes=[dts])
        kb.op("act", lambda: nc.scalar.activation(dts[:], dts[:], AF.Exp), reads=[dts], writes=[dts])
        kb.op("act", lambda: nc.scalar.activation(dts[:], dts[:], AF.Ln, bias=1.0, scale=1.0), reads=[dts], writes=[dts])
        kb.dma("act", o_dt[tsl, :].rearrange("(t p) h -> p t h", p=128), dts[:], reads=[dts])

        def head(src_n_pairs, rope_src, gains, dst, h):
            psn = psr.next()
            kb.mm(psn, psn[:, 0:TG], src_n_pairs)
            sqn = sqr.next()
            kb.op("act", lambda: nc.scalar.activation(sqn[:], psn[:, 0:TG], AF.Square), reads=[psn], writes=[sqn])
            if rope_src is None:
                psr_ = psr.next()
                kb.mm(psr_, psr_[0:64, 0:TG], [(wuq_s[:, j, h * 256 + 128:h * 256 + 192], cqn[j][:], [wuq_s, cqn[j]]) for j in range(6)])
                pss_ = psr.next()
                kb.mm(pss_, pss_[0:64, 0:TG], [(wuq_s[:, j, h * 256 + 192:h * 256 + 256], cqn[j][:], [wuq_s, cqn[j]]) for j in range(6)])
                sqr_t = sqr.next()
                kb.op("act", lambda: nc.scalar.activation(sqr_t[0:64, :], psr_[0:64, 0:TG], AF.Square), reads=[psr_], writes=[sqr_t])
                r_ap, s_ap, r_t, s_t = psr_[0:64, 0:TG], pss_[0:64, 0:TG], psr_, pss_
            else:
                sqr_t = krsq
                r_ap, s_ap, r_t, s_t = kr_s[:], krs_s[:], kr_s, krs_s
            pss = psr.next()
            kb.op("pe", lambda: nc.tensor.matmul(pss[:, 0:TG], ones[:], sqn[:], start=True, stop=False),
                  reads=[ones, sqn], writes=[pss], inc=False)
            kb.op("pe", lambda: nc.tensor.matmul(pss[:, 0:TG], ones[0:64, :], sqr_t[0:64, :], start=False, stop=True),
                  reads=[ones, sqr_t], writes=[pss])
            r = tmpr.next()
            _rstd(kb, pss, r, 192, epsb)
            on = stgb.next()
            kb.op("dve", lambda: nc.vector.scalar_tensor_tensor(on[:], psn[:, 0:TG], gains[:, 0:1], r[:], ALU.mult, ALU.mult),
                  reads=[psn, gains, r], writes=[on])
            kb.dma("act", dst[h, 0:128, tsl], on[:], reads=[on])
            t1 = tmpr.next()
            t2 = tmpr.next()
            kb.op("dve", lambda: nc.vector.scalar_tensor_tensor(t1[0:64, :], r_ap, gains[0:64, 1:2], r[0:64, :], ALU.mult, ALU.mult),
                  reads=[r_t, gains, r], writes=[t1])
            kb.op("pool", lambda: nc.gpsimd.tensor_tensor(t1[0:64, :], t1[0:64, :], cos2[:], ALU.mult),
                  reads=[t1, cos2], writes=[t1])
            kb.op("dve", lambda: nc.vector.scalar_tensor_tensor(t2[0:64, :], s_ap, gains[0:64, 2:3], r[0:64, :], ALU.mult, ALU.mult),
                  reads=[s_t, gains, r], writes=[t2])
            kb.op("pool", lambda: nc.gpsimd.tensor_tensor(t2[0:64, :], t2[0:64, :], sin2[:], ALU.mult),
                  reads=[t2, sin2], writes=[t2])
            orp = stgb.next()
            kb.op("dve", lambda: nc.vector.tensor_tensor(orp[0:64, :], t1[0:64, :], t2[0:64, :], ALU.add),
                  reads=[t1, t2], writes=[orp])
            kb.dma("act", dst[h, 128:192, tsl], orp[0:64, :], reads=[orp])

        for h in range(8):
            head([(wuq_s[:, j, h * 256:h * 256 + 128], cqn[j][:], [wuq_s, cqn[j]]) for j in range(6)],
                 None, qg_s, o_q, h)
        for h in range(8):
            head([(wukk_s[:, j, h * 128:(h + 1) * 128], ckvn[j][:], [wukk_s, ckvn[j]]) for j in range(4)],
                 True, kg_s, o_k, h)
        for tt in range(TG // 128):
            for hf in range(2):
                ps = psr.next()
                kb.mm(ps, ps[:, 0:512], [(ckvn[j][:, tt * 128:(tt + 1) * 128], wukv_s[:, j, hf * 512:(hf + 1) * 512],
                                          [ckvn[j], wukv_s]) for j in range(4)])
                vb = stgb.next()
                kb.op("act", lambda: nc.scalar.copy(vb[:, 0:512], ps[:, 0:512]), reads=[ps], writes=[vb])
                kb.dma("act", o_v[t0 + tt * 128:t0 + (tt + 1) * 128, hf * 512:(hf + 1) * 512], vb[:, 0:512], reads=[vb])
    kb.finish()
    return nc


def _pk(v, n):
    return np.ascontiguousarray(np.asarray(v, np.float32).reshape(n, 128).T)


def _blockify(w, nb):
    w = w.reshape(KD, 128, nb, 128)
    return np.ascontiguousarray(w.transpose(2, 1, 0, 3).reshape(nb, 128, KD * 128))


def prep_A(inp, layer, S, mod):
    w_in = inp["w_in"][layer]
    cols = np.zeros((D, NBA * 128), np.float32)
    cols[:, 0:1536] = w_in[:, OFF["xbc"]:OFF["xbc"] + 1536]
    cols[:, 1536:2304] = w_in[:, OFF["cq"]:OFF["cq"] + 768]
    cols[:, 2304:2816] = w_in[:, OFF["ckv"]:OFF["ckv"] + 512]
    kr = w_in[:, OFF["kr"]:OFF["kr"] + 64]
    cols[:, 2816:2880] = kr
    cols[:, 2880:2912] = kr[:, 32:64]
    cols[:, 2912:2944] = kr[:, 0:32]
    cols[:, 2944:2960] = w_in[:, OFF["dt"]:OFF["dt"] + 16]
    wuq = inp["mla_w_uq"][layer].reshape(768, 8, 192)
    wuq2 = np.concatenate([wuq, wuq[:, :, 160:192], wuq[:, :, 128:160]], axis=2).reshape(768, 2048)
    wukv = inp["mla_w_ukv"][layer].reshape(512, 8, 256)

    def gain3(g):
        o = np.zeros((128, 3), np.float32)
        o[:, 0] = g[0:128]
        o[0:64, 1] = g[128:192]
        o[0:32, 2] = g[160:192]
        o[32:64, 2] = g[128:160]
        return o
    half = 32
    invf = (np.float32(10000.0) ** (-np.arange(half, dtype=np.float32) * np.float32(2.0) / np.float32(64))).astype(np.float32)
    rc = np.zeros((64, 4), np.float32)
    rc[:, 0] = np.concatenate([invf, invf])
    sgn = np.concatenate([-np.ones(32), np.ones(32)]).astype(np.float32)
    rc[:, 1] = sgn
    rc[:, 2] = -sgn * np.float32(math.pi)
    rc[:, 3] = -np.float32(math.pi)
    return dict(
        modT=np.ascontiguousarray(mod[:, 0:32]),
        winb=_blockify(cols, NBA),
        dtb_bc=np.ascontiguousarray(np.broadcast_to(inp["ssd_dt_bias"][layer][None, :], (128, 16))).astype(np.float32),
        qnorm_pk=_pk(inp["mla_q_norm"][layer], 6),
        kvnorm_pk=_pk(inp["mla_kv_norm"][layer], 4),
        wuq=np.ascontiguousarray(wuq2),
        wukv_k=np.ascontiguousarray(wukv[:, :, 0:128].reshape(512, 1024)),
        wukv_v=np.ascontiguousarray(wukv[:, :, 128:256].reshape(512, 1024)),
        qgain=gain3(inp["mla_q_gain"][layer]),
        kgain=gain3(inp["mla_k_gain"][layer]),
        ropec=rc,
    )


def run_A(inp, layer, xT, S, NTOK, mod, dbg=False):
    ncore = S // NTOK
    shared = prep_A(inp, layer, S, mod)
    pos = np.asarray(inp["positions"][0, :S]).astype(np.int32)
    maps = []
    for i in range(ncore):
        m = dict(shared)
        m["xT"] = np.ascontiguousarray(xT[:, i * NTOK:(i + 1) * NTOK])
        m["pos_bc"] = np.ascontiguousarray(np.broadcast_to(pos[None, i * NTOK:(i + 1) * NTOK], (64, NTOK)))
        maps.append(m)
    nc = build_A(NTOK, dbg)
    res = run_bass_kernel_spmd(nc, maps, core_ids=list(range(ncore)))
    R = res.results
    if dbg:
        return R
    out = dict(
        xbcT=np.concatenate([r["xbcT"] for r in R], axis=1),
        dt=np.concatenate([r["dt"] for r in R], axis=0),
        qT=np.concatenate([r["qT"] for r in R], axis=2),
        kT=np.concatenate([r["kT"] for r in R], axis=2),
        v=np.concatenate([r["v"] for r in R], axis=0),
    )
    return out


def build_B(S, do_att=True, do_ssd=True, stage=9):
    QG = 512
    NQG = S // QG
    NKB = S // 128
    NCH = S // 128
    nc = bass.Bass("TRN2", target_bir_lowering=False)
    qn_d = _din(nc, "qn", [128, S], BF16)
    qr_d = _din(nc, "qr", [64, S], BF16)
    kn_d = _din(nc, "kn", [128, S], BF16)
    kr_d = _din(nc, "kr", [64, S], BF16)
    v_d = _din(nc, "vb", [128, NKB, 128], BF16)
    mask_d = _din(nc, "masks", [128, 4, QG], BF16)
    slab_d = _din(nc, "slab", [384, S])
    convw_d = _din(nc, "convw", [128, 3, 4])
    convb_d = _din(nc, "convb", [128, 3])
    dt_d = _din(nc, "dth", [128, NCH, 2])
    alog_d = _din(nc, "alog_bc", [128, 2])
    dsk_d = _din(nc, "dskip_bc", [128, 2])
    U_d = _din(nc, "U", [128, 128])
    negm_d = _din(nc, "negmask", [128, 128])
    id_d = _din(nc, "ident", [128, 128])
    o_att = _dout(nc, "yatt", [128, S], BF16)
    o_ssd = _dout(nc, "yssd", [128, S], F32)

    kb = KB(nc)
    V = nc.vector
    A = nc.scalar
    ones = kb.sb([128, 128], BF16, "ones")
    onesf = kb.sb([128, 128], F32, "onesf")
    kb.op("pool", lambda: nc.gpsimd.memset(ones[:], 1.0), writes=[ones])
    kb.op("pool", lambda: nc.gpsimd.memset(onesf[:], 1.0), writes=[onesf])

    def load(ap, shape, dt, name, q="sp"):
        t = kb.sb(shape, dt, name)
        kb.dma(q, t[:], ap, writes=[t])
        return t

    if do_att:
        kn = load(kn_d, [128, S], BF16, "kn")
        kr = load(kr_d, [64, S], BF16, "kr")
        vb = load(v_d, [128, NKB, 128], BF16, "vb")
        masks = load(mask_d, [128, 4, QG], BF16, "masks")
        qnr = kb.ring(2, [128, QG], BF16, "qn")
        qrr = kb.ring(2, [64, QG], BF16, "qr")
        ptr = kb.ring(4, [128, QG], BF16, "pt")
        ps_s = kb.ring(2, [128, QG], F32, "pss", psum=True)
        ps_o = kb.ring(1, [128, QG], F32, "pso", psum=True)
        ps_d = kb.ring(1, [128, QG], F32, "psd", psum=True)
        rec = kb.sb([128, QG], F32, "rec")
        yst = kb.ring(2, [128, QG], BF16, "yst")
        scale = 192.0 ** -0.5
    if do_ssd:
        bX = kb.ps([128, 512], F32, "bankX")
        bY = kb.ps([128, 512], F32, "bankY")
        bZ = [kb.ps([128, 512], F32, "bankZ%d" % h) for h in range(2)]
        p_xt = View(bX, bX[:, 0:128], "p_xt")
        p_bt = View(bX, bX[:, 128:256], "p_bt")
        p_g = View(bX, bX[:, 256:384], "p_g")
        p_acs = View(bX, bX[:, 384:386], "p_acs")
        p_abc = [View(bY, bY[:, h * 128:(h + 1) * 128], "p_abc%d" % h) for h in range(2)]
        p_y = [View(bZ[h], bZ[h][:, 0:128], "p_y%d" % h) for h in range(2)]
        p_s = [View(bZ[h], bZ[h][:, 128:192], "p_s%d" % h) for h in range(2)]
        convw = load(convw_d, [128, 3, 4], F32, "convw")
        convb = load(convb_d, [128, 3], F32, "convb")
        dth = load(dt_d, [128, NCH, 2], F32, "dth")
        alog = load(alog_d, [128, 2], F32, "alog")
        dsk = load(dsk_d, [128, 2], F32, "dsk")
        U = load(U_d, [128, 128], F32, "U")
        negm = load(negm_d, [128, 128], F32, "negm")
        identf = load(id_d, [128, 128], F32, "identf")
        ident = kb.sb([128, 128], BF16, "ident")
        kb.op("dve", lambda: V.tensor_copy(ident[:], identf[:]), reads=[identf], writes=[ident])
        aneg = kb.sb([128, 2], F32, "aneg")
        kb.op("act", lambda: A.activation(aneg[:], alog[:], AF.Exp), reads=[alog], writes=[aneg])
        kb.op("dve", lambda: V.tensor_scalar_mul(aneg[:], aneg[:], -1.0), reads=[aneg], writes=[aneg])
        dI = [kb.sb([128, 128], BF16, "dI%d" % h) for h in range(2)]
        for h in range(2):
            kb.op("dve", lambda: V.tensor_scalar_mul(dI[h][:], identf[:], dsk[:, h:h + 1]), reads=[identf, dsk], writes=[dI[h]])
        CW = 512
        slabr = [kb.ring(2, [128, CW + 3], F32, "slab%d" % j) for j in range(3)]
        accr = kb.ring(2, [128, CW], F32, "cacc")
        xcT = [kb.ring(2, [128, CW], BF16, "xcT%d" % j) for j in range(3)]
        HT = [kb.sb([128, 64], F32, "HT%d" % h) for h in range(2)]
        Hbf = [kb.sb([128, 64], BF16, "Hbf%d" % h) for h in range(2)]
        for h in range(2):
            kb.op("pool", lambda: nc.gpsimd.memset(HT[h][:], 0.0), writes=[HT[h]])
            kb.op("pool", lambda: nc.gpsimd.memset(Hbf[h][:], 0.0), writes=[Hbf[h]])
        xtokr = kb.ring(2, [128, 128], BF16, "xtok")
        btokr = kb.ring(2, [128, 128], BF16, "btok")
        da = kb.ring(2, [128, 2], F32, "da")
        acs = kb.ring(2, [128, 2], F32, "acs")
        darep = kb.ring(2, [128, 128], F32, "darep")
        argr = kb.ring(2, [128, 128], F32, "arg")
        LTr = kb.ring(2, [128, 128], F32, "LT")
        MTr = kb.ring(2, [128, 128], BF16, "MT")
        Er = kb.ring(2, [128, 128], F32, "E")
        CPr = kb.ring(2, [128, 128], BF16, "CP")
        xdtr = kb.ring(2, [128, 64], BF16, "xdt")
        xdtdr = kb.ring(2, [128, 64], BF16, "xdtd")
        cdecr = kb.ring(2, [128, 1], F32, "cdec")
        stmpr = kb.ring(2, [128, 64], F32, "stmp")
        ystg = [kb.ring(2, [64, CW], F32, "ystg%d" % h) for h in range(2)]

    def att_group(g):
        q0 = g * QG
        qn = qnr.next()
        qr = qrr.next()
        kb.dma("sp", qn[:], qn_d[:, q0:q0 + QG], writes=[qn])
        kb.dma("sp", qr[:], qr_d[:, q0:q0 + QG], writes=[qr])
        po = ps_o.next()
        pd = ps_d.next()
        nkb = (g + 1) * 4
        for i in range(nkb):
            ksl = slice(i * 128, (i + 1) * 128)
            ps = ps_s.next()
            kb.op("pe", lambda: nc.tensor.matmul(ps[:], kn[:, ksl], qn[:], start=True, stop=False),
                  reads=[kn, qn], writes=[ps], inc=False)
            kb.op("pe", lambda: nc.tensor.matmul(ps[:], kr[:, ksl], qr[:], start=False, stop=True),
                  reads=[kr, qr], writes=[ps])
            pt = ptr.next()
            kb.op("act", lambda: A.activation(pt[:], ps[:], AF.Exp, scale=scale), reads=[ps], writes=[pt])
            d = i - g * 4
            if d >= 0:
                kb.op("dve", lambda: V.tensor_tensor(pt[:], pt[:], masks[:, d, :], ALU.mult), reads=[pt, masks], writes=[pt])
            kb.op("pe", lambda: nc.tensor.matmul(po[:], vb[:, i, :], pt[:], start=(i == 0), stop=(i == nkb - 1)),
                  reads=[vb, pt], writes=[po], inc=False)
            kb.op("pe", lambda: nc.tensor.matmul(pd[:], ones[:], pt[:], start=(i == 0), stop=(i == nkb - 1)),
                  reads=[ones, pt], writes=[pd])
        kb.op("dve", lambda: V.reciprocal(rec[:], pd[:]), reads=[pd], writes=[rec])
        ys = yst.next()
        kb.op("dve", lambda: V.tensor_tensor(ys[:], po[:], rec[:], ALU.mult), reads=[po, rec], writes=[ys])
        kb.dma("act", o_att[:, q0:q0 + QG], ys[:], reads=[ys])

    def ssd_piece(pc):
        t0 = pc * CW
        cur = []
        for j in range(3):
            sl = slabr[j].next()
            if pc == 0:
                kb.op("pool", lambda: nc.gpsimd.memset(sl[:, 0:3], 0.0), writes=[sl])
                kb.dma("sp", sl[:, 3:CW + 3], slab_d[j * 128:(j + 1) * 128, 0:CW], writes=[sl])
            else:
                kb.dma("sp", sl[:], slab_d[j * 128:(j + 1) * 128, t0 - 3:t0 + CW], writes=[sl])
            acc = accr.next()
            kb.op("dve", lambda: V.tensor_scalar_mul(acc[:], sl[:, 0:CW], convw[:, j, 0:1]), reads=[sl, convw], writes=[acc])
            for k in range(1, 4):
                kb.op("dve", lambda: V.scalar_tensor_tensor(acc[:], sl[:, k:k + CW], convw[:, j, k:k + 1], acc[:], ALU.mult, ALU.add),
                      reads=[sl, convw, acc], writes=[acc])
            o = xcT[j].next()
            kb.op("act", lambda: A.activation(o[:], acc[:], AF.Silu, bias=convb[:, j:j + 1], scale=1.0),
                  reads=[acc, convb], writes=[o])
            cur.append(o)
        xT_, BT_, CT_ = cur
        if stage < 2:
            return
        yst2 = [ystg[h].next() for h in range(2)]
        for cc in range(CW // 128):
            c = pc * (CW // 128) + cc
            csl = slice(cc * 128, (cc + 1) * 128)
            kb.op("pe", lambda: nc.tensor.matmul(p_xt[:], xT_[:, csl], ident[:], start=True, stop=True),
                  reads=[xT_, ident], writes=[p_xt])
            kb.op("pe", lambda: nc.tensor.matmul(p_bt[:], BT_[:, csl], ident[:], start=True, stop=True),
                  reads=[BT_, ident], writes=[p_bt])
            kb.op("pe", lambda: nc.tensor.matmul(p_g[:], BT_[:, csl], CT_[:, csl], start=True, stop=True),
                  reads=[BT_, CT_], writes=[p_g])
            xtok = xtokr.next()
            btok = btokr.next()
            kb.op("act", lambda: A.copy(xtok[:], p_xt[:]), reads=[p_xt], writes=[xtok])
            kb.op("act", lambda: A.copy(btok[:], p_bt[:]), reads=[p_bt], writes=[btok])
            if stage < 3:
                continue
            da_t = da.next()
            kb.op("dve", lambda: V.tensor_tensor(da_t[:], dth[:, c, :], aneg[:], ALU.mult), reads=[dth, aneg], writes=[da_t])
            kb.op("pe", lambda: nc.tensor.matmul(p_acs[:], U[:], da_t[:], start=True, stop=True),
                  reads=[U, da_t], writes=[p_acs])
            dr = []
            for h in range(2):
                drt = darep.next()
                kb.op("act", lambda: A.activation(drt[:], onesf[:], AF.Identity, scale=da_t[:, h:h + 1]),
                      reads=[onesf, da_t], writes=[drt])
                dr.append(drt)
            for h in range(2):
                kb.op("pe", lambda: nc.tensor.matmul(p_abc[h][:], dr[h][:], U[:], start=True, stop=True),
                      reads=[dr[h], U], writes=[p_abc[h]])
            if stage < 4:
                continue
            acs_t = acs.next()
            kb.op("dve", lambda: V.tensor_copy(acs_t[:], p_acs[:]), reads=[p_acs], writes=[acs_t])
            for h in range(2):
                Abc = p_abc[h][:]
                psa = p_abc[h]
                arg = argr.next()
                kb.op("dve", lambda: V.scalar_tensor_tensor(arg[:], Abc, acs_t[:, h:h + 1], negm[:], ALU.subtract, ALU.add),
                      reads=[psa, acs_t, negm], writes=[arg])
                LT = LTr.next()
                kb.op("act", lambda: A.activation(LT[:], arg[:], AF.Exp), reads=[arg], writes=[LT])
                MT = MTr.next()
                kb.op("dve", lambda: V.tensor_tensor(MT[:], p_g[:], LT[:], ALU.mult), reads=[p_g, LT], writes=[MT])
                if stage < 5:
                    continue
                E = Er.next()
                kb.op("act", lambda: A.activation(E[:], Abc, AF.Exp), reads=[psa], writes=[E])
                CP = CPr.next()
                kb.op("pool", lambda: nc.gpsimd.tensor_tensor(CP[:], CT_[:, csl], E[:], ALU.mult), reads=[CT_, E], writes=[CP])
                xdt = xdtr.next()
                kb.op("dve", lambda: V.tensor_scalar_mul(xdt[:], xtok[:, h * 64:(h + 1) * 64], dth[:, c, h:h + 1]),
                      reads=[xtok, dth], writes=[xdt])
                xdtd = xdtdr.next()
                kb.op("dve", lambda: V.tensor_scalar_mul(xdtd[:], xdt[:], LT[:, 127:128]), reads=[xdt, LT], writes=[xdtd])
                cdec = cdecr.next()
                kb.op("act", lambda: A.copy(cdec[:], E[:, 127:128]), reads=[E], writes=[cdec])
                if stage < 6:
                    continue
                psy = p_y[h]
                pss_ = p_s[h]
                kb.op("pe", lambda: nc.tensor.matmul(psy[0:64, 0:128], xdt[:], MT[:], start=True, stop=False),
                      reads=[xdt, MT], writes=[psy], inc=False)
                kb.op("pe", lambda: nc.tensor.matmul(psy[0:64, 0:128], Hbf[h][:], CP[:], start=False, stop=False),
                      reads=[Hbf[h], CP], writes=[psy], inc=False)
                kb.op("pe", lambda: nc.tensor.matmul(psy[0:64, 0:128], xtok[:, h * 64:(h + 1) * 64], dI[h][:], start=False, stop=True),
                      reads=[xtok, dI[h]], writes=[psy], inc=False)
                kb.op("pe", lambda: nc.tensor.matmul(pss_[:], btok[:], xdtd[:], start=True, stop=True),
                      reads=[btok, xdtd], writes=[psy, pss_])
                if stage < 7:
                    continue
                kb.op("act", lambda: A.copy(yst2[h][:, csl], psy[0:64, 0:128]), reads=[psy], writes=[yst2[h]])
                if stage < 8:
                    continue
                stmp = stmpr.next()
                kb.op("act", lambda: A.copy(stmp[:], pss_[:]), reads=[pss_], writes=[stmp])
                kb.op("dve", lambda: V.scalar_tensor_tensor(HT[h][:], HT[h][:], cdec[:, 0:1], stmp[:], ALU.mult, ALU.add),
                      reads=[HT[h], cdec, stmp], writes=[HT[h]])
                kb.op("pool", lambda: nc.gpsimd.tensor_copy(Hbf[h][:], HT[h][:]), reads=[HT[h]], writes=[Hbf[h]])
        if stage < 9:
            return
        for h in range(2):
            kb.dma("act", o_ssd[h * 64:(h + 1) * 64, t0:t0 + CW], yst2[h][:], reads=[yst2[h]])

    for g in range(NQG):
        if do_att:
            att_group(g)
        if do_ssd:
            ssd_piece(g)
    kb.finish()
    return nc


def consts_B():
    k = np.arange(128)[:, None]
    q = np.arange(512)[None, :]
    masks = np.stack([(k + d * 128 <= q) for d in range(4)], axis=1).astype(np.float32).astype(ml_dtypes.bfloat16)
    s = np.arange(128)[:, None]
    l = np.arange(128)[None, :]
    U = (s <= l).astype(np.float32)
    negm = np.where(s <= l, 0.0, -30000.0).astype(np.float32)
    return dict(masks=np.ascontiguousarray(masks), U=U, negmask=negm, ident=np.eye(128, dtype=np.float32))


def run_B(inp, layer, A_out, S, do_att=True, do_ssd=True, stage=9):
    cst = consts_B()
    qT, kT, v = A_out["qT"], A_out["kT"], A_out["v"]
    xbcT, dt = A_out["xbcT"], A_out["dt"]
    cw = inp["ssd_conv_w"][layer]
    cb = inp["ssd_conv_b"][layer]
    NCH = S // 128
    maps = []
    for i in range(8):
        g = i // 4
        rows = np.concatenate([np.arange(i * 128, (i + 1) * 128), 1024 + g * 128 + np.arange(128),
                               1280 + g * 128 + np.arange(128)])
        m = dict(cst)
        m["qn"] = np.ascontiguousarray(qT[i, 0:128])
        m["qr"] = np.ascontiguousarray(qT[i, 128:192])
        m["kn"] = np.ascontiguousarray(kT[i, 0:128])
        m["kr"] = np.ascontiguousarray(kT[i, 128:192])
        m["vb"] = np.ascontiguousarray(v[:, i * 128:(i + 1) * 128].reshape(S // 128, 128, 128).transpose(1, 0, 2))
        m["slab"] = np.ascontiguousarray(xbcT[rows])
        m["convw"] = np.ascontiguousarray(cw[:, rows].T.reshape(3, 128, 4).transpose(1, 0, 2))
        m["convb"] = np.ascontiguousarray(cb[rows].reshape(3, 128).T)
        m["dth"] = np.ascontiguousarray(dt[:, 2 * i:2 * i + 2].reshape(NCH, 128, 2).transpose(1, 0, 2))
        m["alog_bc"] = np.ascontiguousarray(np.broadcast_to(inp["ssd_a_log"][layer][None, 2 * i:2 * i + 2], (128, 2))).astype(np.float32)
        m["dskip_bc"] = np.ascontiguousarray(np.broadcast_to(inp["ssd_d"][layer][None, 2 * i:2 * i + 2], (128, 2))).astype(np.float32)
        maps.append(m)
    nc = build_B(S, do_att, do_ssd, stage)
    res = run_bass_kernel_spmd(nc, maps, core_ids=list(range(8)))
    R = res.results
    return dict(yattT=np.concatenate([r["yatt"] for r in R], axis=0),
                yssdT=np.concatenate([r["yssd"] for r in R], axis=0))


NBC = 96


def build_C1(NTOK, dbg=False):
    TG = 512
    NG = NTOK // TG
    nc = bass.Bass("TRN2", target_bir_lowering=False)
    xTh = _din(nc, "xTh", [D, NTOK + 16])
    yatt_d = _din(nc, "yattT", [1024, NTOK], BF16)
    yssd_d = _din(nc, "yssdT", [1024, NTOK])
    mod_d = _din(nc, "modT", [128, 48])
    winb = _din(nc, "winb", [NBC, 128, KD * 128])
    wpool_d = _din(nc, "wpoolb", [4, 128, 2 * 256])
    pscale_d = _din(nc, "pscale_pk", [128, 8])
    hflag_d = _din(nc, "haloflag", [128, 1])
    pcorr_d = _din(nc, "pcorr", [128, 4, 16])
    snorm_d = _din(nc, "ssdnorm_pk", [128, 8])
    lng_d = _din(nc, "lng_bc", [128, 1024])
    lnb_d = _din(nc, "lnb_bc", [128, 1024])
    wsT_d = _din(nc, "wsT", [128, 8, 128])
    um_d = _din(nc, "Umask", [128, 128])
    bs_d = _din(nc, "bs_row", [1, 1024])
    wbr_d = _din(nc, "wbrb", [4, 16, 128, 8 * 128])
    wout_d = _din(nc, "woutb", [16, 128, KD * 128])
    o_x = _dout(nc, "xmidT", [D, NTOK])
    if dbg:
        o_dy = _dout(nc, "dbg_y", [4, 1024, 512], BF16)
        o_dm = _dout(nc, "dbg_m", [2048, 512], BF16)

    kb = KB(nc)
    V = nc.vector
    A = nc.scalar
    G = nc.gpsimd
    psr = kb.ring(7, [128, 512], F32, "ps", psum=True)
    ps_stat = kb.ps([128, 512], F32, "ps_stat")
    ones = kb.sb([128, 128], BF16, "ones")
    onesf = kb.sb([1, 128], F32, "onesf")
    epsb = kb.sb([128, 1], F32, "eps")
    kb.op("pool", lambda: G.memset(ones[:], 1.0), writes=[ones])
    kb.op("pool", lambda: G.memset(onesf[:], 1.0), writes=[onesf])
    kb.op("pool", lambda: G.memset(epsb[:], EPS), writes=[epsb])
    modT = _load_mod(kb, mod_d, 48)
    onepT = kb.sb([128, 16], F32, "onep")
    kb.op("dve", lambda: V.tensor_scalar_add(onepT[:], modT[:, 16:32], 1.0), reads=[modT], writes=[onepT])

    def load(ap, shape, dt=F32, name="c", q="sp"):
        t = kb.sb(shape, dt, name)
        kb.dma(q, t[:], ap, writes=[t])
        return t
    pscale = load(pscale_d, [128, 8])
    hflag = load(hflag_d, [128, 1])
    pcorr = load(pcorr_d, [128, 4, 16])
    snorm = load(snorm_d, [128, 8])
    lng = load(lng_d, [128, 1024])
    lnb = load(lnb_d, [128, 1024])
    wsTf = load(wsT_d, [128, 8, 128])
    um = load(um_d, [128, 128])
    bs = load(bs_d, [1, 1024])
    wsm = kb.sb([128, 8, 128], BF16, "wsm")
    for gi in range(8):
        kb.op("dve", lambda: V.tensor_tensor(wsm[:, gi, :], wsTf[:, gi, :], um[:], ALU.mult), reads=[wsTf, um], writes=[wsm])
    wpool_s = kb.sb([128, 4, 512], BF16, "wpool")
    for wg in range(4):
        kb.dma("pool", wpool_s[:, wg, :], wpool_d[wg], writes=[wpool_s])

    xkr = kb.ring(4, [128, TG], F32, "xk")
    hT = [kb.sb([128, TG], BF16, "h%d" % k) for k in range(KD)]
    xh = kb.sb([128, KD, 16], F32, "xh")
    hTh = kb.sb([128, KD, 16], BF16, "hTh")
    sqh = kb.sb([128, KD, 16], BF16, "sqh")
    rstd_h = kb.sb([128, 16], F32, "rstdh")
    tmph = kb.sb([128, 16], F32, "tmph")
    rstd_x = kb.sb([128, TG], F32, "rstdx")
    sqr = kb.ring(3, [128, TG], BF16, "sq")
    tmpr = kb.ring(4, [128, TG], F32, "tmp")
    wbr = kb.ring(2, [128, KD, 128], BF16, "wb")
    wbbr = kb.ring(3, [128, 8, 128], BF16, "wbb")
    Ar = kb.ring(2, [128, TG + 16], F32, "A")
    Sr = kb.ring(2, [128, TG + 16], F32, "S")
    plr = kb.ring(4, [128, TG], BF16, "pl")
    ypool = [kb.sb([128, TG], BF16, "ypool%d" % j) for j in range(8)]
    yssd = [kb.sb([128, TG], BF16, "yssd%d" % j) for j in range(8)]
    yssdf = kb.ring(2, [128, TG], F32, "yssdf")
    yatt = [kb.sb([128, TG], BF16, "yatt%d" % j) for j in range(8)]
    ug = [kb.sb([128, TG], BF16, "ug%d" % j) for j in range(8)]
    ysgu = ug
    vt = [kb.sb([128, 1024], F32, "vt%d" % t) for t in range(TG // 128)]
    vl = [kb.sb([128, 1024], BF16, "vl%d" % t) for t in range(TG // 128)]
    vsum = kb.sb([128, 4, 8], F32, "vsum")
    vst = kb.sb([128, 4, 4], F32, "vst")
    junk = kb.sb([128, 1024], BF16, "junk")
    merged = [kb.sb([128, TG], BF16, "mrg%d" % j) for j in range(KD)]
    accr = kb.ring(2, [128, TG], F32, "acc")
    xTv = xTh.rearrange("(k p) t -> p k t", p=128)

    def load_wblock(b):
        wb = wbr.next()
        kb.dma("pool", wb[:], winb[b].rearrange("p (k c) -> p k c", k=KD), writes=[wb])
        return wb

    def hp(wb, c0, c1):
        return [(wb[:, k, c0:c1], hT[k][:], [wb, hT[k]]) for k in range(KD)]

    for g in range(NG):
        t0 = g * TG
        tsl = slice(t0, t0 + TG)
        kb.dma("sp", xh[:], xTv[:, :, t0:t0 + 16], writes=[xh])
        def xload(k):
            xt = xkr.next()
            kb.dma("sp", xt[:], xTv[:, k, t0 + 16:t0 + 16 + TG], writes=[xt])
            return xt
        for j in range(8):
            kb.dma("sp", yatt[j][:], yatt_d[j * 128:(j + 1) * 128, tsl], writes=[yatt[j]])
        _emit_norm_mod2(kb, xload, hT, modT, onepT, ones, epsb, psr, sqr, tmpr, TG, rstd_x)
        kb.op("act", lambda: A.activation(sqh[:], xh[:], AF.Square), reads=[xh], writes=[sqh])
        ps = psr.next()
        kb.mm(ps, ps[:, 0:16], [(ones[:], sqh[:, k, :], [ones, sqh]) for k in range(KD)])
        kb.op("act", lambda: A.activation(rstd_h[:], ps[:, 0:16], AF.Sqrt, bias=epsb[:, 0:1], scale=1.0 / D),
              reads=[ps, epsb], writes=[rstd_h])
        kb.op("dve", lambda: V.reciprocal(rstd_h[:], rstd_h[:]), reads=[rstd_h], writes=[rstd_h])
        for k in range(KD):
            kb.op("dve", lambda: V.scalar_tensor_tensor(tmph[:], xh[:, k, :], onepT[:, k:k + 1], rstd_h[:], ALU.mult, ALU.mult),
                  reads=[xh, onepT, rstd_h], writes=[tmph])
            kb.op("dve", lambda: V.tensor_scalar_add(hTh[:, k, :], tmph[:], modT[:, k:k + 1]), reads=[tmph, modT], writes=[hTh])

        plc = {}
        for blk in range(8):
            plc[blk] = plr.next()
            wb = load_wblock(blk)
            ps = psr.next()
            kb.mm(ps, ps[:, 0:TG], hp(wb, 0, 128))
            ps2 = psr.next()
            kb.mm(ps2, ps2[:, 0:16], [(wb[:, k, :], hTh[:, k, :], [wb, hTh]) for k in range(KD)])
            At = Ar.next()
            kb.op("act", lambda: A.copy(At[:, 16:16 + TG], ps[:, 0:TG]), reads=[ps], writes=[At])
            if g == 0:
                kb.op("dve", lambda: V.tensor_scalar_mul(At[:, 0:16], ps2[:, 0:16], hflag[:, 0:1]), reads=[ps2, hflag], writes=[At])
            else:
                kb.op("dve", lambda: V.tensor_copy(At[:, 0:16], ps2[:, 0:16]), reads=[ps2], writes=[At])
            wi = blk // 2
            W = TG + 16
            cur = At
            sh = 1
            for step in range(wi + 1):
                St = Sr.next()
                eng = "dve" if step % 2 == 0 else "pool"
                E_ = V if eng == "dve" else G
                kb.op(eng, lambda: E_.tensor_tensor(St[:, sh:W], cur[:, sh:W], cur[:, 0:W - sh], ALU.add),
                      reads=[cur], writes=[St])
                cur = St
                sh *= 2
            win = float(2 ** (wi + 1))
            kb.op("dve", lambda: V.scalar_tensor_tensor(plc[blk][:], cur[:, 16:W], 1.0 / win, At[:, 16:W], ALU.mult, ALU.subtract),
                  reads=[cur, At], writes=[plc[blk]])
            if g == 0:
                kb.op("dve", lambda: V.tensor_tensor(cur[:, 16:32], cur[:, 16:32], pcorr[:, wi, :], ALU.mult),
                      reads=[cur, pcorr], writes=[cur])
                kb.op("dve", lambda: V.scalar_tensor_tensor(plc[blk][:, 0:16], cur[:, 16:32], 1.0 / win, At[:, 16:32], ALU.mult, ALU.subtract),
                      reads=[cur, At], writes=[plc[blk]])
            if blk % 2 == 0:
                continue
            wg = blk // 2
            for dh in range(2):
                ps = psr.next()
                kb.mm(ps, ps[:, 0:TG], [(wpool_s[:, wg, kc * 256 + dh * 128:kc * 256 + (dh + 1) * 128], plc[wg * 2 + kc][:],
                                         [wpool_s, plc[wg * 2 + kc]]) for kc in range(2)])
                j = wg * 2 + dh
                kb.op("act", lambda: A.activation(ypool[j][:], ps[:, 0:TG], AF.Identity, scale=pscale[:, j:j + 1]),
                      reads=[ps, pscale], writes=[ypool[j]])

        pss = ps_stat
        for blk in range(8):
            yf = yssdf.next()
            kb.dma("sp", yf[:], yssd_d[blk * 128:(blk + 1) * 128, tsl], writes=[yf])
            wb = load_wblock(8 + blk)
            ps = psr.next()
            kb.mm(ps, ps[:, 0:TG], hp(wb, 0, 128))
            sz = tmpr.next()
            kb.op("act", lambda: A.activation(sz[:], ps[:, 0:TG], AF.Silu), reads=[ps], writes=[sz])
            kb.op("dve", lambda: V.tensor_tensor(sz[:], sz[:], yf[:], ALU.mult), reads=[sz, yf], writes=[sz])
            kb.op("pool", lambda: G.tensor_copy(yssd[blk][:], sz[:]), reads=[sz], writes=[yssd[blk]])
            sq = sqr.next()
            kb.op("act", lambda: A.activation(sq[:], sz[:], AF.Square), reads=[sz], writes=[sq])
            kb.op("pe", lambda: nc.tensor.matmul(pss[:, 0:TG], ones[:], sq[:], start=(blk == 0), stop=(blk == 7)),
                  reads=[ones, sq], writes=[pss])
        r = tmpr.next()
        _rstd(kb, pss, r, 1024, epsb)
        for blk in range(8):
            kb.op("dve", lambda: V.scalar_tensor_tensor(yssd[blk][:], yssd[blk][:], snorm[:, blk:blk + 1], r[:], ALU.mult, ALU.mult),
                  reads=[yssd[blk], snorm, r], writes=[yssd[blk]])

        for blk in range(8):
            wb = load_wblock(16 + blk)
            ps = psr.next()
            kb.mm(ps, ps[:, 0:TG], hp(wb, 0, 128))
            kb.op("act", lambda: A.activation(ug[blk][:], ps[:, 0:TG], AF.Gelu), reads=[ps], writes=[ug[blk]])
        for blk in range(8):
            wb = load_wblock(24 + blk)
            ps = psr.next()
            for tt in range(4):
                kb.mm(ps, ps[:, tt * 128:(tt + 1) * 128],
                      [(hT[k][:, tt * 128:(tt + 1) * 128], wb[:, k, :], [wb, hT[k]]) for k in range(KD)])
            for tt in range(4):
                kb.op("act", lambda: A.activation(vt[tt][:, blk * 128:(blk + 1) * 128], ps[:, tt * 128:(tt + 1) * 128], AF.Gelu,
                                                  accum_out=vsum[:, tt, blk:blk + 1]),
                      reads=[ps], writes=[vt[tt], vsum])
        for tt in range(4):
            kb.op("dve", lambda: V.reduce_sum(vst[:, tt, 0:1], vsum[:, tt, :], axis=AX.X), reads=[vsum], writes=[vst])
            kb.op("dve", lambda: V.tensor_scalar_mul(vst[:, tt, 0:1], vst[:, tt, 0:1], 1.0 / 1024), reads=[vst], writes=[vst])
            kb.op("dve", lambda: V.tensor_scalar_sub(vt[tt][:], vt[tt][:], vst[:, tt, 0:1]), reads=[vt[tt], vst], writes=[vt[tt]])
            kb.op("act", lambda: A.activation(junk[:], vt[tt][:], AF.Square, accum_out=vst[:, tt, 1:2]),
                  reads=[vt[tt]], writes=[junk, vst])
            kb.op("act", lambda: A.activation(vst[:, tt, 2:3], vst[:, tt, 1:2], AF.Sqrt, bias=epsb[:, 0:1], scale=1.0 / 1024),
                  reads=[vst, epsb], writes=[vst])
            kb.op("dve", lambda: V.reciprocal(vst[:, tt, 3:4], vst[:, tt, 2:3]), reads=[vst], writes=[vst])
            kb.op("dve", lambda: V.scalar_tensor_tensor(vt[tt][:], vt[tt][:], vst[:, tt, 3:4], lng[:], ALU.mult, ALU.mult),
                  reads=[vt[tt], vst, lng], writes=[vt[tt]])
            kb.op("pool", lambda: G.tensor_tensor(vl[tt][:], vt[tt][:], lnb[:], ALU.add), reads=[vt[tt], lnb], writes=[vl[tt]])
        for gi in range(8):
            ps = psr.next()
            for tt in range(4):
                kb.op("pe", lambda: nc.tensor.matmul(ps[:, tt * 128:(tt + 1) * 128], vl[tt][:, gi * 128:(gi + 1) * 128], wsm[:, gi, :],
                                                     start=True, stop=False), reads=[vl[tt], wsm], writes=[ps], inc=False)
                kb.op("pe", lambda: nc.tensor.matmul(ps[:, tt * 128:(tt + 1) * 128], onesf[0:1, :], bs[0:1, gi * 128:(gi + 1) * 128],
                                                     start=False, stop=True), reads=[onesf, bs], writes=[ps], inc=(tt == 3))
            kb.op("dve", lambda: V.tensor_tensor(ysgu[gi][:], ps[:, 0:TG], ug[gi][:], ALU.mult), reads=[ps, ug[gi]], writes=[ysgu[gi]])

        ybr = [ypool, yssd, yatt, ysgu]
        if dbg and g == 0:
            for b in range(4):
                for j in range(8):
                    kb.dma("act", o_dy[b, j * 128:(j + 1) * 128, :], ybr[b][j][:], reads=[ybr[b][j]])
        for dc in range(KD):
            acc = accr.next()
            for b in range(4):
                wbb = wbbr.next()
                kb.dma("pool", wbb[:], wbr_d[b, dc].rearrange("p (k c) -> p k c", k=8), writes=[wbb])
                psP = psr.next()
                kb.mm(psP, psP[:, 0:TG], [(wbb[:, j, :], ybr[b][j][:], [wbb, ybr[b][j]]) for j in range(8)])
                wg_ = load_wblock(32 + b * 16 + dc)
                psG = psr.next()
                kb.mm(psG, psG[:, 0:TG], hp(wg_, 0, 128))
                sg = tmpr.next()
                kb.op("act", lambda: A.activation(sg[:], psG[:, 0:TG], AF.Sigmoid), reads=[psG], writes=[sg])
                if b == 0:
                    kb.op("dve", lambda: V.tensor_tensor(acc[:], psP[:, 0:TG], sg[:], ALU.mult), reads=[psP, sg], writes=[acc])
                else:
                    kb.op("dve", lambda: V.tensor_tensor(sg[:], psP[:, 0:TG], sg[:], ALU.mult), reads=[psP, sg], writes=[sg])
                    if b < 3:
                        kb.op("pool", lambda: G.tensor_tensor(acc[:], acc[:], sg[:], ALU.add), reads=[acc, sg], writes=[acc])
                    else:
                        kb.op("pool", lambda: G.tensor_tensor(merged[dc][:], acc[:], sg[:], ALU.add), reads=[acc, sg], writes=[merged[dc]])
        if dbg and g == 0:
            for j in range(KD):
                kb.dma("act", o_dm[j * 128:(j + 1) * 128, :], merged[j][:], reads=[merged[j]])
        for dc in range(KD):
            wo = wbr.next()
            kb.dma("pool", wo[:], wout_d[dc].rearrange("p (k c) -> p k c", k=KD), writes=[wo])
            ps = psr.next()
            kb.mm(ps, ps[:, 0:TG], [(wo[:, k, :], merged[k][:], [wo, merged[k]]) for k in range(KD)])
            xt = xload(dc)
            kb.op("dve", lambda: V.scalar_tensor_tensor(xt[:], ps[:, 0:TG], modT[:, 32 + dc:33 + dc], xt[:], ALU.mult, ALU.add),
                  reads=[ps, modT, xt], writes=[xt])
            kb.dma("act", o_x[dc * 128:(dc + 1) * 128, tsl], xt[:], reads=[xt])
    kb.finish()
    return nc


def prep_C1(inp, layer, mod):
    w_in = inp["w_in"][layer]
    cols = np.concatenate([w_in[:, OFF["a_in"]:OFF["a_in"] + 1024], w_in[:, OFF["z"]:OFF["z"] + 1024],
                           w_in[:, OFF["uv"]:OFF["uv"] + 2048], w_in[:, OFF["gates"]:OFF["gates"] + 8192]], axis=1)
    wp = inp["w_pool"][layer]
    wpoolb = np.ascontiguousarray(wp.reshape(4, 2, 128, 256).transpose(0, 2, 1, 3).reshape(4, 128, 512))
    wbr = inp["w_branch"][layer]
    wbrb = np.ascontiguousarray(wbr.reshape(4, 8, 128, 16, 128).transpose(0, 3, 2, 1, 4).reshape(4, 16, 128, 1024))
    wout = inp["w_out"][layer]
    s = np.arange(128)[:, None]
    t = np.arange(128)[None, :]
    return dict(
        modT=np.ascontiguousarray(mod[:, 0:48]),
        winb=_blockify(np.ascontiguousarray(cols), NBC),
        wpoolb=wpoolb,
        pscale_pk=_pk(inp["pool_scale"][layer], 8),
        ssdnorm_pk=_pk(inp["ssd_norm"][layer], 8),
        lng_bc=np.ascontiguousarray(np.broadcast_to(inp["sgu_ln_gain"][layer][None, :], (128, 1024))).astype(np.float32),
        lnb_bc=np.ascontiguousarray(np.broadcast_to(inp["sgu_ln_bias"][layer][None, :], (128, 1024))).astype(np.float32),
        wsT=np.ascontiguousarray(inp["sgu_w_s"][layer].transpose(2, 0, 1)),
        Umask=(s <= t).astype(np.float32),
        bs_row=np.ascontiguousarray(inp["sgu_b_s"][layer].reshape(1, 1024)),
        wbrb=wbrb,
        woutb=_blockify(np.ascontiguousarray(wout), 16),
    )


def run_C1(inp, layer, xT, B_out, S, NTOK, mod, dbg=False):
    ncore = S // NTOK
    shared = prep_C1(inp, layer, mod)
    xTh = np.concatenate([np.zeros((D, 16), np.float32), xT], axis=1)
    maps = []
    for i in range(ncore):
        m = dict(shared)
        m["xTh"] = np.ascontiguousarray(xTh[:, i * NTOK:(i + 1) * NTOK + 16])
        m["yattT"] = np.ascontiguousarray(B_out["yattT"][:, i * NTOK:(i + 1) * NTOK])
        m["yssdT"] = np.ascontiguousarray(B_out["yssdT"][:, i * NTOK:(i + 1) * NTOK])
        m["haloflag"] = np.full((128, 1), 0.0 if i == 0 else 1.0, np.float32)
        pc = np.ones((128, 4, 16), np.float32)
        if i == 0:
            for wi, win in enumerate((2, 4, 8, 16)):
                tt = np.arange(16)
                pc[:, wi, :] = (win / np.minimum(tt + 1, win))[None, :]
        m["pcorr"] = pc
        maps.append(m)
    nc = build_C1(NTOK, dbg)
    res = run_bass_kernel_spmd(nc, maps, core_ids=list(range(ncore)))
    if dbg:
        return res.results
    return np.concatenate([r["xmidT"] for r in res.results], axis=1)


def build_C2(NTOK, NF, expert):
    TG = 512
    NG = NTOK // TG
    nc = bass.Bass("TRN2", target_bir_lowering=False)
    xT = _din(nc, "xT", [D, NTOK])
    mod_d = _din(nc, "modT", [128, 48])
    w1_d = _din(nc, "w1b", [NF, 128, KD * 128])
    w3_d = _din(nc, "w3b", [NF, 128, KD * 128])
    w2_d = _din(nc, "w2b", [16, 128, NF * 128])
    if expert:
        wrow_d = _din(nc, "wrow", [1, NTOK])
    o_x = _dout(nc, "xoutT", [D, NTOK])

    kb = KB(nc)
    V = nc.vector
    A = nc.scalar
    G = nc.gpsimd
    psr = kb.ring(8, [128, 512], F32, "ps", psum=True)
    ones = kb.sb([128, 128], BF16, "ones")
    onesf = kb.sb([1, 128], F32, "onesf")
    epsb = kb.sb([128, 1], F32, "eps")
    kb.op("pool", lambda: G.memset(ones[:], 1.0), writes=[ones])
    kb.op("pool", lambda: G.memset(onesf[:], 1.0), writes=[onesf])
    kb.op("pool", lambda: G.memset(epsb[:], EPS), writes=[epsb])
    modT = _load_mod(kb, mod_d, 48)
    onepT = kb.sb([128, 16], F32, "onep")
    kb.op("dve", lambda: V.tensor_scalar_add(onepT[:], modT[:, 16:32], 1.0), reads=[modT], writes=[onepT])

    xk = [kb.sb([128, TG], F32, "x%d" % k) for k in range(KD)]
    hT = [kb.sb([128, TG], BF16, "h%d" % k) for k in range(KD)]
    rstd_x = kb.sb([128, TG], F32, "rstdx")
    sqr = kb.ring(3, [128, TG], BF16, "sq")
    tmpr = kb.ring(4, [128, TG], F32, "tmp")
    wbr = kb.ring(4, [128, KD, 128], BF16, "wb")
    w2r = kb.ring(2, [128, NF, 128], BF16, "w2")
    gt = [kb.sb([128, TG], BF16, "g%d" % f) for f in range(NF)]
    if expert:
        wrow = kb.sb([1, TG], F32, "wrow")
        wbc = kb.sb([128, TG], F32, "wbc")
    xTv = xT.rearrange("(k p) t -> p k t", p=128)

    for g in range(NG):
        t0 = g * TG
        tsl = slice(t0, t0 + TG)
        for k in range(KD):
            kb.dma("sp", xk[k][:], xTv[:, k, tsl], writes=[xk[k]])
        if expert:
            kb.dma("sp", wrow[:], wrow_d[:, tsl], writes=[wrow])
            ps = psr.next()
            kb.op("pe", lambda: nc.tensor.matmul(ps[:, 0:TG], onesf[0:1, :], wrow[0:1, :], start=True, stop=True),
                  reads=[onesf, wrow], writes=[ps])
            kb.op("act", lambda: A.copy(wbc[:], ps[:, 0:TG]), reads=[ps], writes=[wbc])
        _emit_norm_mod2(kb, lambda k: xk[k], hT, modT, onepT, ones, epsb, psr, sqr, tmpr, TG, rstd_x)
        for f in range(NF):
            w1 = wbr.next()
            kb.dma("pool", w1[:], w1_d[f].rearrange("p (k c) -> p k c", k=KD), writes=[w1])
            w3 = wbr.next()
            kb.dma("pool", w3[:], w3_d[f].rearrange("p (k c) -> p k c", k=KD), writes=[w3])
            p1 = psr.next()
            kb.mm(p1, p1[:, 0:TG], [(w1[:, k, :], hT[k][:], [w1, hT[k]]) for k in range(KD)])
            p3 = psr.next()
            kb.mm(p3, p3[:, 0:TG], [(w3[:, k, :], hT[k][:], [w3, hT[k]]) for k in range(KD)])
            s1 = tmpr.next()
            kb.op("act", lambda: A.activation(s1[:], p1[:, 0:TG], AF.Silu), reads=[p1], writes=[s1])
            if expert:
                kb.op("pool", lambda: G.tensor_tensor(s1[:], s1[:], wbc[:], ALU.mult), reads=[s1, wbc], writes=[s1])
            kb.op("dve", lambda: V.tensor_tensor(gt[f][:], p3[:, 0:TG], s1[:], ALU.mult), reads=[p3, s1], writes=[gt[f]])
        for dc in range(KD):
            w2 = w2r.next()
            kb.dma("pool", w2[:], w2_d[dc].rearrange("p (f c) -> p f c", f=NF), writes=[w2])
            ps = psr.next()
            kb.mm(ps, ps[:, 0:TG], [(w2[:, f, :], gt[f][:], [w2, gt[f]]) for f in range(NF)])
            if expert:
                kb.op("act", lambda: A.activation(xk[dc][:], ps[:, 0:TG], AF.Identity, scale=modT[:, 32 + dc:33 + dc]),
                      reads=[ps, modT], writes=[xk[dc]])
            else:
                kb.op("dve", lambda: V.scalar_tensor_tensor(xk[dc][:], ps[:, 0:TG], modT[:, 32 + dc:33 + dc], xk[dc][:], ALU.mult, ALU.add),
                      reads=[ps, modT, xk[dc]], writes=[xk[dc]])
            kb.dma("act", o_x[dc * 128:(dc + 1) * 128, tsl], xk[dc][:], reads=[xk[dc]])
    kb.finish()
    return nc


def _blockify_w2(w2, nf):
    w = w2.reshape(nf, 128, 16, 128)
    return np.ascontiguousarray(w.transpose(2, 1, 0, 3).reshape(16, 128, nf * 128))


def run_C2_dense(inp, layer, xT, S, NTOK, mod):
    ncore = S // NTOK
    idx = layer // 2
    nf = 5632 // 128
    shared = dict(modT=np.ascontiguousarray(mod[:, 48:96]),
                  w1b=_blockify(np.ascontiguousarray(inp["ffn_w1"][idx]), nf),
                  w3b=_blockify(np.ascontiguousarray(inp["ffn_w3"][idx]), nf),
                  w2b=_blockify_w2(inp["ffn_w2"][idx], nf))
    maps = []
    for i in range(ncore):
        m = dict(shared)
        m["xT"] = np.ascontiguousarray(xT[:, i * NTOK:(i + 1) * NTOK])
        maps.append(m)
    res = run_bass_kernel_spmd(build_C2(NTOK, nf, False), maps, core_ids=list(range(ncore)))
    return np.concatenate([r["xoutT"] for r in res.results], axis=1)


def build_R(NTOK):
    TG = 512
    NG = NTOK // TG
    nc = bass.Bass("TRN2", target_bir_lowering=False)
    xT = _din(nc, "xT", [D, NTOK])
    mod_d = _din(nc, "modT", [128, 48])
    wr_d = _din(nc, "wr_pk", [128, KD, 8])
    o_w = _dout(nc, "wt", [NTOK, 8])
    kb = KB(nc)
    V = nc.vector
    A = nc.scalar
    G = nc.gpsimd
    psr = kb.ring(4, [128, 512], F32, "ps", psum=True)
    ones = kb.sb([128, 128], BF16, "ones")
    epsb = kb.sb([128, 1], F32, "eps")
    kb.op("pool", lambda: G.memset(ones[:], 1.0), writes=[ones])
    kb.op("pool", lambda: G.memset(epsb[:], EPS), writes=[epsb])
    modT = _load_mod(kb, mod_d, 48)
    onepT = kb.sb([128, 16], F32, "onep")
    kb.op("dve", lambda: V.tensor_scalar_add(onepT[:], modT[:, 16:32], 1.0), reads=[modT], writes=[onepT])
    wr = kb.sb([128, KD, 8], F32, "wr")
    kb.dma("sp", wr[:], wr_d, writes=[wr])
    xk = [kb.sb([128, TG], F32, "x%d" % k) for k in range(KD)]
    h32 = [kb.sb([128, TG], F32, "h%d" % k) for k in range(KD)]
    sqr = kb.ring(3, [128, TG], BF16, "sq")
    rstd = kb.sb([128, TG], F32, "rstd")
    lg = kb.sb([128, 4, 8], F32, "lg")
    l2 = kb.sb([128, 4, 8], F32, "l2")
    mk1 = kb.sb([128, 4, 8], F32, "mk1")
    mk2 = kb.sb([128, 4, 8], F32, "mk2")
    wt = kb.sb([128, 4, 8], F32, "wt")
    mm_ = kb.sb([128, 4, 4], F32, "mm_")
    xTv = xT.rearrange("(k p) t -> p k t", p=128)
    for g in range(NG):
        tsl = slice(g * TG, (g + 1) * TG)
        for k in range(KD):
            kb.dma("sp", xk[k][:], xTv[:, k, tsl], writes=[xk[k]])
        ps = psr.next()
        for k in range(KD):
            sq = sqr.next()
            kb.op("act", lambda: A.activation(sq[:], xk[k][:], AF.Square), reads=[xk[k]], writes=[sq])
            kb.op("pe", lambda: nc.tensor.matmul(ps[:, 0:TG], ones[:], sq[:], start=(k == 0), stop=(k == KD - 1)),
                  reads=[ones, sq], writes=[ps])
        _rstd(kb, ps, rstd, D, epsb)
        for k in range(KD):
            kb.op("dve", lambda: V.tensor_tensor(h32[k][:], xk[k][:], rstd[:], ALU.mult), reads=[xk[k], rstd], writes=[h32[k]])
            kb.op("act", lambda: A.activation(h32[k][:], h32[k][:], AF.Identity, bias=modT[:, k:k + 1], scale=onepT[:, k:k + 1]),
                  reads=[h32[k], modT, onepT], writes=[h32[k]])
        ps = psr.next()
        for tt in range(4):
            kb.mm(ps, ps[:, tt * 8:(tt + 1) * 8],
                  [(h32[k][:, tt * 128:(tt + 1) * 128], wr[:, k, :], [h32[k], wr]) for k in range(KD)])
        kb.op("dve", lambda: V.tensor_copy(lg[:].rearrange("p a b -> p (a b)"), ps[:, 0:32]), reads=[ps], writes=[lg])
        for tt in range(4):
            kb.op("dve", lambda: V.reduce_max(mm_[:, tt, 0:1], lg[:, tt, :], axis=AX.X), reads=[lg], writes=[mm_])
            kb.op("dve", lambda: V.tensor_scalar(mk1[:, tt, :], lg[:, tt, :], mm_[:, tt, 0:1], None, ALU.is_equal),
                  reads=[lg, mm_], writes=[mk1])
            kb.op("dve", lambda: V.scalar_tensor_tensor(l2[:, tt, :], mk1[:, tt, :], -1e30, lg[:, tt, :], ALU.mult, ALU.add),
                  reads=[mk1, lg], writes=[l2])
            kb.op("dve", lambda: V.reduce_max(mm_[:, tt, 1:2], l2[:, tt, :], axis=AX.X), reads=[l2], writes=[mm_])
            kb.op("dve", lambda: V.tensor_scalar(mk2[:, tt, :], l2[:, tt, :], mm_[:, tt, 1:2], None, ALU.is_equal),
                  reads=[l2, mm_], writes=[mk2])
            kb.op("dve", lambda: V.tensor_tensor(mm_[:, tt, 2:3], mm_[:, tt, 1:2], mm_[:, tt, 0:1], ALU.subtract), reads=[mm_], writes=[mm_])
            kb.op("act", lambda: A.activation(mm_[:, tt, 2:3], mm_[:, tt, 2:3], AF.Exp), reads=[mm_], writes=[mm_])
            kb.op("dve", lambda: V.tensor_scalar_add(mm_[:, tt, 3:4], mm_[:, tt, 2:3], 1.0), reads=[mm_], writes=[mm_])
            kb.op("dve", lambda: V.reciprocal(mm_[:, tt, 3:4], mm_[:, tt, 3:4]), reads=[mm_], writes=[mm_])
            kb.op("dve", lambda: V.tensor_tensor(mm_[:, tt, 2:3], mm_[:, tt, 2:3], mm_[:, tt, 3:4], ALU.mult), reads=[mm_], writes=[mm_])
            kb.op("dve", lambda: V.tensor_scalar_mul(wt[:, tt, :], mk1[:, tt, :], mm_[:, tt, 3:4]), reads=[mk1, mm_], writes=[wt])
            kb.op("dve", lambda: V.scalar_tensor_tensor(wt[:, tt, :], mk2[:, tt, :], mm_[:, tt, 2:3], wt[:, tt, :], ALU.mult, ALU.add),
                  reads=[mk2, mm_, wt], writes=[wt])
        kb.dma("act", o_w[tsl, :].rearrange("(t p) e -> p t e", p=128), wt[:], reads=[wt])
    kb.finish()
    return nc


def run_R(inp, layer, xT, S, NTOK, mod):
    ncore = S // NTOK
    idx = layer // 2
    shared = dict(modT=np.ascontiguousarray(mod[:, 48:96]),
                  wr_pk=np.ascontiguousarray(inp["moe_router"][idx].reshape(KD, 128, 8).transpose(1, 0, 2)))
    maps = []
    for i in range(ncore):
        m = dict(shared)
        m["xT"] = np.ascontiguousarray(xT[:, i * NTOK:(i + 1) * NTOK])
        maps.append(m)
    res = run_bass_kernel_spmd(build_R(NTOK), maps, core_ids=list(range(ncore)))
    return np.concatenate([r["wt"] for r in res.results], axis=0)


def build_S(NTOK):
    TG = 512
    nc = bass.Bass("TRN2", target_bir_lowering=False)
    xT = _din(nc, "xT", [D, NTOK])
    y0 = _din(nc, "y0T", [D, NTOK])
    y1 = _din(nc, "y1T", [D, NTOK])
    o = _dout(nc, "xoutT", [D, NTOK])
    kb = KB(nc)
    V = nc.vector
    ar = kb.ring(3, [128, TG], F32, "a")
    br = kb.ring(3, [128, TG], F32, "b")
    cr = kb.ring(3, [128, TG], F32, "c")
    for k in range(KD):
        for g in range(NTOK // TG):
            rs = slice(k * 128, (k + 1) * 128)
            tsl = slice(g * TG, (g + 1) * TG)
            a, b, c = ar.next(), br.next(), cr.next()
            kb.dma("sp", a[:], xT[rs, tsl], writes=[a])
            kb.dma("sp", b[:], y0[rs, tsl], writes=[b])
            kb.dma("sp", c[:], y1[rs, tsl], writes=[c])
            kb.op("dve", lambda: V.tensor_tensor(b[:], b[:], c[:], ALU.add), reads=[b, c], writes=[b])
            kb.op("dve", lambda: V.tensor_tensor(a[:], a[:], b[:], ALU.add), reads=[a, b], writes=[a])
            kb.dma("act", o[rs, tsl], a[:], reads=[a])
    kb.finish()
    return nc


def run_moe(inp, layer, xT, S, NTOK, mod):
    idx = layer // 2
    nf = 7168 // 128
    wt = run_R(inp, layer, xT, S, NTOK, mod)
    sel = wt > 0
    lists = [np.nonzero(sel[:, e])[0] for e in range(8)]
    cap = max(512, int(-(-max(len(l) for l in lists) // 512) * 512))
    maps = []
    for e in range(8):
        tl = lists[e]
        xg = np.zeros((D, cap), np.float32)
        xg[:, :len(tl)] = xT[:, tl]
        wrow = np.zeros((1, cap), np.float32)
        wrow[0, :len(tl)] = wt[tl, e]
        maps.append(dict(xT=xg, wrow=wrow, modT=np.ascontiguousarray(mod[:, 48:96]),
                         w1b=_blockify(np.ascontiguousarray(inp["moe_w1"][idx, e]), nf),
                         w3b=_blockify(np.ascontiguousarray(inp["moe_w3"][idx, e]), nf),
                         w2b=_blockify_w2(inp["moe_w2"][idx, e], nf)))
    res = run_bass_kernel_spmd(build_C2(cap, nf, True), maps, core_ids=list(range(8)))
    y = [np.zeros((D, S), np.float32), np.zeros((D, S), np.float32)]
    nsel = np.zeros(S, np.int64)
    for e in range(8):
        tl = lists[e]
        ye = res.results[e]["xoutT"][:, :len(tl)]
        slot = nsel[tl]
        for sidx in (0, 1):
            m = slot == sidx
            y[sidx][:, tl[m]] = ye[:, m]
        nsel[tl] += 1
    ncore = S // NTOK
    maps = []
    for i in range(ncore):
        sl = slice(i * NTOK, (i + 1) * NTOK)
        maps.append(dict(xT=np.ascontiguousarray(xT[:, sl]), y0T=np.ascontiguousarray(y[0][:, sl]), y1T=np.ascontiguousarray(y[1][:, sl])))
    res = run_bass_kernel_spmd(build_S(NTOK), maps, core_ids=list(range(ncore)))
    return np.concatenate([r["xoutT"] for r in res.results], axis=1)


def kernel(**inp):
    inp = {k: np.asarray(v) for k, v in inp.items()}
    S = inp["x"].shape[1]
    NTOK = S // 8
    xT = np.ascontiguousarray(inp["x"][0].T)
    mods = run_M(inp)
    for layer in range(2):
        A_out = run_A(inp, layer, xT, S, NTOK, mods[layer])
        B_out = run_B(inp, layer, A_out, S)
        del A_out
        xmid = run_C1(inp, layer, xT, B_out, S, NTOK, mods[layer])
        del B_out
        if layer % 2 == 0:
            xT = run_C2_dense(inp, layer, xmid, S, NTOK, mods[layer])
        else:
            xT = run_moe(inp, layer, xmid, S, NTOK, mods[layer])
    return np.ascontiguousarray(xT.T)[None].astype(np.float32)
```

```python
import math
import numpy as np
import ml_dtypes
import concourse.bass as bass
import concourse.mybir as mybir
from concourse.bass_utils import run_bass_kernel_spmd

F32 = mybir.dt.float32
BF16 = mybir.dt.bfloat16
I32 = mybir.dt.int32
AF = mybir.ActivationFunctionType
ALU = mybir.AluOpType
AX = mybir.AxisListType

D = 2048
KD = 16
EPS = 1e-6
OFF = dict(a_in=0, z=1024, xbc=2048, dt=3584, cq=3600, ckv=4368, kr=4880, uv=4944, gates=6992)


class Tl:
    __slots__ = ("t", "name", "last_w", "readers", "root")

    def __init__(self, t, name):
        self.t = t
        self.name = name
        self.last_w = None
        self.readers = {}
        self.root = self

    def __getitem__(self, idx):
        return self.t[idx]


class View:
    __slots__ = ("t", "name", "root")

    def __init__(self, parent, ap, name):
        self.t = ap
        self.name = name
        self.root = parent.root

    def __getitem__(self, idx):
        return self.t[idx]


class Ring:
    def __init__(self, tiles):
        self.tiles = tiles
        self.i = 0

    def next(self):
        t = self.tiles[self.i]
        self.i = (self.i + 1) % len(self.tiles)
        return t


class KB:
    NDMA = 6

    def __init__(self, nc):
        self.nc = nc
        self.E = {"pe": nc.tensor, "act": nc.scalar, "dve": nc.vector,
                  "pool": nc.gpsimd, "sp": nc.sync}
        self.sems = {}
        self.cnt = {}
        for k in self.E:
            self.sems[k] = nc.alloc_semaphore("c_" + k)
            self.cnt[k] = 0
        self.dma_rr = {}
        for q in ("sp", "pool", "act"):
            self.dma_rr[q] = 0
            for i in range(self.NDMA):
                key = ("d", q, i)
                self.sems[key] = nc.alloc_semaphore("d_%s_%d" % (q, i))
                self.cnt[key] = 0
        self.seen = {k: {} for k in self.E}
        self.ntile = 0
        self.pending_out = []

    def sb(self, shape, dt, name="t"):
        self.ntile += 1
        return Tl(self.nc.alloc_sbuf_tensor("%s_%d" % (name, self.ntile), list(shape), dt), name)

    def ps(self, shape, dt=F32, name="p"):
        self.ntile += 1
        return Tl(self.nc.alloc_psum_tensor("%s_%d" % (name, self.ntile), list(shape), dt), name)

    def ring(self, n, shape, dt, name="r", psum=False):
        f = self.ps if psum else self.sb
        return Ring([f(shape, dt, name) for _ in range(n)])

    def _wait(self, e, deps):
        need = {}
        for d in deps:
            if d is None:
                continue
            k, v = d
            if k == e and e == "pe":
                continue
            if need.get(k, 0) < v:
                need[k] = v
        seen = self.seen[e]
        for k, v in need.items():
            if seen.get(k, 0) >= v:
                continue
            self.E[e].wait_ge(self.sems[k], v)
            seen[k] = v

    def _deps(self, reads, writes):
        deps = []
        for r in reads:
            deps.append(r.root.last_w)
        for w in writes:
            deps.append(w.root.last_w)
            deps.extend(w.root.readers.items())
        return deps

    def _commit(self, tok, reads, writes):
        writes = [w.root for w in writes]
        reads = [r.root for r in reads]
        for w in writes:
            w.last_w = tok
            w.readers = {}
        k, v = tok
        for r in reads:
            if r in writes:
                continue
            if r.readers.get(k, 0) < v:
                r.readers[k] = v

    def op(self, e, fn, reads=(), writes=(), inc=True):
        self._wait(e, self._deps(reads, writes))
        ins = fn()
        if inc:
            self.cnt[e] += 1
            ins.then_inc(self.sems[e], 1)
            tok = (e, self.cnt[e])
        else:
            tok = (e, self.cnt[e] + 1)
        self._commit(tok, reads, writes)
        return ins

    def dma(self, q, out, in_, reads=(), writes=(), **kw):
        i = self.dma_rr[q]
        self.dma_rr[q] = (i + 1) % self.NDMA
        key = ("d", q, i)
        deps = self._deps(reads, writes)
        deps.append((key, self.cnt[key]))
        self._wait(q, deps)
        ins = self.E[q].dma_start(out=out, in_=in_, **kw)
        self.cnt[key] += 16
        ins.then_inc(self.sems[key], 16)
        tok = (key, self.cnt[key])
        self._commit(tok, reads, writes)
        if not writes:
            self.pending_out.append(tok)
        return ins

    def finish(self):
        last = {}
        for k, v in self.pending_out:
            last[k] = max(last.get(k, 0), v)
        self._wait("sp", list(last.items()))
        self._wait("sp", [(k, self.cnt[k]) for k in ("pe", "act", "dve", "pool") if self.cnt[k]])

    def mm(self, ps, out_ap, pairs):
        nc = self.nc
        n = len(pairs)
        for i, (l, r, rd) in enumerate(pairs):
            self.op("pe", lambda: nc.tensor.matmul(out_ap, l, r, start=(i == 0), stop=(i == n - 1)),
                    reads=rd, writes=[ps], inc=(i == n - 1))


def _din(nc, name, shape, dt=F32):
    return nc.dram_tensor(name, list(shape), dt, kind="ExternalInput").ap()


def _dout(nc, name, shape, dt=F32):
    return nc.dram_tensor(name, list(shape), dt, kind="ExternalOutput").ap()


def _rstd(kb, ps_ss, out, n, epsb):
    nc = kb.nc
    kb.op("act", lambda: nc.scalar.activation(out[:], ps_ss[:], AF.Sqrt, bias=epsb[:, 0:1], scale=1.0 / n),
          reads=[ps_ss, epsb], writes=[out])
    kb.op("dve", lambda: nc.vector.reciprocal(out[:], out[:]), reads=[out], writes=[out])


def _emit_mod(kb, c_pk, wada, bada, ncol, psr):
    nc = kb.nc
    nj = ncol // 128
    cs = kb.sb([128, 16], F32, "cs")
    cact = kb.sb([128, 16], F32, "cact")
    bsb = kb.sb([128, nj], F32, "bada")
    modT = kb.sb([128, nj], F32, "modT")
    kb.dma("sp", cs[:], c_pk, writes=[cs])
    kb.dma("sp", bsb[:], bada, writes=[bsb])
    kb.op("act", lambda: nc.scalar.activation(cact[:], cs[:], AF.Silu), reads=[cs], writes=[cact])
    war = kb.ring(2, [128, 16, 128], F32, "wada")
    ps = psr.next()
    for j in range(nj):
        wa = war.next()
        kb.dma("sp", wa[:], wada[j].rearrange("p (k c) -> p k c", k=16), writes=[wa])
        kb.mm(ps, ps[:, j:j + 1], [(wa[:, k, :], cact[:, k:k + 1], [wa, cact]) for k in range(16)])
    kb.op("dve", lambda: nc.vector.tensor_tensor(modT[:], ps[:, 0:nj], bsb[:], ALU.add),
          reads=[ps, bsb], writes=[modT])
    return modT


def _load_mod(kb, mod_d, nj):
    t = kb.sb([128, nj], F32, "modT")
    kb.dma("sp", t[:], mod_d, writes=[t])
    return t


def build_M():
    nc = bass.Bass("TRN2", target_bir_lowering=False)
    c_pk = _din(nc, "c_pk", [128, 16])
    wada = _din(nc, "wada", [24, 128, KD * 128])
    bada = _din(nc, "bada_pk", [128, 24])
    o = _dout(nc, "modT", [128, 24])
    kb = KB(nc)
    psr = kb.ring(2, [128, 512], F32, "ps", psum=True)
    modT = _emit_mod(kb, c_pk, wada, bada, 24 * 128, psr)
    kb.dma("sp", o, modT[:], reads=[modT])
    kb.finish()
    return nc


def run_M(inp):
    maps = []
    for i in range(8):
        w = np.concatenate([inp["w_ada"][l][:, i * 1536:(i + 1) * 1536] for l in range(2)], axis=1)
        b = np.concatenate([inp["b_ada"][l][i * 1536:(i + 1) * 1536] for l in range(2)])
        maps.append(dict(c_pk=_pk(inp["c"][0], 16), wada=_blockify(np.ascontiguousarray(w), 24), bada_pk=_pk(b, 24)))
    res = run_bass_kernel_spmd(build_M(), maps, core_ids=list(range(8)))
    out = []
    for l in range(2):
        out.append(np.ascontiguousarray(np.concatenate([r["modT"][:, l * 12:(l + 1) * 12] for r in res.results], axis=1)))
    return out


def _emit_norm_mod(kb, xk, hT, shiftT, onepT, ones, epsb, psr, sqr, tmpr, TG, rstd_t):
    nc = kb.nc
    ps = psr.next()
    for k in range(KD):
        sq = sqr.next()
        kb.op("act", lambda: nc.scalar.activation(sq[:], xk[k][:], AF.Square), reads=[xk[k]], writes=[sq])
        kb.op("pe", lambda: nc.tensor.matmul(ps[:, 0:TG], ones[:], sq[:], start=(k == 0), stop=(k == KD - 1)),
              reads=[ones, sq], writes=[ps], inc=True)
    rstd = rstd_t
    _rstd(kb, ps, rstd, D, epsb)
    for k in range(KD):
        tmp = tmpr.next()
        kb.op("dve", lambda: nc.vector.tensor_tensor(tmp[:], xk[k][:], rstd[:], ALU.mult),
              reads=[xk[k], rstd], writes=[tmp])
        kb.op("act", lambda: nc.scalar.activation(hT[k][:], tmp[:], AF.Identity,
                                                  bias=shiftT[:, k:k + 1], scale=onepT[:, k:k + 1]),
              reads=[tmp, shiftT, onepT], writes=[hT[k]])


PI_LO = 3.1415925
C1_2PI = 6.28125
C2_2PI = 2.0 * math.pi - 6.28125


def _emit_rope_tables(kb, posi, ang, angk, angi, sin2, cos2, rc_s):
    nc = kb.nc
    V = nc.vector
    kb.op("dve", lambda: V.tensor_copy(ang[:], posi[:]), reads=[posi], writes=[ang])
    kb.op("dve", lambda: V.tensor_scalar_mul(ang[:], ang[:], rc_s[:, 0:1]), reads=[ang, rc_s], writes=[ang])
    kb.op("dve", lambda: V.tensor_scalar_mul(angk[:], ang[:], 1.0 / (2.0 * math.pi)), reads=[ang], writes=[angk])
    kb.op("dve", lambda: V.tensor_copy(angi[:], angk[:]), reads=[angk], writes=[angi])
    kb.op("dve", lambda: V.tensor_copy(angk[:], angi[:]), reads=[angi], writes=[angk])
    kb.op("dve", lambda: V.scalar_tensor_tensor(ang[:], angk[:], -C1_2PI, ang[:], ALU.mult, ALU.add),
          reads=[angk, ang], writes=[ang])
    kb.op("dve", lambda: V.scalar_tensor_tensor(ang[:], angk[:], -C2_2PI, ang[:], ALU.mult, ALU.add),
          reads=[angk, ang], writes=[ang])

    def wrap(t):
        kb.op("dve", lambda: V.tensor_scalar(angk[:], t[:], math.pi, -2.0 * math.pi, ALU.is_gt, ALU.mult),
              reads=[t], writes=[angk])
        kb.op("dve", lambda: V.tensor_tensor(t[:], t[:], angk[:], ALU.add), reads=[t, angk], writes=[t])
        kb.op("dve", lambda: V.tensor_scalar(angk[:], t[:], -math.pi, 2.0 * math.pi, ALU.is_lt, ALU.mult),
              reads=[t], writes=[angk])
        kb.op("dve", lambda: V.tensor_tensor(t[:], t[:], angk[:], ALU.add), reads=[t, angk], writes=[t])
        kb.op("dve", lambda: V.tensor_scalar(t[:], t[:], PI_LO, -PI_LO, ALU.min, ALU.max), reads=[t], writes=[t])
    wrap(ang)
    kb.op("dve", lambda: V.tensor_scalar_add(cos2[:], ang[:], 0.5 * math.pi), reads=[ang], writes=[cos2])
    wrap(cos2)
    kb.op("act", lambda: nc.scalar.activation(sin2[:], ang[:], AF.Sin, scale=rc_s[:, 1:2]),
          reads=[ang, rc_s], writes=[sin2])
    kb.op("act", lambda: nc.scalar.activation(cos2[:], cos2[:], AF.Sin), reads=[cos2], writes=[cos2])

def _emit_norm_mod2(kb, xload, hT, shiftT, onepT, ones, epsb, psr, sqr, tmpr, TG, rstd_t, after=None):
    nc = kb.nc
    ps = psr.next()
    for k in range(KD):
        xt = xload(k)
        sq = sqr.next()
        kb.op("act", lambda: nc.scalar.activation(sq[:], xt[:], AF.Square), reads=[xt], writes=[sq])
        kb.op("pe", lambda: nc.tensor.matmul(ps[:, 0:TG], ones[:], sq[:], start=(k == 0), stop=(k == KD - 1)),
              reads=[ones, sq], writes=[ps], inc=True)
    _rstd(kb, ps, rstd_t, D, epsb)
    for k in range(KD):
        xt = xload(k)
        tmp = tmpr.next()
        kb.op("dve", lambda: nc.vector.tensor_tensor(tmp[:], xt[:], rstd_t[:], ALU.mult),
              reads=[xt, rstd_t], writes=[tmp])
        kb.op("act", lambda: nc.scalar.activation(hT[k][:], tmp[:], AF.Identity,
                                                  bias=shiftT[:, k:k + 1], scale=onepT[:, k:k + 1]),
              reads=[tmp, shiftT, onepT], writes=[hT[k]])
        if after is not None:
            after(k, tmp)


NBA = 24


def build_A(NTOK, dbg=False):
    TG = 512
    NG = NTOK // TG
    nc = bass.Bass("TRN2", target_bir_lowering=False)
    xT = _din(nc, "xT", [D, NTOK])
    mod_d = _din(nc, "modT", [128, 32])
    winb = _din(nc, "winb", [NBA, 128, KD * 128])
    dtb = _din(nc, "dtb_bc", [128, 16])
    qn = _din(nc, "qnorm_pk", [128, 6])
    kvn = _din(nc, "kvnorm_pk", [128, 4])
    wuq = _din(nc, "wuq", [768, 2048])
    wukk = _din(nc, "wukv_k", [512, 1024])
    wukv = _din(nc, "wukv_v", [512, 1024])
    qg = _din(nc, "qgain", [128, 3])
    kg = _din(nc, "kgain", [128, 3])
    pos = _din(nc, "pos_bc", [64, NTOK], I32)
    rc = _din(nc, "ropec", [64, 4])
    o_xbc = _dout(nc, "xbcT", [1536, NTOK])
    o_dt = _dout(nc, "dt", [NTOK, 16])
    o_q = _dout(nc, "qT", [8, 192, NTOK], BF16)
    o_k = _dout(nc, "kT", [8, 192, NTOK], BF16)
    o_v = _dout(nc, "v", [NTOK, 1024], BF16)
    if dbg:
        o_dh = _dout(nc, "dbg_h", [D, TG], BF16)
        o_dm = _dout(nc, "dbg_mod", [128, 32])

    kb = KB(nc)
    psr = kb.ring(8, [128, 512], F32, "ps", psum=True)
    ones = kb.sb([128, 128], BF16, "ones")
    epsb = kb.sb([128, 1], F32, "eps")
    kb.op("pool", lambda: nc.gpsimd.memset(ones[:], 1.0), writes=[ones])
    kb.op("pool", lambda: nc.gpsimd.memset(epsb[:], EPS), writes=[epsb])

    modT = _load_mod(kb, mod_d, 32)
    onepT = kb.sb([128, 16], F32, "onep")
    kb.op("dve", lambda: nc.vector.tensor_scalar_add(onepT[:], modT[:, 16:32], 1.0), reads=[modT], writes=[onepT])

    def load_small(ap, shape, dt=F32, name="c"):
        t = kb.sb(shape, dt, name)
        kb.dma("sp", t[:], ap, writes=[t])
        return t
    dtb_s = load_small(dtb, [128, 16])
    qn_s = load_small(qn, [128, 6])
    kvn_s = load_small(kvn, [128, 4])
    qg_s = load_small(qg, [128, 3])
    kg_s = load_small(kg, [128, 3])
    rc_s = load_small(rc, [64, 4])
    wuq_s = kb.sb([128, 6, 2048], BF16, "wuq")
    wukk_s = kb.sb([128, 4, 1024], BF16, "wukk")
    wukv_s = kb.sb([128, 4, 1024], BF16, "wukv")
    wuq_v = wuq.rearrange("(k p) c -> p k c", p=128)
    for j in range(6):
        kb.dma("pool", wuq_s[:, j, :], wuq_v[:, j, :], writes=[wuq_s])
    kb.dma("pool", wukk_s[:], wukk.rearrange("(k p) c -> p k c", p=128), writes=[wukk_s])
    kb.dma("pool", wukv_s[:], wukv.rearrange("(k p) c -> p k c", p=128), writes=[wukv_s])

    xk = [kb.sb([128, TG], F32, "x%d" % k) for k in range(KD)]
    hT = [kb.sb([128, TG], BF16, "h%d" % k) for k in range(KD)]
    sqr = kb.ring(3, [128, TG], BF16, "sq")
    tmpr = kb.ring(6, [128, TG], F32, "tmp")
    rstd_x = kb.sb([128, TG], F32, "rstdx")
    wbr = kb.ring(3, [128, KD, 128], BF16, "wb")
    stg = kb.ring(3, [128, TG], F32, "stg")
    stgb = kb.ring(4, [128, TG], BF16, "stgb")
    cq_s = [kb.sb([128, TG], F32, "cq%d" % j) for j in range(6)]
    cqn = [kb.sb([128, TG], BF16, "cqn%d" % j) for j in range(6)]
    ckv_s = [kb.sb([128, TG], F32, "ckv%d" % j) for j in range(4)]
    ckvn = [kb.sb([128, TG], BF16, "ckvn%d" % j) for j in range(4)]
    kr_s = kb.sb([64, TG], F32, "kr")
    krs_s = kb.sb([64, TG], F32, "krs")
    krsq = kb.sb([64, TG], BF16, "krsq")
    posi = kb.sb([64, TG], I32, "posi")
    ang = kb.sb([64, TG], F32, "ang")
    angk = kb.sb([64, TG], F32, "angk")
    angi = kb.sb([64, TG], I32, "angi")
    cos2 = kb.sb([64, TG], F32, "cos2")
    sin2 = kb.sb([64, TG], F32, "sin2")
    dts = kb.sb([128, 4, 16], F32, "dts")
    xTv = xT.rearrange("(k p) t -> p k t", p=128)

    def load_wblock(b):
        wb = wbr.next()
        kb.dma("pool", wb[:], winb[b].rearrange("p (k c) -> p k c", k=KD), writes=[wb])
        return wb

    for g in range(NG):
        t0 = g * TG
        tsl = slice(t0, t0 + TG)
        for k in range(KD):
            kb.dma("sp", xk[k][:], xTv[:, k, tsl], writes=[xk[k]])
        kb.dma("sp", posi[:], pos[:, tsl], writes=[posi])
        _emit_norm_mod(kb, xk, hT, modT, onepT, ones, epsb, psr, sqr, tmpr, TG, rstd_x)
        _emit_rope_tables(kb, posi, ang, angk, angi, sin2, cos2, rc_s)
        if dbg and g == 0:
            for k in range(KD):
                kb.dma("act", o_dh[k * 128:(k + 1) * 128, :], hT[k][:], reads=[hT[k]])
            kb.dma("act", o_dm, modT[:], reads=[modT])

        def hp(wb, c0, c1):
            return [(wb[:, k, c0:c1], hT[k][:], [wb, hT[k]]) for k in range(KD)]

        for b in range(12):
            wb = load_wblock(b)
            ps = psr.next()
            kb.mm(ps, ps[:, 0:TG], hp(wb, 0, 128))
            st = stg.next()
            if b % 2 == 0:
                kb.op("act", lambda: nc.scalar.copy(st[:], ps[:, 0:TG]), reads=[ps], writes=[st])
            else:
                kb.op("dve", lambda: nc.vector.tensor_copy(st[:], ps[:, 0:TG]), reads=[ps], writes=[st])
            kb.dma("act", o_xbc[b * 128:(b + 1) * 128, tsl], st[:], reads=[st])

        def lat(b0, nb, dst, dstn, gains, nfeat):
            pss = psr.next()
            for j in range(nb):
                wb = load_wblock(b0 + j)
                ps = psr.next()
                kb.mm(ps, ps[:, 0:TG], hp(wb, 0, 128))
                kb.op("act", lambda: nc.scalar.copy(dst[j][:], ps[:, 0:TG]), reads=[ps], writes=[dst[j]])
                sq = sqr.next()
                kb.op("act", lambda: nc.scalar.activation(sq[:], ps[:, 0:TG], AF.Square), reads=[ps], writes=[sq])
                kb.op("pe", lambda: nc.tensor.matmul(pss[:, 0:TG], ones[:], sq[:], start=(j == 0), stop=(j == nb - 1)),
                      reads=[ones, sq], writes=[pss])
            r = tmpr.next()
            _rstd(kb, pss, r, nfeat, epsb)
            for j in range(nb):
                kb.op("dve", lambda: nc.vector.scalar_tensor_tensor(dstn[j][:], dst[j][:], gains[:, j:j + 1], r[:],
                                                                    ALU.mult, ALU.mult),
                      reads=[dst[j], gains, r], writes=[dstn[j]])
        lat(12, 6, cq_s, cqn, qn_s, 768)
        lat(18, 4, ckv_s, ckvn, kvn_s, 512)

        wb = load_wblock(22)
        ps = psr.next()
        kb.mm(ps, ps[0:64, 0:TG], hp(wb, 0, 64))
        kb.op("act", lambda: nc.scalar.copy(kr_s[:], ps[0:64, 0:TG]), reads=[ps], writes=[kr_s])
        kb.op("act", lambda: nc.scalar.activation(krsq[:], ps[0:64, 0:TG], AF.Square), reads=[ps], writes=[krsq])
        ps = psr.next()
        kb.mm(ps, ps[0:64, 0:TG], hp(wb, 64, 128))
        kb.op("act", lambda: nc.scalar.copy(krs_s[:], ps[0:64, 0:TG]), reads=[ps], writes=[krs_s])

        wb = load_wblock(23)
        ps = psr.next()
        for tt in range(TG // 128):
            kb.mm(ps, ps[:, tt * 16:(tt + 1) * 16],
                  [(hT[k][:, tt * 128:(tt + 1) * 128], wb[:, k, 0:16], [wb, hT[k]]) for k in range(KD)])
        for tt in range(TG // 128):
            kb.op("dve", lambda: nc.vector.tensor_tensor(dts[:, tt, :], ps[:, tt * 16:(tt + 1) * 16], dtb_s[:], ALU.add),
                  reads=[ps, dtb_s], writes=[dts])
        kb.op("act", lambda: nc.scalar.activation(dts[:], dts[:], AF.Exp), reads=[dts], writes=[dts])
        kb.op("act", lambda: nc.scalar.activation(dts[:], dts[:], AF.Ln, bias=1.0, scale=1.0), reads=[dts], writes=[dts])
        kb.dma("act", o_dt[tsl, :].rearrange("(t p) h -> p t h", p=128), dts[:], reads=[dts])

        def head(src_n_pairs, rope_src, gains, dst, h):
            psn = psr.next()
            kb.mm(psn, psn[:, 0:TG], src_n_pairs)
            sqn = sqr.next()
            kb.op("act", lambda: nc.scalar.activation(sqn[:], psn[:, 0:TG], AF.Square), reads=[psn], writes=[sqn])
            if rope_src is None:
                psr_ = psr.next()
                kb.mm(psr_, psr_[0:64, 0:TG], [(wuq_s[:, j, h * 256 + 128:h * 256 + 192], cqn[j][:], [wuq_s, cqn[j]]) for j in range(6)])
                pss_ = psr.next()
                kb.mm(pss_, pss_[0:64, 0:TG], [(wuq_s[:, j, h * 256 + 192:h * 256 + 256], cqn[j][:], [wuq_s, cqn[j]]) for j in range(6)])
                sqr_t = sqr.next()
                kb.op("act", lambda: nc.scalar.activation(sqr_t[0:64, :], psr_[0:64, 0:TG], AF.Square), reads=[psr_], writes=[sqr_t])
                r_ap, s_ap, r_t, s_t = psr_[0:64, 0:TG], pss_[0:64, 0:TG], psr_, pss_
            else:
                sqr_t = krsq
                r_ap, s_ap, r_t, s_t = kr_s[:], krs_s[:], kr_s, krs_s
            pss = psr.next()
            kb.op("pe", lambda: nc.tensor.matmul(pss[:, 0:TG], ones[:], sqn[:], start=True, stop=False),
                  reads=[ones, sqn], writes=[pss], inc=False)
            kb.op("pe", lambda: nc.tensor.matmul(pss[:, 0:TG], ones[0:64, :], sqr_t[0:64, :], start=False, stop=True),
                  reads=[ones, sqr_t], writes=[pss])
            r = tmpr.next()
            _rstd(kb, pss, r, 192, epsb)
            on = stgb.next()
            kb.op("dve", lambda: nc.vector.scalar_tensor_tensor(on[:], psn[:, 0:TG], gains[:, 0:1], r[:], ALU.mult, ALU.mult),
                  reads=[psn, gains, r], writes=[on])
            kb.dma("act", dst[h, 0:128, tsl], on[:], reads=[on])
            t1 = tmpr.next()
            t2 = tmpr.next()
            kb.op("dve", lambda: nc.vector.scalar_tensor_tensor(t1[0:64, :], r_ap, gains[0:64, 1:2], r[0:64, :], ALU.mult, ALU.mult),
                  reads=[r_t, gains, r], writes=[t1])
            kb.op("pool", lambda: nc.gpsimd.tensor_tensor(t1[0:64, :], t1[0:64, :], cos2[:], ALU.mult),
                  reads=[t1, cos2], writes=[t1])
            kb.op("dve", lambda: nc.vector.scalar_tensor_tensor(t2[0:64, :], s_ap, gains[0:64, 2:3], r[0:64, :], ALU.mult, ALU.mult),
                  reads=[s_t, gains, r], writes=[t2])
            kb.op("pool", lambda: nc.gpsimd.tensor_tensor(t2[0:64, :], t2[0:64, :], sin2[:], ALU.mult),
                  reads=[t2, sin2], writes=[t2])
            orp = stgb.next()
            kb.op("dve", lambda: nc.vector.tensor_tensor(orp[0:64, :], t1[0:64, :], t2[0:64, :], ALU.add),
                  reads=[t1, t2], writes=[orp])
            kb.dma("act", dst[h, 128:192, tsl], orp[0:64, :], reads=[orp])

        for h in range(8):
            head([(wuq_s[:, j, h * 256:h * 256 + 128], cqn[j][:], [wuq_s, cqn[j]]) for j in range(6)],
                 None, qg_s, o_q, h)
        for h in range(8):
            head([(wukk_s[:, j, h * 128:(h + 1) * 128], ckvn[j][:], [wukk_s, ckvn[j]]) for j in range(4)],
                 True, kg_s, o_k, h)
        for tt in range(TG // 128):
            for hf in range(2):
                ps = psr.next()
                kb.mm(ps, ps[:, 0:512], [(ckvn[j][:, tt * 128:(tt + 1) * 128], wukv_s[:, j, hf * 512:(hf + 1) * 512],
                                          [ckvn[j], wukv_s]) for j in range(4)])
                vb = stgb.next()
                kb.op("act", lambda: nc.scalar.copy(vb[:, 0:512], ps[:, 0:512]), reads=[ps], writes=[vb])
                kb.dma("act", o_v[t0 + tt * 128:t0 + (tt + 1) * 128, hf * 512:(hf + 1) * 512], vb[:, 0:512], reads=[vb])
    kb.finish()
    return nc


def _pk(v, n):
    return np.ascontiguousarray(np.asarray(v, np.float32).reshape(n, 128).T)


def _blockify(w, nb):
    w = w.reshape(KD, 128, nb, 128)
    return np.ascontiguousarray(w.transpose(2, 1, 0, 3).reshape(nb, 128, KD * 128))


def prep_A(inp, layer, S, mod):
    w_in = inp["w_in"][layer]
    cols = np.zeros((D, NBA * 128), np.float32)
    cols[:, 0:1536] = w_in[:, OFF["xbc"]:OFF["xbc"] + 1536]
    cols[:, 1536:2304] = w_in[:, OFF["cq"]:OFF["cq"] + 768]
    cols[:, 2304:2816] = w_in[:, OFF["ckv"]:OFF["ckv"] + 512]
    kr = w_in[:, OFF["kr"]:OFF["kr"] + 64]
    cols[:, 2816:2880] = kr
    cols[:, 2880:2912] = kr[:, 32:64]
    cols[:, 2912:2944] = kr[:, 0:32]
    cols[:, 2944:2960] = w_in[:, OFF["dt"]:OFF["dt"] + 16]
    wuq = inp["mla_w_uq"][layer].reshape(768, 8, 192)
    wuq2 = np.concatenate([wuq, wuq[:, :, 160:192], wuq[:, :, 128:160]], axis=2).reshape(768, 2048)
    wukv = inp["mla_w_ukv"][layer].reshape(512, 8, 256)

    def gain3(g):
        o = np.zeros((128, 3), np.float32)
        o[:, 0] = g[0:128]
        o[0:64, 1] = g[128:192]
        o[0:32, 2] = g[160:192]
        o[32:64, 2] = g[128:160]
        return o
    half = 32
    invf = (np.float32(10000.0) ** (-np.arange(half, dtype=np.float32) * np.float32(2.0) / np.float32(64))).astype(np.float32)
    rc = np.zeros((64, 4), np.float32)
    rc[:, 0] = np.concatenate([invf, invf])
    sgn = np.concatenate([-np.ones(32), np.ones(32)]).astype(np.float32)
    rc[:, 1] = sgn
    rc[:, 2] = -sgn * np.float32(math.pi)
    rc[:, 3] = -np.float32(math.pi)
    return dict(
        modT=np.ascontiguousarray(mod[:, 0:32]),
        winb=_blockify(cols, NBA),
        dtb_bc=np.ascontiguousarray(np.broadcast_to(inp["ssd_dt_bias"][layer][None, :], (128, 16))).astype(np.float32),
        qnorm_pk=_pk(inp["mla_q_norm"][layer], 6),
        kvnorm_pk=_pk(inp["mla_kv_norm"][layer], 4),
        wuq=np.ascontiguousarray(wuq2),
        wukv_k=np.ascontiguousarray(wukv[:, :, 0:128].reshape(512, 1024)),
        wukv_v=np.ascontiguousarray(wukv[:, :, 128:256].reshape(512, 1024)),
        qgain=gain3(inp["mla_q_gain"][layer]),
        kgain=gain3(inp["mla_k_gain"][layer]),
        ropec=rc,
    )


def run_A(inp, layer, xT, S, NTOK, mod, dbg=False):
    ncore = S // NTOK
    shared = prep_A(inp, layer, S, mod)
    pos = np.asarray(inp["positions"][0, :S]).astype(np.int32)
    maps = []
    for i in range(ncore):
        m = dict(shared)
        m["xT"] = np.ascontiguousarray(xT[:, i * NTOK:(i + 1) * NTOK])
        m["pos_bc"] = np.ascontiguousarray(np.broadcast_to(pos[None, i * NTOK:(i + 1) * NTOK], (64, NTOK)))
        maps.append(m)
    nc = build_A(NTOK, dbg)
    res = run_bass_kernel_spmd(nc, maps, core_ids=list(range(ncore)))
    R = res.results
    if dbg:
        return R
    out = dict(
        xbcT=np.concatenate([r["xbcT"] for r in R], axis=1),
        dt=np.concatenate([r["dt"] for r in R], axis=0),
        qT=np.concatenate([r["qT"] for r in R], axis=2),
        kT=np.concatenate([r["kT"] for r in R], axis=2),
        v=np.concatenate([r["v"] for r in R], axis=0),
    )
    return out


def build_B(S, do_att=True, do_ssd=True, stage=9):
    QG = 512
    NQG = S // QG
    NKB = S // 128
    NCH = S // 128
    nc = bass.Bass("TRN2", target_bir_lowering=False)
    qn_d = _din(nc, "qn", [128, S], BF16)
    qr_d = _din(nc, "qr", [64, S], BF16)
    kn_d = _din(nc, "kn", [128, S], BF16)
    kr_d = _din(nc, "kr", [64, S], BF16)
    v_d = _din(nc, "vb", [128, NKB, 128], BF16)
    mask_d = _din(nc, "masks", [128, 4, QG], BF16)
    slab_d = _din(nc, "slab", [384, S])
    convw_d = _din(nc, "convw", [128, 3, 4])
    convb_d = _din(nc, "convb", [128, 3])
    dt_d = _din(nc, "dth", [128, NCH, 2])
    alog_d = _din(nc, "alog_bc", [128, 2])
    dsk_d = _din(nc, "dskip_bc", [128, 2])
    U_d = _din(nc, "U", [128, 128])
    negm_d = _din(nc, "negmask", [128, 128])
    id_d = _din(nc, "ident", [128, 128])
    o_att = _dout(nc, "yatt", [128, S], BF16)
    o_ssd = _dout(nc, "yssd", [128, S], F32)

    kb = KB(nc)
    V = nc.vector
    A = nc.scalar
    ones = kb.sb([128, 128], BF16, "ones")
    onesf = kb.sb([128, 128], F32, "onesf")
    kb.op("pool", lambda: nc.gpsimd.memset(ones[:], 1.0), writes=[ones])
    kb.op("pool", lambda: nc.gpsimd.memset(onesf[:], 1.0), writes=[onesf])

    def load(ap, shape, dt, name, q="sp"):
        t = kb.sb(shape, dt, name)
        kb.dma(q, t[:], ap, writes=[t])
        return t

    if do_att:
        kn = load(kn_d, [128, S], BF16, "kn")
        kr = load(kr_d, [64, S], BF16, "kr")
        vb = load(v_d, [128, NKB, 128], BF16, "vb")
        masks = load(mask_d, [128, 4, QG], BF16, "masks")
        qnr = kb.ring(2, [128, QG], BF16, "qn")
        qrr = kb.ring(2, [64, QG], BF16, "qr")
        ptr = kb.ring(4, [128, QG], BF16, "pt")
        ps_s = kb.ring(2, [128, QG], F32, "pss", psum=True)
        ps_o = kb.ring(1, [128, QG], F32, "pso", psum=True)
        ps_d = kb.ring(1, [128, QG], F32, "psd", psum=True)
        rec = kb.sb([128, QG], F32, "rec")
        yst = kb.ring(2, [128, QG], BF16, "yst")
        scale = 192.0 ** -0.5
    if do_ssd:
        bX = kb.ps([128, 512], F32, "bankX")
        bY = kb.ps([128, 512], F32, "bankY")
        bZ = [kb.ps([128, 512], F32, "bankZ%d" % h) for h in range(2)]
        p_xt = View(bX, bX[:, 0:128], "p_xt")
        p_bt = View(bX, bX[:, 128:256], "p_bt")
        p_g = View(bX, bX[:, 256:384], "p_g")
        p_acs = View(bX, bX[:, 384:386], "p_acs")
        p_abc = [View(bY, bY[:, h * 128:(h + 1) * 128], "p_abc%d" % h) for h in range(2)]
        p_y = [View(bZ[h], bZ[h][:, 0:128], "p_y%d" % h) for h in range(2)]
        p_s = [View(bZ[h], bZ[h][:, 128:192], "p_s%d" % h) for h in range(2)]
        convw = load(convw_d, [128, 3, 4], F32, "convw")
        convb = load(convb_d, [128, 3], F32, "convb")
        dth = load(dt_d, [128, NCH, 2], F32, "dth")
        alog = load(alog_d, [128, 2], F32, "alog")
        dsk = load(dsk_d, [128, 2], F32, "dsk")
        U = load(U_d, [128, 128], F32, "U")
        negm = load(negm_d, [128, 128], F32, "negm")
        identf = load(id_d, [128, 128], F32, "identf")
        ident = kb.sb([128, 128], BF16, "ident")
        kb.op("dve", lambda: V.tensor_copy(ident[:], identf[:]), reads=[identf], writes=[ident])
        aneg = kb.sb([128, 2], F32, "aneg")
        kb.op("act", lambda: A.activation(aneg[:], alog[:], AF.Exp), reads=[alog], writes=[aneg])
        kb.op("dve", lambda: V.tensor_scalar_mul(aneg[:], aneg[:], -1.0), reads=[aneg], writes=[aneg])
        dI = [kb.sb([128, 128], BF16, "dI%d" % h) for h in range(2)]
        for h in range(2):
            kb.op("dve", lambda: V.tensor_scalar_mul(dI[h][:], identf[:], dsk[:, h:h + 1]), reads=[identf, dsk], writes=[dI[h]])
        CW = 512
        slabr = [kb.ring(2, [128, CW + 3], F32, "slab%d" % j) for j in range(3)]
        accr = kb.ring(2, [128, CW], F32, "cacc")
        xcT = [kb.ring(2, [128, CW], BF16, "xcT%d" % j) for j in range(3)]
        HT = [kb.sb([128, 64], F32, "HT%d" % h) for h in range(2)]
        Hbf = [kb.sb([128, 64], BF16, "Hbf%d" % h) for h in range(2)]
        for h in range(2):
            kb.op("pool", lambda: nc.gpsimd.memset(HT[h][:], 0.0), writes=[HT[h]])
            kb.op("pool", lambda: nc.gpsimd.memset(Hbf[h][:], 0.0), writes=[Hbf[h]])
        xtokr = kb.ring(2, [128, 128], BF16, "xtok")
        btokr = kb.ring(2, [128, 128], BF16, "btok")
        da = kb.ring(2, [128, 2], F32, "da")
        acs = kb.ring(2, [128, 2], F32, "acs")
        darep = kb.ring(2, [128, 128], F32, "darep")
        argr = kb.ring(2, [128, 128], F32, "arg")
        LTr = kb.ring(2, [128, 128], F32, "LT")
        MTr = kb.ring(2, [128, 128], BF16, "MT")
        Er = kb.ring(2, [128, 128], F32, "E")
        CPr = kb.ring(2, [128, 128], BF16, "CP")
        xdtr = kb.ring(2, [128, 64], BF16, "xdt")
        xdtdr = kb.ring(2, [128, 64], BF16, "xdtd")
        cdecr = kb.ring(2, [128, 1], F32, "cdec")
        stmpr = kb.ring(2, [128, 64], F32, "stmp")
        ystg = [kb.ring(2, [64, CW], F32, "ystg%d" % h) for h in range(2)]

    def att_group(g, gen):
        q0 = g * QG
        qn = qnr.next()
        qr = qrr.next()
        kb.dma("sp", qn[:], qn_d[:, q0:q0 + QG], writes=[qn])
        kb.dma("sp", qr[:], qr_d[:, q0:q0 + QG], writes=[qr])
        po = ps_o.next()
        pd = ps_d.next()
        nkb = (g + 1) * 4

        def qk(i):
            ksl = slice(i * 128, (i + 1) * 128)
            ps = ps_s.next()
            kb.op("pe", lambda: nc.tensor.matmul(ps[:], kn[:, ksl], qn[:], start=True, stop=False),
                  reads=[kn, qn], writes=[ps], inc=False)
            kb.op("pe", lambda: nc.tensor.matmul(ps[:], kr[:, ksl], qr[:], start=False, stop=True),
                  reads=[kr, qr], writes=[ps])
            return ps
        cur = qk(0)
        for i in range(nkb):
            nxt = qk(i + 1) if i + 1 < nkb else None
            ps = cur
            pt = ptr.next()
            kb.op("act", lambda: A.activation(pt[:], ps[:], AF.Exp, scale=scale), reads=[ps], writes=[pt])
            d = i - g * 4
            if d >= 0:
                kb.op("dve", lambda: V.tensor_tensor(pt[:], pt[:], masks[:, d, :], ALU.mult), reads=[pt, masks], writes=[pt])
            kb.op("pe", lambda: nc.tensor.matmul(po[:], vb[:, i, :], pt[:], start=(i == 0), stop=(i == nkb - 1)),
                  reads=[vb, pt], writes=[po], inc=False)
            kb.op("pe", lambda: nc.tensor.matmul(pd[:], ones[:], pt[:], start=(i == 0), stop=(i == nkb - 1)),
                  reads=[ones, pt], writes=[pd])
            if gen is not None:
                next(gen, None)
            cur = nxt
        kb.op("dve", lambda: V.reciprocal(rec[:], pd[:]), reads=[pd], writes=[rec])
        ys = yst.next()
        kb.op("dve", lambda: V.tensor_tensor(ys[:], po[:], rec[:], ALU.mult), reads=[po, rec], writes=[ys])
        kb.dma("act", o_att[:, q0:q0 + QG], ys[:], reads=[ys])

    def ssd_piece(pc):
        t0 = pc * CW
        cur = []
        for j in range(3):
            sl = slabr[j].next()
            if pc == 0:
                kb.op("pool", lambda: nc.gpsimd.memset(sl[:, 0:3], 0.0), writes=[sl])
                kb.dma("sp", sl[:, 3:CW + 3], slab_d[j * 128:(j + 1) * 128, 0:CW], writes=[sl])
            else:
                kb.dma("sp", sl[:], slab_d[j * 128:(j + 1) * 128, t0 - 3:t0 + CW], writes=[sl])
            acc = accr.next()
            kb.op("dve", lambda: V.tensor_scalar_mul(acc[:], sl[:, 0:CW], convw[:, j, 0:1]), reads=[sl, convw], writes=[acc])
            for k in range(1, 4):
                kb.op("dve", lambda: V.scalar_tensor_tensor(acc[:], sl[:, k:k + CW], convw[:, j, k:k + 1], acc[:], ALU.mult, ALU.add),
                      reads=[sl, convw, acc], writes=[acc])
            o = xcT[j].next()
            kb.op("act", lambda: A.activation(o[:], acc[:], AF.Silu, bias=convb[:, j:j + 1], scale=1.0),
                  reads=[acc, convb], writes=[o])
            cur.append(o)
            yield
        xT_, BT_, CT_ = cur
        if stage < 2:
            return
        yst2 = [ystg[h].next() for h in range(2)]
        for cc in range(CW // 128):
            c = pc * (CW // 128) + cc
            csl = slice(cc * 128, (cc + 1) * 128)
            kb.op("pe", lambda: nc.tensor.matmul(p_xt[:], xT_[:, csl], ident[:], start=True, stop=True),
                  reads=[xT_, ident], writes=[p_xt])
            kb.op("pe", lambda: nc.tensor.matmul(p_bt[:], BT_[:, csl], ident[:], start=True, stop=True),
                  reads=[BT_, ident], writes=[p_bt])
            kb.op("pe", lambda: nc.tensor.matmul(p_g[:], BT_[:, csl], CT_[:, csl], start=True, stop=True),
                  reads=[BT_, CT_], writes=[p_g])
            yield
            xtok = xtokr.next()
            btok = btokr.next()
            kb.op("act", lambda: A.copy(xtok[:], p_xt[:]), reads=[p_xt], writes=[xtok])
            kb.op("act", lambda: A.copy(btok[:], p_bt[:]), reads=[p_bt], writes=[btok])
            if stage < 3:
                continue
            da_t = da.next()
            kb.op("dve", lambda: V.tensor_tensor(da_t[:], dth[:, c, :], aneg[:], ALU.mult), reads=[dth, aneg], writes=[da_t])
            kb.op("pe", lambda: nc.tensor.matmul(p_acs[:], U[:], da_t[:], start=True, stop=True),
                  reads=[U, da_t], writes=[p_acs])
            dr = []
            for h in range(2):
                drt = darep.next()
                kb.op("act", lambda: A.activation(drt[:], onesf[:], AF.Identity, scale=da_t[:, h:h + 1]),
                      reads=[onesf, da_t], writes=[drt])
                dr.append(drt)
            for h in range(2):
                kb.op("pe", lambda: nc.tensor.matmul(p_abc[h][:], dr[h][:], U[:], start=True, stop=True),
                      reads=[dr[h], U], writes=[p_abc[h]])
            yield
            if stage < 4:
                continue
            acs_t = acs.next()
            kb.op("dve", lambda: V.tensor_copy(acs_t[:], p_acs[:]), reads=[p_acs], writes=[acs_t])
            for h in range(2):
                Abc = p_abc[h][:]
                psa = p_abc[h]
                arg = argr.next()
                kb.op("dve", lambda: V.scalar_tensor_tensor(arg[:], Abc, acs_t[:, h:h + 1], negm[:], ALU.subtract, ALU.add),
                      reads=[psa, acs_t, negm], writes=[arg])
                yield
                LT = LTr.next()
                kb.op("act", lambda: A.activation(LT[:], arg[:], AF.Exp), reads=[arg], writes=[LT])
                MT = MTr.next()
                kb.op("dve", lambda: V.tensor_tensor(MT[:], p_g[:], LT[:], ALU.mult), reads=[p_g, LT], writes=[MT])
                if stage < 5:
                    continue
                E = Er.next()
                kb.op("act", lambda: A.activation(E[:], Abc, AF.Exp), reads=[psa], writes=[E])
                CP = CPr.next()
                kb.op("pool", lambda: nc.gpsimd.tensor_tensor(CP[:], CT_[:, csl], E[:], ALU.mult), reads=[CT_, E], writes=[CP])
                yield
                xdt = xdtr.next()
                kb.op("dve", lambda: V.tensor_scalar_mul(xdt[:], xtok[:, h * 64:(h + 1) * 64], dth[:, c, h:h + 1]),
                      reads=[xtok, dth], writes=[xdt])
                xdtd = xdtdr.next()
                kb.op("dve", lambda: V.tensor_scalar_mul(xdtd[:], xdt[:], LT[:, 127:128]), reads=[xdt, LT], writes=[xdtd])
                cdec = cdecr.next()
                kb.op("act", lambda: A.copy(cdec[:], E[:, 127:128]), reads=[E], writes=[cdec])
                yield
                if stage < 6:
                    continue
                psy = p_y[h]
                pss_ = p_s[h]
                kb.op("pe", lambda: nc.tensor.matmul(psy[0:64, 0:128], xdt[:], MT[:], start=True, stop=False),
                      reads=[xdt, MT], writes=[psy], inc=False)
                kb.op("pe", lambda: nc.tensor.matmul(psy[0:64, 0:128], Hbf[h][:], CP[:], start=False, stop=False),
                      reads=[Hbf[h], CP], writes=[psy], inc=False)
                kb.op("pe", lambda: nc.tensor.matmul(psy[0:64, 0:128], xtok[:, h * 64:(h + 1) * 64], dI[h][:], start=False, stop=True),
                      reads=[xtok, dI[h]], writes=[psy], inc=False)
                kb.op("pe", lambda: nc.tensor.matmul(pss_[:], btok[:], xdtd[:], start=True, stop=True),
                      reads=[btok, xdtd], writes=[psy, pss_])
                yield
                if stage < 7:
                    continue
                kb.op("act", lambda: A.copy(yst2[h][:, csl], psy[0:64, 0:128]), reads=[psy], writes=[yst2[h]])
                if stage < 8:
                    continue
                stmp = stmpr.next()
                kb.op("act", lambda: A.copy(stmp[:], pss_[:]), reads=[pss_], writes=[stmp])
                kb.op("dve", lambda: V.scalar_tensor_tensor(HT[h][:], HT[h][:], cdec[:, 0:1], stmp[:], ALU.mult, ALU.add),
                      reads=[HT[h], cdec, stmp], writes=[HT[h]])
                kb.op("pool", lambda: nc.gpsimd.tensor_copy(Hbf[h][:], HT[h][:]), reads=[HT[h]], writes=[Hbf[h]])
        if stage < 9:
            return
        for h in range(2):
            kb.dma("act", o_ssd[h * 64:(h + 1) * 64, t0:t0 + CW], yst2[h][:], reads=[yst2[h]])

    def ssd_all():
        for pc in range(S // CW):
            yield from ssd_piece(pc)

    gen = ssd_all() if do_ssd else None
    if do_att:
        for g in range(NQG):
            att_group(g, gen)
    if gen is not None:
        for _ in gen:
            pass
    kb.finish()
    return nc


def consts_B():
    k = np.arange(128)[:, None]
    q = np.arange(512)[None, :]
    masks = np.stack([(k + d * 128 <= q) for d in range(4)], axis=1).astype(np.float32).astype(ml_dtypes.bfloat16)
    s = np.arange(128)[:, None]
    l = np.arange(128)[None, :]
    U = (s <= l).astype(np.float32)
    negm = np.where(s <= l, 0.0, -30000.0).astype(np.float32)
    return dict(masks=np.ascontiguousarray(masks), U=U, negmask=negm, ident=np.eye(128, dtype=np.float32))


def run_B(inp, layer, A_out, S, do_att=True, do_ssd=True, stage=9):
    cst = consts_B()
    qT, kT, v = A_out["qT"], A_out["kT"], A_out["v"]
    xbcT, dt = A_out["xbcT"], A_out["dt"]
    cw = inp["ssd_conv_w"][layer]
    cb = inp["ssd_conv_b"][layer]
    NCH = S // 128
    maps = []
    for i in range(8):
        g = i // 4
        rows = np.concatenate([np.arange(i * 128, (i + 1) * 128), 1024 + g * 128 + np.arange(128),
                               1280 + g * 128 + np.arange(128)])
        m = dict(cst)
        m["qn"] = np.ascontiguousarray(qT[i, 0:128])
        m["qr"] = np.ascontiguousarray(qT[i, 128:192])
        m["kn"] = np.ascontiguousarray(kT[i, 0:128])
        m["kr"] = np.ascontiguousarray(kT[i, 128:192])
        m["vb"] = np.ascontiguousarray(v[:, i * 128:(i + 1) * 128].reshape(S // 128, 128, 128).transpose(1, 0, 2))
        m["slab"] = np.ascontiguousarray(xbcT[rows])
        m["convw"] = np.ascontiguousarray(cw[:, rows].T.reshape(3, 128, 4).transpose(1, 0, 2))
        m["convb"] = np.ascontiguousarray(cb[rows].reshape(3, 128).T)
        m["dth"] = np.ascontiguousarray(dt[:, 2 * i:2 * i + 2].reshape(NCH, 128, 2).transpose(1, 0, 2))
        m["alog_bc"] = np.ascontiguousarray(np.broadcast_to(inp["ssd_a_log"][layer][None, 2 * i:2 * i + 2], (128, 2))).astype(np.float32)
        m["dskip_bc"] = np.ascontiguousarray(np.broadcast_to(inp["ssd_d"][layer][None, 2 * i:2 * i + 2], (128, 2))).astype(np.float32)
        maps.append(m)
    nc = build_B(S, do_att, do_ssd, stage)
    res = run_bass_kernel_spmd(nc, maps, core_ids=list(range(8)))
    R = res.results
    return dict(yattT=np.concatenate([r["yatt"] for r in R], axis=0),
                yssdT=np.concatenate([r["yssd"] for r in R], axis=0))


NBC = 96


def build_C1(NTOK, dbg=False):
    TG = 512
    NG = NTOK // TG
    nc = bass.Bass("TRN2", target_bir_lowering=False)
    xTh = _din(nc, "xTh", [D, NTOK + 16])
    yatt_d = _din(nc, "yattT", [1024, NTOK], BF16)
    yssd_d = _din(nc, "yssdT", [1024, NTOK])
    mod_d = _din(nc, "modT", [128, 48])
    winb = _din(nc, "winb", [NBC, 128, KD * 128])
    wpool_d = _din(nc, "wpoolb", [4, 128, 2 * 256])
    pscale_d = _din(nc, "pscale_pk", [128, 8])
    hflag_d = _din(nc, "haloflag", [128, 1])
    pcorr_d = _din(nc, "pcorr", [128, 4, 16])
    snorm_d = _din(nc, "ssdnorm_pk", [128, 8])
    lng_d = _din(nc, "lng_bc", [128, 1024])
    lnb_d = _din(nc, "lnb_bc", [128, 1024])
    wsT_d = _din(nc, "wsT", [128, 8, 128])
    um_d = _din(nc, "Umask", [128, 128])
    bs_d = _din(nc, "bs_row", [1, 1024])
    wbr_d = _din(nc, "wbrb", [4, 16, 128, 8 * 128])
    wout_d = _din(nc, "woutb", [16, 128, KD * 128])
    o_x = _dout(nc, "xmidT", [D, NTOK])
    if dbg:
        o_dy = _dout(nc, "dbg_y", [4, 1024, 512], BF16)
        o_dm = _dout(nc, "dbg_m", [2048, 512], BF16)

    kb = KB(nc)
    V = nc.vector
    A = nc.scalar
    G = nc.gpsimd
    psr = kb.ring(7, [128, 512], F32, "ps", psum=True)
    ps_stat = kb.ps([128, 512], F32, "ps_stat")
    ones = kb.sb([128, 128], BF16, "ones")
    onesf = kb.sb([1, 128], F32, "onesf")
    epsb = kb.sb([128, 1], F32, "eps")
    kb.op("pool", lambda: G.memset(ones[:], 1.0), writes=[ones])
    kb.op("pool", lambda: G.memset(onesf[:], 1.0), writes=[onesf])
    kb.op("pool", lambda: G.memset(epsb[:], EPS), writes=[epsb])
    modT = _load_mod(kb, mod_d, 48)
    onepT = kb.sb([128, 16], F32, "onep")
    kb.op("dve", lambda: V.tensor_scalar_add(onepT[:], modT[:, 16:32], 1.0), reads=[modT], writes=[onepT])

    def load(ap, shape, dt=F32, name="c", q="sp"):
        t = kb.sb(shape, dt, name)
        kb.dma(q, t[:], ap, writes=[t])
        return t
    pscale = load(pscale_d, [128, 8])
    hflag = load(hflag_d, [128, 1])
    pcorr = load(pcorr_d, [128, 4, 16])
    snorm = load(snorm_d, [128, 8])
    lng = load(lng_d, [128, 1024])
    lnb = load(lnb_d, [128, 1024])
    wsTf = load(wsT_d, [128, 8, 128])
    um = load(um_d, [128, 128])
    bs = load(bs_d, [1, 1024])
    wsm = kb.sb([128, 8, 128], BF16, "wsm")
    for gi in range(8):
        kb.op("dve", lambda: V.tensor_tensor(wsm[:, gi, :], wsTf[:, gi, :], um[:], ALU.mult), reads=[wsTf, um], writes=[wsm])
    wpool_s = kb.sb([128, 4, 512], BF16, "wpool")
    for wg in range(4):
        kb.dma("pool", wpool_s[:, wg, :], wpool_d[wg], writes=[wpool_s])

    xkr = kb.ring(4, [128, TG], F32, "xk")
    hT = [kb.sb([128, TG], BF16, "h%d" % k) for k in range(KD)]
    xh = kb.sb([128, KD, 16], F32, "xh")
    hTh = kb.sb([128, KD, 16], BF16, "hTh")
    sqh = kb.sb([128, KD, 16], BF16, "sqh")
    rstd_h = kb.sb([128, 16], F32, "rstdh")
    tmph = kb.sb([128, 16], F32, "tmph")
    rstd_x = kb.sb([128, TG], F32, "rstdx")
    sqr = kb.ring(3, [128, TG], BF16, "sq")
    tmpr = kb.ring(4, [128, TG], F32, "tmp")
    wbr = kb.ring(2, [128, KD, 128], BF16, "wb")
    wbbr = kb.ring(3, [128, 8, 128], BF16, "wbb")
    Ar = kb.ring(2, [128, TG + 16], F32, "A")
    Sr = kb.ring(2, [128, TG + 16], F32, "S")
    plr = kb.ring(4, [128, TG], BF16, "pl")
    ypool = [kb.sb([128, TG], BF16, "ypool%d" % j) for j in range(8)]
    yssd = [kb.sb([128, TG], BF16, "yssd%d" % j) for j in range(8)]
    yssdf = kb.ring(2, [128, TG], F32, "yssdf")
    yatt = [kb.sb([128, TG], BF16, "yatt%d" % j) for j in range(8)]
    ug = [kb.sb([128, TG], BF16, "ug%d" % j) for j in range(8)]
    ysgu = ug
    vt = [kb.sb([128, 1024], F32, "vt%d" % t) for t in range(TG // 128)]
    vl = [kb.sb([128, 1024], BF16, "vl%d" % t) for t in range(TG // 128)]
    vsum = kb.sb([128, 4, 8], F32, "vsum")
    vst = kb.sb([128, 4, 4], F32, "vst")
    junk = kb.sb([128, 1024], BF16, "junk")
    merged = [kb.sb([128, TG], BF16, "mrg%d" % j) for j in range(KD)]
    accr = kb.ring(2, [128, TG], F32, "acc")
    xTv = xTh.rearrange("(k p) t -> p k t", p=128)

    def load_wblock(b):
        wb = wbr.next()
        kb.dma("pool", wb[:], winb[b].rearrange("p (k c) -> p k c", k=KD), writes=[wb])
        return wb

    def hp(wb, c0, c1):
        return [(wb[:, k, c0:c1], hT[k][:], [wb, hT[k]]) for k in range(KD)]

    for g in range(NG):
        t0 = g * TG
        tsl = slice(t0, t0 + TG)
        kb.dma("sp", xh[:], xTv[:, :, t0:t0 + 16], writes=[xh])
        def xload(k):
            xt = xkr.next()
            kb.dma("sp", xt[:], xTv[:, k, t0 + 16:t0 + 16 + TG], writes=[xt])
            return xt
        for j in range(8):
            kb.dma("sp", yatt[j][:], yatt_d[j * 128:(j + 1) * 128, tsl], writes=[yatt[j]])
        _emit_norm_mod2(kb, xload, hT, modT, onepT, ones, epsb, psr, sqr, tmpr, TG, rstd_x)
        kb.op("act", lambda: A.activation(sqh[:], xh[:], AF.Square), reads=[xh], writes=[sqh])
        ps = psr.next()
        kb.mm(ps, ps[:, 0:16], [(ones[:], sqh[:, k, :], [ones, sqh]) for k in range(KD)])
        kb.op("act", lambda: A.activation(rstd_h[:], ps[:, 0:16], AF.Sqrt, bias=epsb[:, 0:1], scale=1.0 / D),
              reads=[ps, epsb], writes=[rstd_h])
        kb.op("dve", lambda: V.reciprocal(rstd_h[:], rstd_h[:]), reads=[rstd_h], writes=[rstd_h])
        for k in range(KD):
            kb.op("dve", lambda: V.scalar_tensor_tensor(tmph[:], xh[:, k, :], onepT[:, k:k + 1], rstd_h[:], ALU.mult, ALU.mult),
                  reads=[xh, onepT, rstd_h], writes=[tmph])
            kb.op("dve", lambda: V.tensor_scalar_add(hTh[:, k, :], tmph[:], modT[:, k:k + 1]), reads=[tmph, modT], writes=[hTh])

        plc = {}
        for blk in range(8):
            plc[blk] = plr.next()
            wb = load_wblock(blk)
            ps = psr.next()
            kb.mm(ps, ps[:, 0:TG], hp(wb, 0, 128))
            ps2 = psr.next()
            kb.mm(ps2, ps2[:, 0:16], [(wb[:, k, :], hTh[:, k, :], [wb, hTh]) for k in range(KD)])
            At = Ar.next()
            kb.op("act", lambda: A.copy(At[:, 16:16 + TG], ps[:, 0:TG]), reads=[ps], writes=[At])
            if g == 0:
                kb.op("dve", lambda: V.tensor_scalar_mul(At[:, 0:16], ps2[:, 0:16], hflag[:, 0:1]), reads=[ps2, hflag], writes=[At])
            else:
                kb.op("dve", lambda: V.tensor_copy(At[:, 0:16], ps2[:, 0:16]), reads=[ps2], writes=[At])
            wi = blk // 2
            W = TG + 16
            cur = At
            sh = 1
            for step in range(wi + 1):
                St = Sr.next()
                eng = "dve" if step % 2 == 0 else "pool"
                E_ = V if eng == "dve" else G
                kb.op(eng, lambda: E_.tensor_tensor(St[:, sh:W], cur[:, sh:W], cur[:, 0:W - sh], ALU.add),
                      reads=[cur], writes=[St])
                cur = St
                sh *= 2
            win = float(2 ** (wi + 1))
            kb.op("dve", lambda: V.scalar_tensor_tensor(plc[blk][:], cur[:, 16:W], 1.0 / win, At[:, 16:W], ALU.mult, ALU.subtract),
                  reads=[cur, At], writes=[plc[blk]])
            if g == 0:
                kb.op("dve", lambda: V.tensor_tensor(cur[:, 16:32], cur[:, 16:32], pcorr[:, wi, :], ALU.mult),
                      reads=[cur, pcorr], writes=[cur])
                kb.op("dve", lambda: V.scalar_tensor_tensor(plc[blk][:, 0:16], cur[:, 16:32], 1.0 / win, At[:, 16:32], ALU.mult, ALU.subtract),
                      reads=[cur, At], writes=[plc[blk]])
            if blk % 2 == 0:
                continue
            wg = blk // 2
            for dh in range(2):
                ps = psr.next()
                kb.mm(ps, ps[:, 0:TG], [(wpool_s[:, wg, kc * 256 + dh * 128:kc * 256 + (dh + 1) * 128], plc[wg * 2 + kc][:],
                                         [wpool_s, plc[wg * 2 + kc]]) for kc in range(2)])
                j = wg * 2 + dh
                kb.op("act", lambda: A.activation(ypool[j][:], ps[:, 0:TG], AF.Identity, scale=pscale[:, j:j + 1]),
                      reads=[ps, pscale], writes=[ypool[j]])

        pss = ps_stat
        for blk in range(8):
            yf = yssdf.next()
            kb.dma("sp", yf[:], yssd_d[blk * 128:(blk + 1) * 128, tsl], writes=[yf])
            wb = load_wblock(8 + blk)
            ps = psr.next()
            kb.mm(ps, ps[:, 0:TG], hp(wb, 0, 128))
            sz = tmpr.next()
            kb.op("act", lambda: A.activation(sz[:], ps[:, 0:TG], AF.Silu), reads=[ps], writes=[sz])
            kb.op("dve", lambda: V.tensor_tensor(sz[:], sz[:], yf[:], ALU.mult), reads=[sz, yf], writes=[sz])
            kb.op("pool", lambda: G.tensor_copy(yssd[blk][:], sz[:]), reads=[sz], writes=[yssd[blk]])
            sq = sqr.next()
            kb.op("act", lambda: A.activation(sq[:], sz[:], AF.Square), reads=[sz], writes=[sq])
            kb.op("pe", lambda: nc.tensor.matmul(pss[:, 0:TG], ones[:], sq[:], start=(blk == 0), stop=(blk == 7)),
                  reads=[ones, sq], writes=[pss])
        r = tmpr.next()
        _rstd(kb, pss, r, 1024, epsb)
        for blk in range(8):
            kb.op("dve", lambda: V.scalar_tensor_tensor(yssd[blk][:], yssd[blk][:], snorm[:, blk:blk + 1], r[:], ALU.mult, ALU.mult),
                  reads=[yssd[blk], snorm, r], writes=[yssd[blk]])

        for blk in range(8):
            wb = load_wblock(16 + blk)
            ps = psr.next()
            kb.mm(ps, ps[:, 0:TG], hp(wb, 0, 128))
            kb.op("act", lambda: A.activation(ug[blk][:], ps[:, 0:TG], AF.Gelu), reads=[ps], writes=[ug[blk]])
        for blk in range(8):
            wb = load_wblock(24 + blk)
            ps = psr.next()
            for tt in range(4):
                kb.mm(ps, ps[:, tt * 128:(tt + 1) * 128],
                      [(hT[k][:, tt * 128:(tt + 1) * 128], wb[:, k, :], [wb, hT[k]]) for k in range(KD)])
            for tt in range(4):
                kb.op("act", lambda: A.activation(vt[tt][:, blk * 128:(blk + 1) * 128], ps[:, tt * 128:(tt + 1) * 128], AF.Gelu,
                                                  accum_out=vsum[:, tt, blk:blk + 1]),
                      reads=[ps], writes=[vt[tt], vsum])
        for tt in range(4):
            kb.op("dve", lambda: V.reduce_sum(vst[:, tt, 0:1], vsum[:, tt, :], axis=AX.X), reads=[vsum], writes=[vst])
            kb.op("dve", lambda: V.tensor_scalar_mul(vst[:, tt, 0:1], vst[:, tt, 0:1], 1.0 / 1024), reads=[vst], writes=[vst])
            kb.op("dve", lambda: V.tensor_scalar_sub(vt[tt][:], vt[tt][:], vst[:, tt, 0:1]), reads=[vt[tt], vst], writes=[vt[tt]])
            kb.op("act", lambda: A.activation(junk[:], vt[tt][:], AF.Square, accum_out=vst[:, tt, 1:2]),
                  reads=[vt[tt]], writes=[junk, vst])
            kb.op("act", lambda: A.activation(vst[:, tt, 2:3], vst[:, tt, 1:2], AF.Sqrt, bias=epsb[:, 0:1], scale=1.0 / 1024),
                  reads=[vst, epsb], writes=[vst])
            kb.op("dve", lambda: V.reciprocal(vst[:, tt, 3:4], vst[:, tt, 2:3]), reads=[vst], writes=[vst])
            kb.op("dve", lambda: V.scalar_tensor_tensor(vt[tt][:], vt[tt][:], vst[:, tt, 3:4], lng[:], ALU.mult, ALU.mult),
                  reads=[vt[tt], vst, lng], writes=[vt[tt]])
            kb.op("pool", lambda: G.tensor_tensor(vl[tt][:], vt[tt][:], lnb[:], ALU.add), reads=[vt[tt], lnb], writes=[vl[tt]])
        for gi in range(8):
            ps = psr.next()
            for tt in range(4):
                kb.op("pe", lambda: nc.tensor.matmul(ps[:, tt * 128:(tt + 1) * 128], vl[tt][:, gi * 128:(gi + 1) * 128], wsm[:, gi, :],
                                                     start=True, stop=False), reads=[vl[tt], wsm], writes=[ps], inc=False)
                kb.op("pe", lambda: nc.tensor.matmul(ps[:, tt * 128:(tt + 1) * 128], onesf[0:1, :], bs[0:1, gi * 128:(gi + 1) * 128],
                                                     start=False, stop=True), reads=[onesf, bs], writes=[ps], inc=(tt == 3))
            kb.op("dve", lambda: V.tensor_tensor(ysgu[gi][:], ps[:, 0:TG], ug[gi][:], ALU.mult), reads=[ps, ug[gi]], writes=[ysgu[gi]])

        ybr = [ypool, yssd, yatt, ysgu]
        if dbg and g == 0:
            for b in range(4):
                for j in range(8):
                    kb.dma("act", o_dy[b, j * 128:(j + 1) * 128, :], ybr[b][j][:], reads=[ybr[b][j]])
        for dc in range(KD):
            acc = accr.next()
            for b in range(4):
                wbb = wbbr.next()
                kb.dma("pool", wbb[:], wbr_d[b, dc].rearrange("p (k c) -> p k c", k=8), writes=[wbb])
                psP = psr.next()
                kb.mm(psP, psP[:, 0:TG], [(wbb[:, j, :], ybr[b][j][:], [wbb, ybr[b][j]]) for j in range(8)])
                wg_ = load_wblock(32 + b * 16 + dc)
                psG = psr.next()
                kb.mm(psG, psG[:, 0:TG], hp(wg_, 0, 128))
                sg = tmpr.next()
                kb.op("act", lambda: A.activation(sg[:], psG[:, 0:TG], AF.Sigmoid), reads=[psG], writes=[sg])
                if b == 0:
                    kb.op("dve", lambda: V.tensor_tensor(acc[:], psP[:, 0:TG], sg[:], ALU.mult), reads=[psP, sg], writes=[acc])
                else:
                    kb.op("dve", lambda: V.tensor_tensor(sg[:], psP[:, 0:TG], sg[:], ALU.mult), reads=[psP, sg], writes=[sg])
                    if b < 3:
                        kb.op("pool", lambda: G.tensor_tensor(acc[:], acc[:], sg[:], ALU.add), reads=[acc, sg], writes=[acc])
                    else:
                        kb.op("pool", lambda: G.tensor_tensor(merged[dc][:], acc[:], sg[:], ALU.add), reads=[acc, sg], writes=[merged[dc]])
        if dbg and g == 0:
            for j in range(KD):
                kb.dma("act", o_dm[j * 128:(j + 1) * 128, :], merged[j][:], reads=[merged[j]])
        for dc in range(KD):
            wo = wbr.next()
            kb.dma("pool", wo[:], wout_d[dc].rearrange("p (k c) -> p k c", k=KD), writes=[wo])
            ps = psr.next()
            kb.mm(ps, ps[:, 0:TG], [(wo[:, k, :], merged[k][:], [wo, merged[k]]) for k in range(KD)])
            xt = xload(dc)
            kb.op("dve", lambda: V.scalar_tensor_tensor(xt[:], ps[:, 0:TG], modT[:, 32 + dc:33 + dc], xt[:], ALU.mult, ALU.add),
                  reads=[ps, modT, xt], writes=[xt])
            kb.dma("act", o_x[dc * 128:(dc + 1) * 128, tsl], xt[:], reads=[xt])
    kb.finish()
    return nc


def prep_C1(inp, layer, mod):
    w_in = inp["w_in"][layer]
    cols = np.concatenate([w_in[:, OFF["a_in"]:OFF["a_in"] + 1024], w_in[:, OFF["z"]:OFF["z"] + 1024],
                           w_in[:, OFF["uv"]:OFF["uv"] + 2048], w_in[:, OFF["gates"]:OFF["gates"] + 8192]], axis=1)
    wp = inp["w_pool"][layer]
    wpoolb = np.ascontiguousarray(wp.reshape(4, 2, 128, 256).transpose(0, 2, 1, 3).reshape(4, 128, 512))
    wbr = inp["w_branch"][layer]
    wbrb = np.ascontiguousarray(wbr.reshape(4, 8, 128, 16, 128).transpose(0, 3, 2, 1, 4).reshape(4, 16, 128, 1024))
    wout = inp["w_out"][layer]
    s = np.arange(128)[:, None]
    t = np.arange(128)[None, :]
    return dict(
        modT=np.ascontiguousarray(mod[:, 0:48]),
        winb=_blockify(np.ascontiguousarray(cols), NBC),
        wpoolb=wpoolb,
        pscale_pk=_pk(inp["pool_scale"][layer], 8),
        ssdnorm_pk=_pk(inp["ssd_norm"][layer], 8),
        lng_bc=np.ascontiguousarray(np.broadcast_to(inp["sgu_ln_gain"][layer][None, :], (128, 1024))).astype(np.float32),
        lnb_bc=np.ascontiguousarray(np.broadcast_to(inp["sgu_ln_bias"][layer][None, :], (128, 1024))).astype(np.float32),
        wsT=np.ascontiguousarray(inp["sgu_w_s"][layer].transpose(2, 0, 1)),
        Umask=(s <= t).astype(np.float32),
        bs_row=np.ascontiguousarray(inp["sgu_b_s"][layer].reshape(1, 1024)),
        wbrb=wbrb,
        woutb=_blockify(np.ascontiguousarray(wout), 16),
    )


def run_C1(inp, layer, xT, B_out, S, NTOK, mod, dbg=False):
    ncore = S // NTOK
    shared = prep_C1(inp, layer, mod)
    xTh = np.concatenate([np.zeros((D, 16), np.float32), xT], axis=1)
    maps = []
    for i in range(ncore):
        m = dict(shared)
        m["xTh"] = np.ascontiguousarray(xTh[:, i * NTOK:(i + 1) * NTOK + 16])
        m["yattT"] = np.ascontiguousarray(B_out["yattT"][:, i * NTOK:(i + 1) * NTOK])
        m["yssdT"] = np.ascontiguousarray(B_out["yssdT"][:, i * NTOK:(i + 1) * NTOK])
        m["haloflag"] = np.full((128, 1), 0.0 if i == 0 else 1.0, np.float32)
        pc = np.ones((128, 4, 16), np.float32)
        if i == 0:
            for wi, win in enumerate((2, 4, 8, 16)):
                tt = np.arange(16)
                pc[:, wi, :] = (win / np.minimum(tt + 1, win))[None, :]
        m["pcorr"] = pc
        maps.append(m)
    nc = build_C1(NTOK, dbg)
    res = run_bass_kernel_spmd(nc, maps, core_ids=list(range(ncore)))
    if dbg:
        return res.results
    return np.concatenate([r["xmidT"] for r in res.results], axis=1)


def build_C2(NTOK, NF, expert, SG=2):
    TG = 512
    if (NTOK // TG) % SG != 0:
        SG = 1
    NSG = NTOK // (TG * SG)
    NH = NF // 2
    nc = bass.Bass("TRN2", target_bir_lowering=False)
    xT = _din(nc, "xT", [D, NTOK])
    mod_d = _din(nc, "modT", [128, 48])
    w1_d = _din(nc, "w1b", [NF, 128, KD * 128])
    w3_d = _din(nc, "w3b", [NF, 128, KD * 128])
    w2_d = _din(nc, "w2b", [16, 128, NF * 128])
    if expert:
        wrow_d = _din(nc, "wrow", [1, NTOK])
    o_x = _dout(nc, "xoutT", [D, NTOK])

    kb = KB(nc)
    V = nc.vector
    A = nc.scalar
    G = nc.gpsimd
    psr = kb.ring(8, [128, 512], F32, "ps", psum=True)
    ones = kb.sb([128, 128], BF16, "ones")
    onesf = kb.sb([1, 128], F32, "onesf")
    epsb = kb.sb([128, 1], F32, "eps")
    kb.op("pool", lambda: G.memset(ones[:], 1.0), writes=[ones])
    kb.op("pool", lambda: G.memset(onesf[:], 1.0), writes=[onesf])
    kb.op("pool", lambda: G.memset(epsb[:], EPS), writes=[epsb])
    modT = _load_mod(kb, mod_d, 48)
    onepT = kb.sb([128, 16], F32, "onep")
    kb.op("dve", lambda: V.tensor_scalar_add(onepT[:], modT[:, 16:32], 1.0), reads=[modT], writes=[onepT])

    xkr = kb.ring(4, [128, TG], F32, "xk")
    hT = [[kb.sb([128, TG], BF16, "h%d_%d" % (s_, k)) for k in range(KD)] for s_ in range(SG)]
    rstd_x = kb.sb([128, TG], F32, "rstdx")
    sqr = kb.ring(2, [128, TG], BF16, "sq")
    tmpr = kb.ring(3, [128, TG], F32, "tmp")
    wbr = kb.ring(4, [128, KD, 128], BF16, "wb")
    w2r = kb.ring(3, [128, NH, 128], BF16, "w2")
    gt = [[kb.sb([128, TG], BF16, "g%d_%d" % (s_, f)) for f in range(NF)] for s_ in range(SG)]
    if expert:
        wrow = kb.sb([1, TG], F32, "wrow")
        wbc = [kb.sb([128, TG], F32, "wbc%d" % s_) for s_ in range(SG)]
    xTv = xT.rearrange("(k p) t -> p k t", p=128)

    for sg in range(NSG):
        tsls = [slice((sg * SG + s_) * TG, (sg * SG + s_ + 1) * TG) for s_ in range(SG)]

        def mk_xload(tsl):
            def xload(k):
                xt = xkr.next()
                kb.dma("sp", xt[:], xTv[:, k, tsl], writes=[xt])
                return xt
            return xload
        for s_ in range(SG):
            if expert:
                kb.dma("sp", wrow[:], wrow_d[:, tsls[s_]], writes=[wrow])
                ps = psr.next()
                kb.op("pe", lambda: nc.tensor.matmul(ps[:, 0:TG], onesf[0:1, :], wrow[0:1, :], start=True, stop=True),
                      reads=[onesf, wrow], writes=[ps])
                kb.op("act", lambda: A.copy(wbc[s_][:], ps[:, 0:TG]), reads=[ps], writes=[wbc[s_]])
            _emit_norm_mod2(kb, mk_xload(tsls[s_]), hT[s_], modT, onepT, ones, epsb, psr, sqr, tmpr, TG, rstd_x)
        for f in range(NF):
            w1 = wbr.next()
            kb.dma("pool", w1[:], w1_d[f].rearrange("p (k c) -> p k c", k=KD), writes=[w1])
            w3 = wbr.next()
            kb.dma("pool", w3[:], w3_d[f].rearrange("p (k c) -> p k c", k=KD), writes=[w3])
            for s_ in range(SG):
                p1 = psr.next()
                kb.mm(p1, p1[:, 0:TG], [(w1[:, k, :], hT[s_][k][:], [w1, hT[s_][k]]) for k in range(KD)])
                p3 = psr.next()
                kb.mm(p3, p3[:, 0:TG], [(w3[:, k, :], hT[s_][k][:], [w3, hT[s_][k]]) for k in range(KD)])
                s1 = tmpr.next()
                kb.op("act", lambda: A.activation(s1[:], p1[:, 0:TG], AF.Silu), reads=[p1], writes=[s1])
                if expert:
                    kb.op("pool", lambda: G.tensor_tensor(s1[:], s1[:], wbc[s_][:], ALU.mult), reads=[s1, wbc[s_]], writes=[s1])
                kb.op("dve", lambda: V.tensor_tensor(gt[s_][f][:], p3[:, 0:TG], s1[:], ALU.mult), reads=[p3, s1], writes=[gt[s_][f]])
        for dc in range(KD):
            w2h = []
            for hf in range(2):
                w2 = w2r.next()
                kb.dma("pool", w2[:], w2_d[dc][:, hf * NH * 128:(hf + 1) * NH * 128].rearrange("p (f c) -> p f c", f=NH), writes=[w2])
                w2h.append(w2)
            for s_ in range(SG):
                ps = psr.next()
                kb.mm(ps, ps[:, 0:TG], [(w2h[f // NH][:, f % NH, :], gt[s_][f][:], [w2h[f // NH], gt[s_][f]]) for f in range(NF)])
                xo = xkr.next()
                if expert:
                    kb.op("act", lambda: A.activation(xo[:], ps[:, 0:TG], AF.Identity, scale=modT[:, 32 + dc:33 + dc]),
                          reads=[ps, modT], writes=[xo])
                else:
                    kb.dma("sp", xo[:], xTv[:, dc, tsls[s_]], writes=[xo])
                    kb.op("dve", lambda: V.scalar_tensor_tensor(xo[:], ps[:, 0:TG], modT[:, 32 + dc:33 + dc], xo[:], ALU.mult, ALU.add),
                          reads=[ps, modT, xo], writes=[xo])
                kb.dma("act", o_x[dc * 128:(dc + 1) * 128, tsls[s_]], xo[:], reads=[xo])
    kb.finish()
    return nc


def _blockify_w2(w2, nf):
    w = w2.reshape(nf, 128, 16, 128)
    return np.ascontiguousarray(w.transpose(2, 1, 0, 3).reshape(16, 128, nf * 128))


def run_C2_dense(inp, layer, xT, S, NTOK, mod):
    ncore = S // NTOK
    idx = layer // 2
    nf = 5632 // 128
    shared = dict(modT=np.ascontiguousarray(mod[:, 48:96]),
                  w1b=_blockify(np.ascontiguousarray(inp["ffn_w1"][idx]), nf),
                  w3b=_blockify(np.ascontiguousarray(inp["ffn_w3"][idx]), nf),
                  w2b=_blockify_w2(inp["ffn_w2"][idx], nf))
    maps = []
    for i in range(ncore):
        m = dict(shared)
        m["xT"] = np.ascontiguousarray(xT[:, i * NTOK:(i + 1) * NTOK])
        maps.append(m)
    res = run_bass_kernel_spmd(build_C2(NTOK, nf, False), maps, core_ids=list(range(ncore)))
    return np.concatenate([r["xoutT"] for r in res.results], axis=1)


def build_R(NTOK):
    TG = 512
    NG = NTOK // TG
    nc = bass.Bass("TRN2", target_bir_lowering=False)
    xT = _din(nc, "xT", [D, NTOK])
    mod_d = _din(nc, "modT", [128, 48])
    wr_d = _din(nc, "wr_pk", [128, KD, 8])
    o_w = _dout(nc, "wt", [NTOK, 8])
    kb = KB(nc)
    V = nc.vector
    A = nc.scalar
    G = nc.gpsimd
    psr = kb.ring(4, [128, 512], F32, "ps", psum=True)
    ones = kb.sb([128, 128], BF16, "ones")
    epsb = kb.sb([128, 1], F32, "eps")
    kb.op("pool", lambda: G.memset(ones[:], 1.0), writes=[ones])
    kb.op("pool", lambda: G.memset(epsb[:], EPS), writes=[epsb])
    modT = _load_mod(kb, mod_d, 48)
    onepT = kb.sb([128, 16], F32, "onep")
    kb.op("dve", lambda: V.tensor_scalar_add(onepT[:], modT[:, 16:32], 1.0), reads=[modT], writes=[onepT])
    wr = kb.sb([128, KD, 8], F32, "wr")
    kb.dma("sp", wr[:], wr_d, writes=[wr])
    xk = [kb.sb([128, TG], F32, "x%d" % k) for k in range(KD)]
    h32 = [kb.sb([128, TG], F32, "h%d" % k) for k in range(KD)]
    sqr = kb.ring(3, [128, TG], BF16, "sq")
    rstd = kb.sb([128, TG], F32, "rstd")
    lg = kb.sb([128, 4, 8], F32, "lg")
    l2 = kb.sb([128, 4, 8], F32, "l2")
    mk1 = kb.sb([128, 4, 8], F32, "mk1")
    mk2 = kb.sb([128, 4, 8], F32, "mk2")
    wt = kb.sb([128, 4, 8], F32, "wt")
    mm_ = kb.sb([128, 4, 4], F32, "mm_")
    xTv = xT.rearrange("(k p) t -> p k t", p=128)
    for g in range(NG):
        tsl = slice(g * TG, (g + 1) * TG)
        for k in range(KD):
            kb.dma("sp", xk[k][:], xTv[:, k, tsl], writes=[xk[k]])
        ps = psr.next()
        for k in range(KD):
            sq = sqr.next()
            kb.op("act", lambda: A.activation(sq[:], xk[k][:], AF.Square), reads=[xk[k]], writes=[sq])
            kb.op("pe", lambda: nc.tensor.matmul(ps[:, 0:TG], ones[:], sq[:], start=(k == 0), stop=(k == KD - 1)),
                  reads=[ones, sq], writes=[ps])
        _rstd(kb, ps, rstd, D, epsb)
        for k in range(KD):
            kb.op("dve", lambda: V.tensor_tensor(h32[k][:], xk[k][:], rstd[:], ALU.mult), reads=[xk[k], rstd], writes=[h32[k]])
            kb.op("act", lambda: A.activation(h32[k][:], h32[k][:], AF.Identity, bias=modT[:, k:k + 1], scale=onepT[:, k:k + 1]),
                  reads=[h32[k], modT, onepT], writes=[h32[k]])
        ps = psr.next()
        for tt in range(4):
            kb.mm(ps, ps[:, tt * 8:(tt + 1) * 8],
                  [(h32[k][:, tt * 128:(tt + 1) * 128], wr[:, k, :], [h32[k], wr]) for k in range(KD)])
        kb.op("dve", lambda: V.tensor_copy(lg[:].rearrange("p a b -> p (a b)"), ps[:, 0:32]), reads=[ps], writes=[lg])
        for tt in range(4):
            kb.op("dve", lambda: V.reduce_max(mm_[:, tt, 0:1], lg[:, tt, :], axis=AX.X), reads=[lg], writes=[mm_])
            kb.op("dve", lambda: V.tensor_scalar(mk1[:, tt, :], lg[:, tt, :], mm_[:, tt, 0:1], None, ALU.is_equal),
                  reads=[lg, mm_], writes=[mk1])
            kb.op("dve", lambda: V.scalar_tensor_tensor(l2[:, tt, :], mk1[:, tt, :], -1e30, lg[:, tt, :], ALU.mult, ALU.add),
                  reads=[mk1, lg], writes=[l2])
            kb.op("dve", lambda: V.reduce_max(mm_[:, tt, 1:2], l2[:, tt, :], axis=AX.X), reads=[l2], writes=[mm_])
            kb.op("dve", lambda: V.tensor_scalar(mk2[:, tt, :], l2[:, tt, :], mm_[:, tt, 1:2], None, ALU.is_equal),
                  reads=[l2, mm_], writes=[mk2])
            kb.op("dve", lambda: V.tensor_tensor(mm_[:, tt, 2:3], mm_[:, tt, 1:2], mm_[:, tt, 0:1], ALU.subtract), reads=[mm_], writes=[mm_])
            kb.op("act", lambda: A.activation(mm_[:, tt, 2:3], mm_[:, tt, 2:3], AF.Exp), reads=[mm_], writes=[mm_])
            kb.op("dve", lambda: V.tensor_scalar_add(mm_[:, tt, 3:4], mm_[:, tt, 2:3], 1.0), reads=[mm_], writes=[mm_])
            kb.op("dve", lambda: V.reciprocal(mm_[:, tt, 3:4], mm_[:, tt, 3:4]), reads=[mm_], writes=[mm_])
            kb.op("dve", lambda: V.tensor_tensor(mm_[:, tt, 2:3], mm_[:, tt, 2:3], mm_[:, tt, 3:4], ALU.mult), reads=[mm_], writes=[mm_])
            kb.op("dve", lambda: V.tensor_scalar_mul(wt[:, tt, :], mk1[:, tt, :], mm_[:, tt, 3:4]), reads=[mk1, mm_], writes=[wt])
            kb.op("dve", lambda: V.scalar_tensor_tensor(wt[:, tt, :], mk2[:, tt, :], mm_[:, tt, 2:3], wt[:, tt, :], ALU.mult, ALU.add),
                  reads=[mk2, mm_, wt], writes=[wt])
        kb.dma("act", o_w[tsl, :].rearrange("(t p) e -> p t e", p=128), wt[:], reads=[wt])
    kb.finish()
    return nc


def run_R(inp, layer, xT, S, NTOK, mod):
    ncore = S // NTOK
    idx = layer // 2
    shared = dict(modT=np.ascontiguousarray(mod[:, 48:96]),
                  wr_pk=np.ascontiguousarray(inp["moe_router"][idx].reshape(KD, 128, 8).transpose(1, 0, 2)))
    maps = []
    for i in range(ncore):
        m = dict(shared)
        m["xT"] = np.ascontiguousarray(xT[:, i * NTOK:(i + 1) * NTOK])
        maps.append(m)
    res = run_bass_kernel_spmd(build_R(NTOK), maps, core_ids=list(range(ncore)))
    return np.concatenate([r["wt"] for r in res.results], axis=0)


def build_S(NTOK):
    TG = 512
    nc = bass.Bass("TRN2", target_bir_lowering=False)
    xT = _din(nc, "xT", [D, NTOK])
    y0 = _din(nc, "y0T", [D, NTOK])
    y1 = _din(nc, "y1T", [D, NTOK])
    o = _dout(nc, "xoutT", [D, NTOK])
    kb = KB(nc)
    V = nc.vector
    ar = kb.ring(3, [128, TG], F32, "a")
    br = kb.ring(3, [128, TG], F32, "b")
    cr = kb.ring(3, [128, TG], F32, "c")
    for k in range(KD):
        for g in range(NTOK // TG):
            rs = slice(k * 128, (k + 1) * 128)
            tsl = slice(g * TG, (g + 1) * TG)
            a, b, c = ar.next(), br.next(), cr.next()
            kb.dma("sp", a[:], xT[rs, tsl], writes=[a])
            kb.dma("sp", b[:], y0[rs, tsl], writes=[b])
            kb.dma("sp", c[:], y1[rs, tsl], writes=[c])
            kb.op("dve", lambda: V.tensor_tensor(b[:], b[:], c[:], ALU.add), reads=[b, c], writes=[b])
            kb.op("dve", lambda: V.tensor_tensor(a[:], a[:], b[:], ALU.add), reads=[a, b], writes=[a])
            kb.dma("act", o[rs, tsl], a[:], reads=[a])
    kb.finish()
    return nc


def run_moe(inp, layer, xT, S, NTOK, mod):
    idx = layer // 2
    nf = 7168 // 128
    wt = run_R(inp, layer, xT, S, NTOK, mod)
    sel = wt > 0
    lists = [np.nonzero(sel[:, e])[0] for e in range(8)]
    cap = max(1024, int(-(-max(len(l) for l in lists) // 1024) * 1024))
    maps = []
    for e in range(8):
        tl = lists[e]
        xg = np.zeros((D, cap), np.float32)
        xg[:, :len(tl)] = xT[:, tl]
        wrow = np.zeros((1, cap), np.float32)
        wrow[0, :len(tl)] = wt[tl, e]
        maps.append(dict(xT=xg, wrow=wrow, modT=np.ascontiguousarray(mod[:, 48:96]),
                         w1b=_blockify(np.ascontiguousarray(inp["moe_w1"][idx, e]), nf),
                         w3b=_blockify(np.ascontiguousarray(inp["moe_w3"][idx, e]), nf),
                         w2b=_blockify_w2(inp["moe_w2"][idx, e], nf)))
    res = run_bass_kernel_spmd(build_C2(cap, nf, True), maps, core_ids=list(range(8)))
    y = [np.zeros((D, S), np.float32), np.zeros((D, S), np.float32)]
    nsel = np.zeros(S, np.int64)
    for e in range(8):
        tl = lists[e]
        ye = res.results[e]["xoutT"][:, :len(tl)]
        slot = nsel[tl]
        for sidx in (0, 1):
            m = slot == sidx
            y[sidx][:, tl[m]] = ye[:, m]
        nsel[tl] += 1
    ncore = S // NTOK
    maps = []
    for i in range(ncore):
        sl = slice(i * NTOK, (i + 1) * NTOK)
        maps.append(dict(xT=np.ascontiguousarray(xT[:, sl]), y0T=np.ascontiguousarray(y[0][:, sl]), y1T=np.ascontiguousarray(y[1][:, sl])))
    res = run_bass_kernel_spmd(build_S(NTOK), maps, core_ids=list(range(ncore)))
    return np.concatenate([r["xoutT"] for r in res.results], axis=1)


def kernel(**inp):
    inp = {k: np.asarray(v) for k, v in inp.items()}
    S = inp["x"].shape[1]
    NTOK = S // 8
    xT = np.ascontiguousarray(inp["x"][0].T)
    mods = run_M(inp)
    for layer in range(2):
        A_out = run_A(inp, layer, xT, S, NTOK, mods[layer])
        B_out = run_B(inp, layer, A_out, S)
        del A_out
        xmid = run_C1(inp, layer, xT, B_out, S, NTOK, mods[layer])
        del B_out
        if layer % 2 == 0:
            xT = run_C2_dense(inp, layer, xmid, S, NTOK, mods[layer])
        else:
            xT = run_moe(inp, layer, xmid, S, NTOK, mods[layer])
    return np.ascontiguousarray(xT.T)[None].astype(np.float32)
```

```python
import math
import numpy as np
import ml_dtypes
import concourse.bass as bass
import concourse.mybir as mybir
from concourse.bass_utils import run_bass_kernel_spmd

F32 = mybir.dt.float32
BF16 = mybir.dt.bfloat16
I32 = mybir.dt.int32
AF = mybir.ActivationFunctionType
ALU = mybir.AluOpType
AX = mybir.AxisListType

D = 2048
KD = 16
EPS = 1e-6
OFF = dict(a_in=0, z=1024, xbc=2048, dt=3584, cq=3600, ckv=4368, kr=4880, uv=4944, gates=6992)


class Tl:
    __slots__ = ("t", "name", "last_w", "readers", "root")

    def __init__(self, t, name):
        self.t = t
        self.name = name
        self.last_w = None
        self.readers = {}
        self.root = self

    def __getitem__(self, idx):
        return self.t[idx]


class View:
    __slots__ = ("t", "name", "root")

    def __init__(self, parent, ap, name):
        self.t = ap
        self.name = name
        self.root = parent.root

    def __getitem__(self, idx):
        return self.t[idx]


class Ring:
    def __init__(self, tiles):
        self.tiles = tiles
        self.i = 0

    def next(self):
        t = self.tiles[self.i]
        self.i = (self.i + 1) % len(self.tiles)
        return t


class KB:
    NDMA = 8

    def __init__(self, nc):
        self.nc = nc
        self.E = {"pe": nc.tensor, "act": nc.scalar, "dve": nc.vector,
                  "pool": nc.gpsimd, "sp": nc.sync}
        self.sems = {}
        self.cnt = {}
        for k in self.E:
            self.sems[k] = nc.alloc_semaphore("c_" + k)
            self.cnt[k] = 0
        self.dma_rr = {}
        for q in ("sp", "pool", "act"):
            self.dma_rr[q] = 0
            for i in range(self.NDMA):
                key = ("d", q, i)
                self.sems[key] = nc.alloc_semaphore("d_%s_%d" % (q, i))
                self.cnt[key] = 0
        self.seen = {k: {} for k in self.E}
        self.ntile = 0
        self.pending_out = []

    def sb(self, shape, dt, name="t"):
        self.ntile += 1
        return Tl(self.nc.alloc_sbuf_tensor("%s_%d" % (name, self.ntile), list(shape), dt), name)

    def ps(self, shape, dt=F32, name="p"):
        self.ntile += 1
        return Tl(self.nc.alloc_psum_tensor("%s_%d" % (name, self.ntile), list(shape), dt), name)

    def ring(self, n, shape, dt, name="r", psum=False):
        f = self.ps if psum else self.sb
        return Ring([f(shape, dt, name) for _ in range(n)])

    def _wait(self, e, deps):
        need = {}
        for d in deps:
            if d is None:
                continue
            k, v = d
            if k == e and e == "pe":
                continue
            if need.get(k, 0) < v:
                need[k] = v
        seen = self.seen[e]
        for k, v in need.items():
            if seen.get(k, 0) >= v:
                continue
            self.E[e].wait_ge(self.sems[k], v)
            seen[k] = v

    def _deps(self, reads, writes):
        deps = []
        for r in reads:
            deps.append(r.root.last_w)
        for w in writes:
            deps.append(w.root.last_w)
            deps.extend(w.root.readers.items())
        return deps

    def _commit(self, tok, reads, writes):
        writes = [w.root for w in writes]
        reads = [r.root for r in reads]
        for w in writes:
            w.last_w = tok
            w.readers = {}
        k, v = tok
        for r in reads:
            if r in writes:
                continue
            if r.readers.get(k, 0) < v:
                r.readers[k] = v

    def op(self, e, fn, reads=(), writes=(), inc=True):
        self._wait(e, self._deps(reads, writes))
        ins = fn()
        if inc:
            self.cnt[e] += 1
            ins.then_inc(self.sems[e], 1)
            tok = (e, self.cnt[e])
        else:
            tok = (e, self.cnt[e] + 1)
        self._commit(tok, reads, writes)
        return ins

    def dma(self, q, out, in_, reads=(), writes=(), **kw):
        i = self.dma_rr[q]
        self.dma_rr[q] = (i + 1) % self.NDMA
        key = ("d", q, i)
        deps = self._deps(reads, writes)
        deps.append((key, self.cnt[key]))
        self._wait(q, deps)
        ins = self.E[q].dma_start(out=out, in_=in_, **kw)
        self.cnt[key] += 16
        ins.then_inc(self.sems[key], 16)
        tok = (key, self.cnt[key])
        self._commit(tok, reads, writes)
        if not writes:
            self.pending_out.append(tok)
        return ins

    def finish(self):
        last = {}
        for k, v in self.pending_out:
            last[k] = max(last.get(k, 0), v)
        self._wait("sp", list(last.items()))
        self._wait("sp", [(k, self.cnt[k]) for k in ("pe", "act", "dve", "pool") if self.cnt[k]])

    def mm(self, ps, out_ap, pairs):
        nc = self.nc
        n = len(pairs)
        for i, (l, r, rd) in enumerate(pairs):
            self.op("pe", lambda: nc.tensor.matmul(out_ap, l, r, start=(i == 0), stop=(i == n - 1)),
                    reads=rd, writes=[ps], inc=(i == n - 1))


def _din(nc, name, shape, dt=F32):
    return nc.dram_tensor(name, list(shape), dt, kind="ExternalInput").ap()


def _dout(nc, name, shape, dt=F32):
    return nc.dram_tensor(name, list(shape), dt, kind="ExternalOutput").ap()


def _rstd(kb, ps_ss, out, n, epsb):
    nc = kb.nc
    kb.op("act", lambda: nc.scalar.activation(out[:], ps_ss[:], AF.Sqrt, bias=epsb[:, 0:1], scale=1.0 / n),
          reads=[ps_ss, epsb], writes=[out])
    kb.op("dve", lambda: nc.vector.reciprocal(out[:], out[:]), reads=[out], writes=[out])


def _emit_mod(kb, c_pk, wada, bada, ncol, psr):
    nc = kb.nc
    nj = ncol // 128
    cs = kb.sb([128, 16], F32, "cs")
    cact = kb.sb([128, 16], F32, "cact")
    bsb = kb.sb([128, nj], F32, "bada")
    modT = kb.sb([128, nj], F32, "modT")
    kb.dma("sp", cs[:], c_pk, writes=[cs])
    kb.dma("sp", bsb[:], bada, writes=[bsb])
    kb.op("act", lambda: nc.scalar.activation(cact[:], cs[:], AF.Silu), reads=[cs], writes=[cact])
    war = kb.ring(2, [128, 16, 128], F32, "wada")
    ps = psr.next()
    for j in range(nj):
        wa = war.next()
        kb.dma("sp", wa[:], wada[j].rearrange("p (k c) -> p k c", k=16), writes=[wa])
        kb.mm(ps, ps[:, j:j + 1], [(wa[:, k, :], cact[:, k:k + 1], [wa, cact]) for k in range(16)])
    kb.op("dve", lambda: nc.vector.tensor_tensor(modT[:], ps[:, 0:nj], bsb[:], ALU.add),
          reads=[ps, bsb], writes=[modT])
    return modT


def _load_mod(kb, mod_d, nj):
    t = kb.sb([128, nj], F32, "modT")
    kb.dma("sp", t[:], mod_d, writes=[t])
    return t


def build_M():
    nc = bass.Bass("TRN2", target_bir_lowering=False)
    c_pk = _din(nc, "c_pk", [128, 16])
    wada = _din(nc, "wada", [24, 128, KD * 128])
    bada = _din(nc, "bada_pk", [128, 24])
    o = _dout(nc, "modT", [128, 24])
    kb = KB(nc)
    psr = kb.ring(2, [128, 512], F32, "ps", psum=True)
    modT = _emit_mod(kb, c_pk, wada, bada, 24 * 128, psr)
    kb.dma("sp", o, modT[:], reads=[modT])
    kb.finish()
    return nc


def run_M(inp):
    maps = []
    for i in range(8):
        w = np.concatenate([inp["w_ada"][l][:, i * 1536:(i + 1) * 1536] for l in range(2)], axis=1)
        b = np.concatenate([inp["b_ada"][l][i * 1536:(i + 1) * 1536] for l in range(2)])
        maps.append(dict(c_pk=_pk(inp["c"][0], 16), wada=_blockify(np.ascontiguousarray(w), 24), bada_pk=_pk(b, 24)))
    res = run_bass_kernel_spmd(build_M(), maps, core_ids=list(range(8)))
    out = []
    for l in range(2):
        out.append(np.ascontiguousarray(np.concatenate([r["modT"][:, l * 12:(l + 1) * 12] for r in res.results], axis=1)))
    return out


def _emit_norm_mod(kb, xk, hT, shiftT, onepT, ones, epsb, psr, sqr, tmpr, TG, rstd_t):
    nc = kb.nc
    ps = psr.next()
    for k in range(KD):
        sq = sqr.next()
        kb.op("act", lambda: nc.scalar.activation(sq[:], xk[k][:], AF.Square), reads=[xk[k]], writes=[sq])
        kb.op("pe", lambda: nc.tensor.matmul(ps[:, 0:TG], ones[:], sq[:], start=(k == 0), stop=(k == KD - 1)),
              reads=[ones, sq], writes=[ps], inc=True)
    rstd = rstd_t
    _rstd(kb, ps, rstd, D, epsb)
    for k in range(KD):
        tmp = tmpr.next()
        kb.op("dve", lambda: nc.vector.tensor_tensor(tmp[:], xk[k][:], rstd[:], ALU.mult),
              reads=[xk[k], rstd], writes=[tmp])
        kb.op("act", lambda: nc.scalar.activation(hT[k][:], tmp[:], AF.Identity,
                                                  bias=shiftT[:, k:k + 1], scale=onepT[:, k:k + 1]),
              reads=[tmp, shiftT, onepT], writes=[hT[k]])


PI_LO = 3.1415925
C1_2PI = 6.28125
C2_2PI = 2.0 * math.pi - 6.28125


def _emit_rope_tables(kb, posi, ang, angk, angi, sin2, cos2, rc_s):
    nc = kb.nc
    V = nc.vector
    kb.op("dve", lambda: V.tensor_copy(ang[:], posi[:]), reads=[posi], writes=[ang])
    kb.op("dve", lambda: V.tensor_scalar_mul(ang[:], ang[:], rc_s[:, 0:1]), reads=[ang, rc_s], writes=[ang])
    kb.op("dve", lambda: V.tensor_scalar_mul(angk[:], ang[:], 1.0 / (2.0 * math.pi)), reads=[ang], writes=[angk])
    kb.op("dve", lambda: V.tensor_copy(angi[:], angk[:]), reads=[angk], writes=[angi])
    kb.op("dve", lambda: V.tensor_copy(angk[:], angi[:]), reads=[angi], writes=[angk])
    kb.op("dve", lambda: V.scalar_tensor_tensor(ang[:], angk[:], -C1_2PI, ang[:], ALU.mult, ALU.add),
          reads=[angk, ang], writes=[ang])
    kb.op("dve", lambda: V.scalar_tensor_tensor(ang[:], angk[:], -C2_2PI, ang[:], ALU.mult, ALU.add),
          reads=[angk, ang], writes=[ang])

    def wrap(t):
        kb.op("dve", lambda: V.tensor_scalar(angk[:], t[:], math.pi, -2.0 * math.pi, ALU.is_gt, ALU.mult),
              reads=[t], writes=[angk])
        kb.op("dve", lambda: V.tensor_tensor(t[:], t[:], angk[:], ALU.add), reads=[t, angk], writes=[t])
        kb.op("dve", lambda: V.tensor_scalar(angk[:], t[:], -math.pi, 2.0 * math.pi, ALU.is_lt, ALU.mult),
              reads=[t], writes=[angk])
        kb.op("dve", lambda: V.tensor_tensor(t[:], t[:], angk[:], ALU.add), reads=[t, angk], writes=[t])
        kb.op("dve", lambda: V.tensor_scalar(t[:], t[:], PI_LO, -PI_LO, ALU.min, ALU.max), reads=[t], writes=[t])
    wrap(ang)
    kb.op("dve", lambda: V.tensor_scalar_add(cos2[:], ang[:], 0.5 * math.pi), reads=[ang], writes=[cos2])
    wrap(cos2)
    kb.op("act", lambda: nc.scalar.activation(sin2[:], ang[:], AF.Sin, scale=rc_s[:, 1:2]),
          reads=[ang, rc_s], writes=[sin2])
    kb.op("act", lambda: nc.scalar.activation(cos2[:], cos2[:], AF.Sin), reads=[cos2], writes=[cos2])

def _emit_norm_mod2(kb, xload, hT, shiftT, onepT, ones, epsb, psr, sqr, tmpr, TG, rstd_t, after=None):
    nc = kb.nc
    ps = psr.next()
    for k in range(KD):
        xt = xload(k)
        sq = sqr.next()
        kb.op("act", lambda: nc.scalar.activation(sq[:], xt[:], AF.Square), reads=[xt], writes=[sq])
        kb.op("pe", lambda: nc.tensor.matmul(ps[:, 0:TG], ones[:], sq[:], start=(k == 0), stop=(k == KD - 1)),
              reads=[ones, sq], writes=[ps], inc=True)
    _rstd(kb, ps, rstd_t, D, epsb)
    for k in range(KD):
        xt = xload(k)
        tmp = tmpr.next()
        kb.op("dve", lambda: nc.vector.tensor_tensor(tmp[:], xt[:], rstd_t[:], ALU.mult),
              reads=[xt, rstd_t], writes=[tmp])
        kb.op("act", lambda: nc.scalar.activation(hT[k][:], tmp[:], AF.Identity,
                                                  bias=shiftT[:, k:k + 1], scale=onepT[:, k:k + 1]),
              reads=[tmp, shiftT, onepT], writes=[hT[k]])
        if after is not None:
            after(k, tmp)


NBA = 24


def build_A(NTOK, dbg=False):
    TG = 512
    NG = NTOK // TG
    nc = bass.Bass("TRN2", target_bir_lowering=False)
    xT = _din(nc, "xT", [D, NTOK])
    mod_d = _din(nc, "modT", [128, 32])
    winb = _din(nc, "winb", [NBA, 128, KD * 128])
    dtb = _din(nc, "dtb_bc", [128, 16])
    qn = _din(nc, "qnorm_pk", [128, 6])
    kvn = _din(nc, "kvnorm_pk", [128, 4])
    wuq = _din(nc, "wuq", [768, 2048])
    wukk = _din(nc, "wukv_k", [512, 1024])
    wukv = _din(nc, "wukv_v", [512, 1024])
    qg = _din(nc, "qgain", [128, 3])
    kg = _din(nc, "kgain", [128, 3])
    pos = _din(nc, "pos_bc", [64, NTOK], I32)
    rc = _din(nc, "ropec", [64, 4])
    o_xbc = _dout(nc, "xbcT", [1536, NTOK])
    o_dt = _dout(nc, "dt", [NTOK, 16])
    o_q = _dout(nc, "qT", [8, 192, NTOK], BF16)
    o_k = _dout(nc, "kT", [8, 192, NTOK], BF16)
    o_v = _dout(nc, "v", [NTOK, 1024], BF16)
    if dbg:
        o_dh = _dout(nc, "dbg_h", [D, TG], BF16)
        o_dm = _dout(nc, "dbg_mod", [128, 32])

    kb = KB(nc)
    psr = kb.ring(8, [128, 512], F32, "ps", psum=True)
    ones = kb.sb([128, 128], BF16, "ones")
    epsb = kb.sb([128, 1], F32, "eps")
    kb.op("pool", lambda: nc.gpsimd.memset(ones[:], 1.0), writes=[ones])
    kb.op("pool", lambda: nc.gpsimd.memset(epsb[:], EPS), writes=[epsb])

    modT = _load_mod(kb, mod_d, 32)
    onepT = kb.sb([128, 16], F32, "onep")
    kb.op("dve", lambda: nc.vector.tensor_scalar_add(onepT[:], modT[:, 16:32], 1.0), reads=[modT], writes=[onepT])

    def load_small(ap, shape, dt=F32, name="c"):
        t = kb.sb(shape, dt, name)
        kb.dma("sp", t[:], ap, writes=[t])
        return t
    dtb_s = load_small(dtb, [128, 16])
    qn_s = load_small(qn, [128, 6])
    kvn_s = load_small(kvn, [128, 4])
    qg_s = load_small(qg, [128, 3])
    kg_s = load_small(kg, [128, 3])
    rc_s = load_small(rc, [64, 4])
    wuq_s = kb.sb([128, 6, 2048], BF16, "wuq")
    wukk_s = kb.sb([128, 4, 1024], BF16, "wukk")
    wukv_s = kb.sb([128, 4, 1024], BF16, "wukv")
    wuq_v = wuq.rearrange("(k p) c -> p k c", p=128)
    for j in range(6):
        kb.dma("pool", wuq_s[:, j, :], wuq_v[:, j, :], writes=[wuq_s])
    kb.dma("pool", wukk_s[:], wukk.rearrange("(k p) c -> p k c", p=128), writes=[wukk_s])
    kb.dma("pool", wukv_s[:], wukv.rearrange("(k p) c -> p k c", p=128), writes=[wukv_s])

    xk = [kb.sb([128, TG], F32, "x%d" % k) for k in range(KD)]
    hT = [kb.sb([128, TG], BF16, "h%d" % k) for k in range(KD)]
    sqr = kb.ring(3, [128, TG], BF16, "sq")
    tmpr = kb.ring(6, [128, TG], F32, "tmp")
    rstd_x = kb.sb([128, TG], F32, "rstdx")
    wbr = kb.ring(7, [128, KD, 128], BF16, "wb")
    stg = kb.ring(3, [128, TG], F32, "stg")
    stgb = kb.ring(4, [128, TG], BF16, "stgb")
    cq_s = [kb.sb([128, TG], F32, "cq%d" % j) for j in range(6)]
    cqn = [kb.sb([128, TG], BF16, "cqn%d" % j) for j in range(6)]
    ckv_s = [kb.sb([128, TG], F32, "ckv%d" % j) for j in range(4)]
    ckvn = [kb.sb([128, TG], BF16, "ckvn%d" % j) for j in range(4)]
    kr_s = kb.sb([64, TG], F32, "kr")
    krs_s = kb.sb([64, TG], F32, "krs")
    krsq = kb.sb([64, TG], BF16, "krsq")
    posi = kb.sb([64, TG], I32, "posi")
    ang = kb.sb([64, TG], F32, "ang")
    angk = kb.sb([64, TG], F32, "angk")
    angi = kb.sb([64, TG], I32, "angi")
    cos2 = kb.sb([64, TG], F32, "cos2")
    sin2 = kb.sb([64, TG], F32, "sin2")
    dts = kb.sb([128, 4, 16], F32, "dts")
    xTv = xT.rearrange("(k p) t -> p k t", p=128)

    def load_wblock(b):
        wb = wbr.next()
        kb.dma("pool", wb[:], winb[b].rearrange("p (k c) -> p k c", k=KD), writes=[wb])
        return wb

    for g in range(NG):
        t0 = g * TG
        tsl = slice(t0, t0 + TG)
        for k in range(KD):
            kb.dma("sp", xk[k][:], xTv[:, k, tsl], writes=[xk[k]])
        kb.dma("sp", posi[:], pos[:, tsl], writes=[posi])
        _emit_norm_mod(kb, xk, hT, modT, onepT, ones, epsb, psr, sqr, tmpr, TG, rstd_x)
        _emit_rope_tables(kb, posi, ang, angk, angi, sin2, cos2, rc_s)
        if dbg and g == 0:
            for k in range(KD):
                kb.dma("act", o_dh[k * 128:(k + 1) * 128, :], hT[k][:], reads=[hT[k]])
            kb.dma("act", o_dm, modT[:], reads=[modT])

        def hp(wb, c0, c1):
            return [(wb[:, k, c0:c1], hT[k][:], [wb, hT[k]]) for k in range(KD)]

        for b in range(12):
            wb = load_wblock(b)
            ps = psr.next()
            kb.mm(ps, ps[:, 0:TG], hp(wb, 0, 128))
            st = stg.next()
            if b % 2 == 0:
                kb.op("act", lambda: nc.scalar.copy(st[:], ps[:, 0:TG]), reads=[ps], writes=[st])
            else:
                kb.op("dve", lambda: nc.vector.tensor_copy(st[:], ps[:, 0:TG]), reads=[ps], writes=[st])
            kb.dma("act", o_xbc[b * 128:(b + 1) * 128, tsl], st[:], reads=[st])

        def lat(b0, nb, dst, dstn, gains, nfeat):
            pss = psr.next()
            for j in range(nb):
                wb = load_wblock(b0 + j)
                ps = psr.next()
                kb.mm(ps, ps[:, 0:TG], hp(wb, 0, 128))
                kb.op("act", lambda: nc.scalar.copy(dst[j][:], ps[:, 0:TG]), reads=[ps], writes=[dst[j]])
                sq = sqr.next()
                kb.op("act", lambda: nc.scalar.activation(sq[:], ps[:, 0:TG], AF.Square), reads=[ps], writes=[sq])
                kb.op("pe", lambda: nc.tensor.matmul(pss[:, 0:TG], ones[:], sq[:], start=(j == 0), stop=(j == nb - 1)),
                      reads=[ones, sq], writes=[pss])
            r = tmpr.next()
            _rstd(kb, pss, r, nfeat, epsb)
            for j in range(nb):
                kb.op("dve", lambda: nc.vector.scalar_tensor_tensor(dstn[j][:], dst[j][:], gains[:, j:j + 1], r[:],
                                                                    ALU.mult, ALU.mult),
                      reads=[dst[j], gains, r], writes=[dstn[j]])
        lat(12, 6, cq_s, cqn, qn_s, 768)
        lat(18, 4, ckv_s, ckvn, kvn_s, 512)

        wb = load_wblock(22)
        ps = psr.next()
        kb.mm(ps, ps[0:64, 0:TG], hp(wb, 0, 64))
        kb.op("act", lambda: nc.scalar.copy(kr_s[:], ps[0:64, 0:TG]), reads=[ps], writes=[kr_s])
        kb.op("act", lambda: nc.scalar.activation(krsq[:], ps[0:64, 0:TG], AF.Square), reads=[ps], writes=[krsq])
        ps = psr.next()
        kb.mm(ps, ps[0:64, 0:TG], hp(wb, 64, 128))
        kb.op("act", lambda: nc.scalar.copy(krs_s[:], ps[0:64, 0:TG]), reads=[ps], writes=[krs_s])

        wb = load_wblock(23)
        ps = psr.next()
        for tt in range(TG // 128):
            kb.mm(ps, ps[:, tt * 16:(tt + 1) * 16],
                  [(hT[k][:, tt * 128:(tt + 1) * 128], wb[:, k, 0:16], [wb, hT[k]]) for k in range(KD)])
        for tt in range(TG // 128):
            kb.op("dve", lambda: nc.vector.tensor_tensor(dts[:, tt, :], ps[:, tt * 16:(tt + 1) * 16], dtb_s[:], ALU.add),
                  reads=[ps, dtb_s], writes=[dts])
        kb.op("act", lambda: nc.scalar.activation(dts[:], dts[:], AF.Exp), reads=[dts], writes=[dts])
        kb.op("act", lambda: nc.scalar.activation(dts[:], dts[:], AF.Ln, bias=1.0, scale=1.0), reads=[dts], writes=[dts])
        kb.dma("act", o_dt[tsl, :].rearrange("(t p) h -> p t h", p=128), dts[:], reads=[dts])

        def head(src_n_pairs, rope_src, gains, dst, h):
            psn = psr.next()
            kb.mm(psn, psn[:, 0:TG], src_n_pairs)
            sqn = sqr.next()
            kb.op("act", lambda: nc.scalar.activation(sqn[:], psn[:, 0:TG], AF.Square), reads=[psn], writes=[sqn])
            if rope_src is None:
                psr_ = psr.next()
                kb.mm(psr_, psr_[0:64, 0:TG], [(wuq_s[:, j, h * 256 + 128:h * 256 + 192], cqn[j][:], [wuq_s, cqn[j]]) for j in range(6)])
                pss_ = psr.next()
                kb.mm(pss_, pss_[0:64, 0:TG], [(wuq_s[:, j, h * 256 + 192:h * 256 + 256], cqn[j][:], [wuq_s, cqn[j]]) for j in range(6)])
                sqr_t = sqr.next()
                kb.op("act", lambda: nc.scalar.activation(sqr_t[0:64, :], psr_[0:64, 0:TG], AF.Square), reads=[psr_], writes=[sqr_t])
                r_ap, s_ap, r_t, s_t = psr_[0:64, 0:TG], pss_[0:64, 0:TG], psr_, pss_
            else:
                sqr_t = krsq
                r_ap, s_ap, r_t, s_t = kr_s[:], krs_s[:], kr_s, krs_s
            pss = psr.next()
            kb.op("pe", lambda: nc.tensor.matmul(pss[:, 0:TG], ones[:], sqn[:], start=True, stop=False),
                  reads=[ones, sqn], writes=[pss], inc=False)
            kb.op("pe", lambda: nc.tensor.matmul(pss[:, 0:TG], ones[0:64, :], sqr_t[0:64, :], start=False, stop=True),
                  reads=[ones, sqr_t], writes=[pss])
            r = tmpr.next()
            _rstd(kb, pss, r, 192, epsb)
            on = stgb.next()
            kb.op("dve", lambda: nc.vector.scalar_tensor_tensor(on[:], psn[:, 0:TG], gains[:, 0:1], r[:], ALU.mult, ALU.mult),
                  reads=[psn, gains, r], writes=[on])
            kb.dma("act", dst[h, 0:128, tsl], on[:], reads=[on])
            t1 = tmpr.next()
            t2 = tmpr.next()
            kb.op("dve", lambda: nc.vector.scalar_tensor_tensor(t1[0:64, :], r_ap, gains[0:64, 1:2], r[0:64, :], ALU.mult, ALU.mult),
                  reads=[r_t, gains, r], writes=[t1])
            kb.op("dve", lambda: nc.vector.tensor_tensor(t1[0:64, :], t1[0:64, :], cos2[:], ALU.mult),
                  reads=[t1, cos2], writes=[t1])
            kb.op("dve", lambda: nc.vector.scalar_tensor_tensor(t2[0:64, :], s_ap, gains[0:64, 2:3], r[0:64, :], ALU.mult, ALU.mult),
                  reads=[s_t, gains, r], writes=[t2])
            kb.op("dve", lambda: nc.vector.tensor_tensor(t2[0:64, :], t2[0:64, :], sin2[:], ALU.mult),
                  reads=[t2, sin2], writes=[t2])
            orp = stgb.next()
            kb.op("dve", lambda: nc.vector.tensor_tensor(orp[0:64, :], t1[0:64, :], t2[0:64, :], ALU.add),
                  reads=[t1, t2], writes=[orp])
            kb.dma("act", dst[h, 128:192, tsl], orp[0:64, :], reads=[orp])

        for h in range(8):
            head([(wuq_s[:, j, h * 256:h * 256 + 128], cqn[j][:], [wuq_s, cqn[j]]) for j in range(6)],
                 None, qg_s, o_q, h)
        for h in range(8):
            head([(wukk_s[:, j, h * 128:(h + 1) * 128], ckvn[j][:], [wukk_s, ckvn[j]]) for j in range(4)],
                 True, kg_s, o_k, h)
        for tt in range(TG // 128):
            for hf in range(2):
                ps = psr.next()
                kb.mm(ps, ps[:, 0:512], [(ckvn[j][:, tt * 128:(tt + 1) * 128], wukv_s[:, j, hf * 512:(hf + 1) * 512],
                                          [ckvn[j], wukv_s]) for j in range(4)])
                vb = stgb.next()
                kb.op("act", lambda: nc.scalar.copy(vb[:, 0:512], ps[:, 0:512]), reads=[ps], writes=[vb])
                kb.dma("act", o_v[t0 + tt * 128:t0 + (tt + 1) * 128, hf * 512:(hf + 1) * 512], vb[:, 0:512], reads=[vb])
    kb.finish()
    return nc


def _pk(v, n):
    return np.ascontiguousarray(np.asarray(v, np.float32).reshape(n, 128).T)


def _blockify(w, nb):
    w = w.reshape(KD, 128, nb, 128)
    return np.ascontiguousarray(w.transpose(2, 1, 0, 3).reshape(nb, 128, KD * 128))


def prep_A(inp, layer, S, mod):
    w_in = inp["w_in"][layer]
    cols = np.zeros((D, NBA * 128), np.float32)
    cols[:, 0:1536] = w_in[:, OFF["xbc"]:OFF["xbc"] + 1536]
    cols[:, 1536:2304] = w_in[:, OFF["cq"]:OFF["cq"] + 768]
    cols[:, 2304:2816] = w_in[:, OFF["ckv"]:OFF["ckv"] + 512]
    kr = w_in[:, OFF["kr"]:OFF["kr"] + 64]
    cols[:, 2816:2880] = kr
    cols[:, 2880:2912] = kr[:, 32:64]
    cols[:, 2912:2944] = kr[:, 0:32]
    cols[:, 2944:2960] = w_in[:, OFF["dt"]:OFF["dt"] + 16]
    wuq = inp["mla_w_uq"][layer].reshape(768, 8, 192)
    wuq2 = np.concatenate([wuq, wuq[:, :, 160:192], wuq[:, :, 128:160]], axis=2).reshape(768, 2048)
    wukv = inp["mla_w_ukv"][layer].reshape(512, 8, 256)

    def gain3(g):
        o = np.zeros((128, 3), np.float32)
        o[:, 0] = g[0:128]
        o[0:64, 1] = g[128:192]
        o[0:32, 2] = g[160:192]
        o[32:64, 2] = g[128:160]
        return o
    half = 32
    invf = (np.float32(10000.0) ** (-np.arange(half, dtype=np.float32) * np.float32(2.0) / np.float32(64))).astype(np.float32)
    rc = np.zeros((64, 4), np.float32)
    rc[:, 0] = np.concatenate([invf, invf])
    sgn = np.concatenate([-np.ones(32), np.ones(32)]).astype(np.float32)
    rc[:, 1] = sgn
    rc[:, 2] = -sgn * np.float32(math.pi)
    rc[:, 3] = -np.float32(math.pi)
    return dict(
        modT=np.ascontiguousarray(mod[:, 0:32]),
        winb=_blockify(cols, NBA),
        dtb_bc=np.ascontiguousarray(np.broadcast_to(inp["ssd_dt_bias"][layer][None, :], (128, 16))).astype(np.float32),
        qnorm_pk=_pk(inp["mla_q_norm"][layer], 6),
        kvnorm_pk=_pk(inp["mla_kv_norm"][layer], 4),
        wuq=np.ascontiguousarray(wuq2),
        wukv_k=np.ascontiguousarray(wukv[:, :, 0:128].reshape(512, 1024)),
        wukv_v=np.ascontiguousarray(wukv[:, :, 128:256].reshape(512, 1024)),
        qgain=gain3(inp["mla_q_gain"][layer]),
        kgain=gain3(inp["mla_k_gain"][layer]),
        ropec=rc,
    )


def run_A(inp, layer, xT, S, NTOK, mod, dbg=False):
    ncore = S // NTOK
    shared = prep_A(inp, layer, S, mod)
    pos = np.asarray(inp["positions"][0, :S]).astype(np.int32)
    maps = []
    for i in range(ncore):
        m = dict(shared)
        m["xT"] = np.ascontiguousarray(xT[:, i * NTOK:(i + 1) * NTOK])
        m["pos_bc"] = np.ascontiguousarray(np.broadcast_to(pos[None, i * NTOK:(i + 1) * NTOK], (64, NTOK)))
        maps.append(m)
    nc = build_A(NTOK, dbg)
    res = run_bass_kernel_spmd(nc, maps, core_ids=list(range(ncore)))
    R = res.results
    if dbg:
        return R
    out = dict(
        xbcT=np.concatenate([r["xbcT"] for r in R], axis=1),
        dt=np.concatenate([r["dt"] for r in R], axis=0),
        qT=np.concatenate([r["qT"] for r in R], axis=2),
        kT=np.concatenate([r["kT"] for r in R], axis=2),
        v=np.concatenate([r["v"] for r in R], axis=0),
    )
    return out


def build_B(S, do_att=True, do_ssd=True, stage=9):
    QG = 512
    NQG = S // QG
    NKB = S // 128
    NCH = S // 128
    nc = bass.Bass("TRN2", target_bir_lowering=False)
    qn_d = _din(nc, "qn", [128, S], BF16)
    qr_d = _din(nc, "qr", [64, S], BF16)
    kn_d = _din(nc, "kn", [128, S], BF16)
    kr_d = _din(nc, "kr", [64, S], BF16)
    v_d = _din(nc, "vb", [128, NKB, 128], BF16)
    mask_d = _din(nc, "masks", [128, 4, QG], BF16)
    slab_d = _din(nc, "slab", [384, S])
    convw_d = _din(nc, "convw", [128, 3, 4])
    convb_d = _din(nc, "convb", [128, 3])
    dt_d = _din(nc, "dth", [128, NCH, 2])
    alog_d = _din(nc, "alog_bc", [128, 2])
    dsk_d = _din(nc, "dskip_bc", [128, 2])
    U_d = _din(nc, "U", [128, 128])
    negm_d = _din(nc, "negmask", [128, 128])
    id_d = _din(nc, "ident", [128, 128])
    o_att = _dout(nc, "yatt", [128, S], BF16)
    o_ssd = _dout(nc, "yssd", [128, S], F32)

    kb = KB(nc)
    V = nc.vector
    A = nc.scalar
    ones = kb.sb([128, 128], BF16, "ones")
    onesf = kb.sb([128, 128], F32, "onesf")
    kb.op("pool", lambda: nc.gpsimd.memset(ones[:], 1.0), writes=[ones])
    kb.op("pool", lambda: nc.gpsimd.memset(onesf[:], 1.0), writes=[onesf])

    def load(ap, shape, dt, name, q="sp"):
        t = kb.sb(shape, dt, name)
        kb.dma(q, t[:], ap, writes=[t])
        return t

    if do_att:
        kn = load(kn_d, [128, S], BF16, "kn")
        kr = load(kr_d, [64, S], BF16, "kr")
        vb = load(v_d, [128, NKB, 128], BF16, "vb")
        masks = load(mask_d, [128, 4, QG], BF16, "masks")
        qnr = kb.ring(2, [128, QG], BF16, "qn")
        qrr = kb.ring(2, [64, QG], BF16, "qr")
        ptr = kb.ring(4, [128, QG], BF16, "pt")
        ps_s = kb.ring(2, [128, QG], F32, "pss", psum=True)
        ps_o = kb.ring(1, [128, QG], F32, "pso", psum=True)
        ps_d = kb.ring(1, [128, QG], F32, "psd", psum=True)
        rec = kb.sb([128, QG], F32, "rec")
        yst = kb.ring(2, [128, QG], BF16, "yst")
        scale = 192.0 ** -0.5
    if do_ssd:
        bX = kb.ps([128, 512], F32, "bankX")
        bY = kb.ps([128, 512], F32, "bankY")
        bZ = [kb.ps([128, 512], F32, "bankZ%d" % h) for h in range(2)]
        p_xt = View(bX, bX[:, 0:128], "p_xt")
        p_bt = View(bX, bX[:, 128:256], "p_bt")
        p_g = View(bX, bX[:, 256:384], "p_g")
        p_acs = View(bX, bX[:, 384:386], "p_acs")
        p_abc = [View(bY, bY[:, h * 128:(h + 1) * 128], "p_abc%d" % h) for h in range(2)]
        p_y = [View(bZ[h], bZ[h][:, 0:128], "p_y%d" % h) for h in range(2)]
        p_s = [View(bZ[h], bZ[h][:, 128:192], "p_s%d" % h) for h in range(2)]
        convw = load(convw_d, [128, 3, 4], F32, "convw")
        convb = load(convb_d, [128, 3], F32, "convb")
        dth = load(dt_d, [128, NCH, 2], F32, "dth")
        alog = load(alog_d, [128, 2], F32, "alog")
        dsk = load(dsk_d, [128, 2], F32, "dsk")
        U = load(U_d, [128, 128], F32, "U")
        negm = load(negm_d, [128, 128], F32, "negm")
        identf = load(id_d, [128, 128], F32, "identf")
        ident = kb.sb([128, 128], BF16, "ident")
        kb.op("dve", lambda: V.tensor_copy(ident[:], identf[:]), reads=[identf], writes=[ident])
        aneg = kb.sb([128, 2], F32, "aneg")
        kb.op("act", lambda: A.activation(aneg[:], alog[:], AF.Exp), reads=[alog], writes=[aneg])
        kb.op("dve", lambda: V.tensor_scalar_mul(aneg[:], aneg[:], -1.0), reads=[aneg], writes=[aneg])
        dI = [kb.sb([128, 128], BF16, "dI%d" % h) for h in range(2)]
        for h in range(2):
            kb.op("dve", lambda: V.tensor_scalar_mul(dI[h][:], identf[:], dsk[:, h:h + 1]), reads=[identf, dsk], writes=[dI[h]])
        CW = 512
        slabr = [kb.ring(2, [128, CW + 3], F32, "slab%d" % j) for j in range(3)]
        accr = kb.ring(2, [128, CW], F32, "cacc")
        xcT = [kb.ring(2, [128, CW], BF16, "xcT%d" % j) for j in range(3)]
        HT = [kb.sb([128, 64], F32, "HT%d" % h) for h in range(2)]
        Hbf = [kb.sb([128, 64], BF16, "Hbf%d" % h) for h in range(2)]
        for h in range(2):
            kb.op("pool", lambda: nc.gpsimd.memset(HT[h][:], 0.0), writes=[HT[h]])
            kb.op("pool", lambda: nc.gpsimd.memset(Hbf[h][:], 0.0), writes=[Hbf[h]])
        xtokr = kb.ring(2, [128, 128], BF16, "xtok")
        btokr = kb.ring(2, [128, 128], BF16, "btok")
        da = kb.ring(2, [128, 2], F32, "da")
        acs = kb.ring(2, [128, 2], F32, "acs")
        darep = kb.ring(2, [128, 128], F32, "darep")
        argr = kb.ring(2, [128, 128], F32, "arg")
        LTr = kb.ring(2, [128, 128], F32, "LT")
        MTr = kb.ring(2, [128, 128], BF16, "MT")
        Er = kb.ring(2, [128, 128], F32, "E")
        CPr = kb.ring(2, [128, 128], BF16, "CP")
        xdtr = kb.ring(2, [128, 64], BF16, "xdt")
        xdtdr = kb.ring(2, [128, 64], BF16, "xdtd")
        cdecr = kb.ring(2, [128, 1], F32, "cdec")
        stmpr = kb.ring(2, [128, 64], F32, "stmp")
        ystg = [kb.ring(2, [64, CW], F32, "ystg%d" % h) for h in range(2)]

    def att_group(g, gen):
        q0 = g * QG
        qn = qnr.next()
        qr = qrr.next()
        kb.dma("sp", qn[:], qn_d[:, q0:q0 + QG], writes=[qn])
        kb.dma("sp", qr[:], qr_d[:, q0:q0 + QG], writes=[qr])
        po = ps_o.next()
        pd = ps_d.next()
        nkb = (g + 1) * 4

        def qk(i):
            ksl = slice(i * 128, (i + 1) * 128)
            ps = ps_s.next()
            kb.op("pe", lambda: nc.tensor.matmul(ps[:], kn[:, ksl], qn[:], start=True, stop=False),
                  reads=[kn, qn], writes=[ps], inc=False)
            kb.op("pe", lambda: nc.tensor.matmul(ps[:], kr[:, ksl], qr[:], start=False, stop=True),
                  reads=[kr, qr], writes=[ps])
            return ps
        cur = qk(0)
        for i in range(nkb):
            nxt = qk(i + 1) if i + 1 < nkb else None
            ps = cur
            pt = ptr.next()
            kb.op("act", lambda: A.activation(pt[:], ps[:], AF.Exp, scale=scale), reads=[ps], writes=[pt])
            d = i - g * 4
            if d >= 0:
                kb.op("dve", lambda: V.tensor_tensor(pt[:], pt[:], masks[:, d, :], ALU.mult), reads=[pt, masks], writes=[pt])
            kb.op("pe", lambda: nc.tensor.matmul(po[:], vb[:, i, :], pt[:], start=(i == 0), stop=(i == nkb - 1)),
                  reads=[vb, pt], writes=[po], inc=False)
            kb.op("pe", lambda: nc.tensor.matmul(pd[:], ones[:], pt[:], start=(i == 0), stop=(i == nkb - 1)),
                  reads=[ones, pt], writes=[pd])
            if gen is not None:
                next(gen, None)
            cur = nxt
        kb.op("dve", lambda: V.reciprocal(rec[:], pd[:]), reads=[pd], writes=[rec])
        ys = yst.next()
        kb.op("dve", lambda: V.tensor_tensor(ys[:], po[:], rec[:], ALU.mult), reads=[po, rec], writes=[ys])
        kb.dma("act", o_att[:, q0:q0 + QG], ys[:], reads=[ys])

    def ssd_piece(pc):
        t0 = pc * CW
        cur = []
        for j in range(3):
            sl = slabr[j].next()
            if pc == 0:
                kb.op("pool", lambda: nc.gpsimd.memset(sl[:, 0:3], 0.0), writes=[sl])
                kb.dma("sp", sl[:, 3:CW + 3], slab_d[j * 128:(j + 1) * 128, 0:CW], writes=[sl])
            else:
                kb.dma("sp", sl[:], slab_d[j * 128:(j + 1) * 128, t0 - 3:t0 + CW], writes=[sl])
            acc = accr.next()
            kb.op("dve", lambda: V.tensor_scalar_mul(acc[:], sl[:, 0:CW], convw[:, j, 0:1]), reads=[sl, convw], writes=[acc])
            for k in range(1, 4):
                kb.op("dve", lambda: V.scalar_tensor_tensor(acc[:], sl[:, k:k + CW], convw[:, j, k:k + 1], acc[:], ALU.mult, ALU.add),
                      reads=[sl, convw, acc], writes=[acc])
            o = xcT[j].next()
            kb.op("act", lambda: A.activation(o[:], acc[:], AF.Silu, bias=convb[:, j:j + 1], scale=1.0),
                  reads=[acc, convb], writes=[o])
            cur.append(o)
            yield
        xT_, BT_, CT_ = cur
        if stage < 2:
            return
        yst2 = [ystg[h].next() for h in range(2)]
        for cc in range(CW // 128):
            c = pc * (CW // 128) + cc
            csl = slice(cc * 128, (cc + 1) * 128)
            kb.op("pe", lambda: nc.tensor.matmul(p_xt[:], xT_[:, csl], ident[:], start=True, stop=True),
                  reads=[xT_, ident], writes=[p_xt])
            kb.op("pe", lambda: nc.tensor.matmul(p_bt[:], BT_[:, csl], ident[:], start=True, stop=True),
                  reads=[BT_, ident], writes=[p_bt])
            kb.op("pe", lambda: nc.tensor.matmul(p_g[:], BT_[:, csl], CT_[:, csl], start=True, stop=True),
                  reads=[BT_, CT_], writes=[p_g])
            yield
            xtok = xtokr.next()
            btok = btokr.next()
            kb.op("act", lambda: A.copy(xtok[:], p_xt[:]), reads=[p_xt], writes=[xtok])
            kb.op("act", lambda: A.copy(btok[:], p_bt[:]), reads=[p_bt], writes=[btok])
            if stage < 3:
                continue
            da_t = da.next()
            kb.op("dve", lambda: V.tensor_tensor(da_t[:], dth[:, c, :], aneg[:], ALU.mult), reads=[dth, aneg], writes=[da_t])
            kb.op("pe", lambda: nc.tensor.matmul(p_acs[:], U[:], da_t[:], start=True, stop=True),
                  reads=[U, da_t], writes=[p_acs])
            dr = []
            for h in range(2):
                drt = darep.next()
                kb.op("act", lambda: A.activation(drt[:], onesf[:], AF.Identity, scale=da_t[:, h:h + 1]),
                      reads=[onesf, da_t], writes=[drt])
                dr.append(drt)
            for h in range(2):
                kb.op("pe", lambda: nc.tensor.matmul(p_abc[h][:], dr[h][:], U[:], start=True, stop=True),
                      reads=[dr[h], U], writes=[p_abc[h]])
            yield
            if stage < 4:
                continue
            acs_t = acs.next()
            kb.op("dve", lambda: V.tensor_copy(acs_t[:], p_acs[:]), reads=[p_acs], writes=[acs_t])
            for h in range(2):
                Abc = p_abc[h][:]
                psa = p_abc[h]
                arg = argr.next()
                kb.op("dve", lambda: V.scalar_tensor_tensor(arg[:], Abc, acs_t[:, h:h + 1], negm[:], ALU.subtract, ALU.add),
                      reads=[psa, acs_t, negm], writes=[arg])
                yield
                LT = LTr.next()
                kb.op("act", lambda: A.activation(LT[:], arg[:], AF.Exp), reads=[arg], writes=[LT])
                MT = MTr.next()
                kb.op("dve", lambda: V.tensor_tensor(MT[:], p_g[:], LT[:], ALU.mult), reads=[p_g, LT], writes=[MT])
                if stage < 5:
                    continue
                E = Er.next()
                kb.op("act", lambda: A.activation(E[:], Abc, AF.Exp), reads=[psa], writes=[E])
                CP = CPr.next()
                kb.op("pool", lambda: nc.gpsimd.tensor_tensor(CP[:], CT_[:, csl], E[:], ALU.mult), reads=[CT_, E], writes=[CP])
                yield
                xdt = xdtr.next()
                kb.op("dve", lambda: V.tensor_scalar_mul(xdt[:], xtok[:, h * 64:(h + 1) * 64], dth[:, c, h:h + 1]),
                      reads=[xtok, dth], writes=[xdt])
                xdtd = xdtdr.next()
                kb.op("dve", lambda: V.tensor_scalar_mul(xdtd[:], xdt[:], LT[:, 127:128]), reads=[xdt, LT], writes=[xdtd])
                cdec = cdecr.next()
                kb.op("act", lambda: A.copy(cdec[:], E[:, 127:128]), reads=[E], writes=[cdec])
                yield
                if stage < 6:
                    continue
                psy = p_y[h]
                pss_ = p_s[h]
                kb.op("pe", lambda: nc.tensor.matmul(psy[0:64, 0:128], xdt[:], MT[:], start=True, stop=False),
                      reads=[xdt, MT], writes=[psy], inc=False)
                kb.op("pe", lambda: nc.tensor.matmul(psy[0:64, 0:128], Hbf[h][:], CP[:], start=False, stop=False),
                      reads=[Hbf[h], CP], writes=[psy], inc=False)
                kb.op("pe", lambda: nc.tensor.matmul(psy[0:64, 0:128], xtok[:, h * 64:(h + 1) * 64], dI[h][:], start=False, stop=True),
                      reads=[xtok, dI[h]], writes=[psy], inc=False)
                kb.op("pe", lambda: nc.tensor.matmul(pss_[:], btok[:], xdtd[:], start=True, stop=True),
                      reads=[btok, xdtd], writes=[psy, pss_])
                yield
                if stage < 7:
                    continue
                kb.op("act", lambda: A.copy(yst2[h][:, csl], psy[0:64, 0:128]), reads=[psy], writes=[yst2[h]])
                if stage < 8:
                    continue
                stmp = stmpr.next()
                kb.op("act", lambda: A.copy(stmp[:], pss_[:]), reads=[pss_], writes=[stmp])
                kb.op("dve", lambda: V.scalar_tensor_tensor(HT[h][:], HT[h][:], cdec[:, 0:1], stmp[:], ALU.mult, ALU.add),
                      reads=[HT[h], cdec, stmp], writes=[HT[h]])
                kb.op("pool", lambda: nc.gpsimd.tensor_copy(Hbf[h][:], HT[h][:]), reads=[HT[h]], writes=[Hbf[h]])
        if stage < 9:
            return
        for h in range(2):
            kb.dma("act", o_ssd[h * 64:(h + 1) * 64, t0:t0 + CW], yst2[h][:], reads=[yst2[h]])

    def ssd_all():
        for pc in range(S // CW):
            yield from ssd_piece(pc)

    gen = ssd_all() if do_ssd else None
    if do_att:
        for g in range(NQG):
            att_group(g, gen)
    if gen is not None:
        for _ in gen:
            pass
    kb.finish()
    return nc


def consts_B():
    k = np.arange(128)[:, None]
    q = np.arange(512)[None, :]
    masks = np.stack([(k + d * 128 <= q) for d in range(4)], axis=1).astype(np.float32).astype(ml_dtypes.bfloat16)
    s = np.arange(128)[:, None]
    l = np.arange(128)[None, :]
    U = (s <= l).astype(np.float32)
    negm = np.where(s <= l, 0.0, -30000.0).astype(np.float32)
    return dict(masks=np.ascontiguousarray(masks), U=U, negmask=negm, ident=np.eye(128, dtype=np.float32))


def run_B(inp, layer, A_out, S, do_att=True, do_ssd=True, stage=9):
    cst = consts_B()
    qT, kT, v = A_out["qT"], A_out["kT"], A_out["v"]
    xbcT, dt = A_out["xbcT"], A_out["dt"]
    cw = inp["ssd_conv_w"][layer]
    cb = inp["ssd_conv_b"][layer]
    NCH = S // 128
    maps = []
    for i in range(8):
        g = i // 4
        rows = np.concatenate([np.arange(i * 128, (i + 1) * 128), 1024 + g * 128 + np.arange(128),
                               1280 + g * 128 + np.arange(128)])
        m = dict(cst)
        m["qn"] = np.ascontiguousarray(qT[i, 0:128])
        m["qr"] = np.ascontiguousarray(qT[i, 128:192])
        m["kn"] = np.ascontiguousarray(kT[i, 0:128])
        m["kr"] = np.ascontiguousarray(kT[i, 128:192])
        m["vb"] = np.ascontiguousarray(v[:, i * 128:(i + 1) * 128].reshape(S // 128, 128, 128).transpose(1, 0, 2))
        m["slab"] = np.ascontiguousarray(xbcT[rows])
        m["convw"] = np.ascontiguousarray(cw[:, rows].T.reshape(3, 128, 4).transpose(1, 0, 2))
        m["convb"] = np.ascontiguousarray(cb[rows].reshape(3, 128).T)
        m["dth"] = np.ascontiguousarray(dt[:, 2 * i:2 * i + 2].reshape(NCH, 128, 2).transpose(1, 0, 2))
        m["alog_bc"] = np.ascontiguousarray(np.broadcast_to(inp["ssd_a_log"][layer][None, 2 * i:2 * i + 2], (128, 2))).astype(np.float32)
        m["dskip_bc"] = np.ascontiguousarray(np.broadcast_to(inp["ssd_d"][layer][None, 2 * i:2 * i + 2], (128, 2))).astype(np.float32)
        maps.append(m)
    nc = build_B(S, do_att, do_ssd, stage)
    res = run_bass_kernel_spmd(nc, maps, core_ids=list(range(8)))
    R = res.results
    return dict(yattT=np.concatenate([r["yatt"] for r in R], axis=0),
                yssdT=np.concatenate([r["yssd"] for r in R], axis=0))


NBC = 96


def build_C1(NTOK, dbg=False):
    TG = 512
    NG = NTOK // TG
    nc = bass.Bass("TRN2", target_bir_lowering=False)
    xTh = _din(nc, "xTh", [D, NTOK + 16])
    yatt_d = _din(nc, "yattT", [1024, NTOK], BF16)
    yssd_d = _din(nc, "yssdT", [1024, NTOK])
    mod_d = _din(nc, "modT", [128, 48])
    winb = _din(nc, "winb", [NBC, 128, KD * 128])
    wpool_d = _din(nc, "wpoolb", [4, 128, 2 * 256])
    pscale_d = _din(nc, "pscale_pk", [128, 8])
    hflag_d = _din(nc, "haloflag", [128, 1])
    pcorr_d = _din(nc, "pcorr", [128, 4, 16])
    snorm_d = _din(nc, "ssdnorm_pk", [128, 8])
    lng_d = _din(nc, "lng_bc", [128, 1024])
    lnb_d = _din(nc, "lnb_bc", [128, 1024])
    wsT_d = _din(nc, "wsT", [128, 8, 128])
    um_d = _din(nc, "Umask", [128, 128])
    bs_d = _din(nc, "bs_row", [1, 1024])
    wbr_d = _din(nc, "wbrb", [4, 16, 128, 8 * 128])
    wout_d = _din(nc, "woutb", [16, 128, KD * 128])
    o_x = _dout(nc, "xmidT", [D, NTOK])
    if dbg:
        o_dy = _dout(nc, "dbg_y", [4, 1024, 512], BF16)
        o_dm = _dout(nc, "dbg_m", [2048, 512], BF16)

    kb = KB(nc)
    V = nc.vector
    A = nc.scalar
    G = nc.gpsimd
    psr = kb.ring(7, [128, 512], F32, "ps", psum=True)
    ps_stat = kb.ps([128, 512], F32, "ps_stat")
    ones = kb.sb([128, 128], BF16, "ones")
    onesf = kb.sb([1, 128], F32, "onesf")
    epsb = kb.sb([128, 1], F32, "eps")
    kb.op("pool", lambda: G.memset(ones[:], 1.0), writes=[ones])
    kb.op("pool", lambda: G.memset(onesf[:], 1.0), writes=[onesf])
    kb.op("pool", lambda: G.memset(epsb[:], EPS), writes=[epsb])
    modT = _load_mod(kb, mod_d, 48)
    onepT = kb.sb([128, 16], F32, "onep")
    kb.op("dve", lambda: V.tensor_scalar_add(onepT[:], modT[:, 16:32], 1.0), reads=[modT], writes=[onepT])

    def load(ap, shape, dt=F32, name="c", q="sp"):
        t = kb.sb(shape, dt, name)
        kb.dma(q, t[:], ap, writes=[t])
        return t
    pscale = load(pscale_d, [128, 8])
    hflag = load(hflag_d, [128, 1])
    pcorr = load(pcorr_d, [128, 4, 16])
    snorm = load(snorm_d, [128, 8])
    lng = load(lng_d, [128, 1024])
    lnb = load(lnb_d, [128, 1024])
    wsTf = load(wsT_d, [128, 8, 128])
    um = load(um_d, [128, 128])
    bs = load(bs_d, [1, 1024])
    wsm = kb.sb([128, 8, 128], BF16, "wsm")
    for gi in range(8):
        kb.op("dve", lambda: V.tensor_tensor(wsm[:, gi, :], wsTf[:, gi, :], um[:], ALU.mult), reads=[wsTf, um], writes=[wsm])
    wpool_s = kb.sb([128, 4, 512], BF16, "wpool")
    for wg in range(4):
        kb.dma("pool", wpool_s[:, wg, :], wpool_d[wg], writes=[wpool_s])

    xkr = kb.ring(4, [128, TG], F32, "xk")
    hT = [kb.sb([128, TG], BF16, "h%d" % k) for k in range(KD)]
    xh = kb.sb([128, KD, 16], F32, "xh")
    hTh = kb.sb([128, KD, 16], BF16, "hTh")
    sqh = kb.sb([128, KD, 16], BF16, "sqh")
    rstd_h = kb.sb([128, 16], F32, "rstdh")
    tmph = kb.sb([128, 16], F32, "tmph")
    rstd_x = kb.sb([128, TG], F32, "rstdx")
    sqr = kb.ring(3, [128, TG], BF16, "sq")
    tmpr = kb.ring(4, [128, TG], F32, "tmp")
    wbr = kb.ring(6, [128, KD, 128], BF16, "wb")
    wbbr = kb.ring(6, [128, 8, 128], BF16, "wbb")
    Ar = kb.ring(2, [128, TG + 16], F32, "A")
    Sr = kb.ring(2, [128, TG + 16], F32, "S")
    plr = kb.ring(4, [128, TG], BF16, "pl")
    ypool = [kb.sb([128, TG], BF16, "ypool%d" % j) for j in range(8)]
    yssd = [kb.sb([128, TG], BF16, "yssd%d" % j) for j in range(8)]
    yssdf = kb.ring(2, [128, TG], F32, "yssdf")
    yatt = [kb.sb([128, TG], BF16, "yatt%d" % j) for j in range(8)]
    ug = [kb.sb([128, TG], BF16, "ug%d" % j) for j in range(8)]
    ysgu = ug
    vt = [kb.sb([128, 1024], F32, "vt%d" % t) for t in range(TG // 128)]
    vl = [kb.sb([128, 1024], BF16, "vl%d" % t) for t in range(TG // 128)]
    vsum = kb.sb([128, 4, 8], F32, "vsum")
    vst = kb.sb([128, 4, 4], F32, "vst")
    junk = kb.sb([128, 1024], BF16, "junk")
    merged = [kb.sb([128, TG], BF16, "mrg%d" % j) for j in range(KD)]
    accr = kb.ring(2, [128, TG], F32, "acc")
    xTv = xTh.rearrange("(k p) t -> p k t", p=128)

    def load_wblock(b):
        wb = wbr.next()
        kb.dma("pool", wb[:], winb[b].rearrange("p (k c) -> p k c", k=KD), writes=[wb])
        return wb

    def hp(wb, c0, c1):
        return [(wb[:, k, c0:c1], hT[k][:], [wb, hT[k]]) for k in range(KD)]

    for g in range(NG):
        t0 = g * TG
        tsl = slice(t0, t0 + TG)
        kb.dma("sp", xh[:], xTv[:, :, t0:t0 + 16], writes=[xh])
        def xload(k):
            xt = xkr.next()
            kb.dma("sp", xt[:], xTv[:, k, t0 + 16:t0 + 16 + TG], writes=[xt])
            return xt
        for j in range(8):
            kb.dma("sp", yatt[j][:], yatt_d[j * 128:(j + 1) * 128, tsl], writes=[yatt[j]])
        _emit_norm_mod2(kb, xload, hT, modT, onepT, ones, epsb, psr, sqr, tmpr, TG, rstd_x)
        kb.op("act", lambda: A.activation(sqh[:], xh[:], AF.Square), reads=[xh], writes=[sqh])
        ps = psr.next()
        kb.mm(ps, ps[:, 0:16], [(ones[:], sqh[:, k, :], [ones, sqh]) for k in range(KD)])
        kb.op("act", lambda: A.activation(rstd_h[:], ps[:, 0:16], AF.Sqrt, bias=epsb[:, 0:1], scale=1.0 / D),
              reads=[ps, epsb], writes=[rstd_h])
        kb.op("dve", lambda: V.reciprocal(rstd_h[:], rstd_h[:]), reads=[rstd_h], writes=[rstd_h])
        for k in range(KD):
            kb.op("dve", lambda: V.scalar_tensor_tensor(tmph[:], xh[:, k, :], onepT[:, k:k + 1], rstd_h[:], ALU.mult, ALU.mult),
                  reads=[xh, onepT, rstd_h], writes=[tmph])
            kb.op("dve", lambda: V.tensor_scalar_add(hTh[:, k, :], tmph[:], modT[:, k:k + 1]), reads=[tmph, modT], writes=[hTh])

        plc = {}
        for blk in range(8):
            plc[blk] = plr.next()
            wb = load_wblock(blk)
            ps = psr.next()
            kb.mm(ps, ps[:, 0:TG], hp(wb, 0, 128))
            ps2 = psr.next()
            kb.mm(ps2, ps2[:, 0:16], [(wb[:, k, :], hTh[:, k, :], [wb, hTh]) for k in range(KD)])
            At = Ar.next()
            kb.op("act", lambda: A.copy(At[:, 16:16 + TG], ps[:, 0:TG]), reads=[ps], writes=[At])
            if g == 0:
                kb.op("dve", lambda: V.tensor_scalar_mul(At[:, 0:16], ps2[:, 0:16], hflag[:, 0:1]), reads=[ps2, hflag], writes=[At])
            else:
                kb.op("dve", lambda: V.tensor_copy(At[:, 0:16], ps2[:, 0:16]), reads=[ps2], writes=[At])
            wi = blk // 2
            W = TG + 16
            cur = At
            sh = 1
            for step in range(wi + 1):
                St = Sr.next()
                eng = "dve"
                E_ = V if eng == "dve" else G
                kb.op(eng, lambda: E_.tensor_tensor(St[:, sh:W], cur[:, sh:W], cur[:, 0:W - sh], ALU.add),
                      reads=[cur], writes=[St])
                cur = St
                sh *= 2
            win = float(2 ** (wi + 1))
            kb.op("dve", lambda: V.scalar_tensor_tensor(plc[blk][:], cur[:, 16:W], 1.0 / win, At[:, 16:W], ALU.mult, ALU.subtract),
                  reads=[cur, At], writes=[plc[blk]])
            if g == 0:
                kb.op("dve", lambda: V.tensor_tensor(cur[:, 16:32], cur[:, 16:32], pcorr[:, wi, :], ALU.mult),
                      reads=[cur, pcorr], writes=[cur])
                kb.op("dve", lambda: V.scalar_tensor_tensor(plc[blk][:, 0:16], cur[:, 16:32], 1.0 / win, At[:, 16:32], ALU.mult, ALU.subtract),
                      reads=[cur, At], writes=[plc[blk]])
            if blk % 2 == 0:
                continue
            wg = blk // 2
            for dh in range(2):
                ps = psr.next()
                kb.mm(ps, ps[:, 0:TG], [(wpool_s[:, wg, kc * 256 + dh * 128:kc * 256 + (dh + 1) * 128], plc[wg * 2 + kc][:],
                                         [wpool_s, plc[wg * 2 + kc]]) for kc in range(2)])
                j = wg * 2 + dh
                kb.op("act", lambda: A.activation(ypool[j][:], ps[:, 0:TG], AF.Identity, scale=pscale[:, j:j + 1]),
                      reads=[ps, pscale], writes=[ypool[j]])

        pss = ps_stat
        for blk in range(8):
            yf = yssdf.next()
            kb.dma("sp", yf[:], yssd_d[blk * 128:(blk + 1) * 128, tsl], writes=[yf])
            wb = load_wblock(8 + blk)
            ps = psr.next()
            kb.mm(ps, ps[:, 0:TG], hp(wb, 0, 128))
            sz = tmpr.next()
            kb.op("act", lambda: A.activation(sz[:], ps[:, 0:TG], AF.Silu), reads=[ps], writes=[sz])
            kb.op("dve", lambda: V.tensor_tensor(sz[:], sz[:], yf[:], ALU.mult), reads=[sz, yf], writes=[sz])
            kb.op("dve", lambda: V.tensor_copy(yssd[blk][:], sz[:]), reads=[sz], writes=[yssd[blk]])
            sq = sqr.next()
            kb.op("act", lambda: A.activation(sq[:], sz[:], AF.Square), reads=[sz], writes=[sq])
            kb.op("pe", lambda: nc.tensor.matmul(pss[:, 0:TG], ones[:], sq[:], start=(blk == 0), stop=(blk == 7)),
                  reads=[ones, sq], writes=[pss])
        r = tmpr.next()
        _rstd(kb, pss, r, 1024, epsb)
        for blk in range(8):
            kb.op("dve", lambda: V.scalar_tensor_tensor(yssd[blk][:], yssd[blk][:], snorm[:, blk:blk + 1], r[:], ALU.mult, ALU.mult),
                  reads=[yssd[blk], snorm, r], writes=[yssd[blk]])

        for blk in range(8):
            wb = load_wblock(16 + blk)
            ps = psr.next()
            kb.mm(ps, ps[:, 0:TG], hp(wb, 0, 128))
            kb.op("act", lambda: A.activation(ug[blk][:], ps[:, 0:TG], AF.Gelu), reads=[ps], writes=[ug[blk]])
        for blk in range(8):
            wb = load_wblock(24 + blk)
            ps = psr.next()
            for tt in range(4):
                kb.mm(ps, ps[:, tt * 128:(tt + 1) * 128],
                      [(hT[k][:, tt * 128:(tt + 1) * 128], wb[:, k, :], [wb, hT[k]]) for k in range(KD)])
            for tt in range(4):
                kb.op("act", lambda: A.activation(vt[tt][:, blk * 128:(blk + 1) * 128], ps[:, tt * 128:(tt + 1) * 128], AF.Gelu,
                                                  accum_out=vsum[:, tt, blk:blk + 1]),
                      reads=[ps], writes=[vt[tt], vsum])
        for tt in range(4):
            kb.op("dve", lambda: V.reduce_sum(vst[:, tt, 0:1], vsum[:, tt, :], axis=AX.X), reads=[vsum], writes=[vst])
            kb.op("dve", lambda: V.tensor_scalar_mul(vst[:, tt, 0:1], vst[:, tt, 0:1], 1.0 / 1024), reads=[vst], writes=[vst])
            kb.op("dve", lambda: V.tensor_scalar_sub(vt[tt][:], vt[tt][:], vst[:, tt, 0:1]), reads=[vt[tt], vst], writes=[vt[tt]])
            kb.op("act", lambda: A.activation(junk[:], vt[tt][:], AF.Square, accum_out=vst[:, tt, 1:2]),
                  reads=[vt[tt]], writes=[junk, vst])
            kb.op("act", lambda: A.activation(vst[:, tt, 2:3], vst[:, tt, 1:2], AF.Sqrt, bias=epsb[:, 0:1], scale=1.0 / 1024),
                  reads=[vst, epsb], writes=[vst])
            kb.op("dve", lambda: V.reciprocal(vst[:, tt, 3:4], vst[:, tt, 2:3]), reads=[vst], writes=[vst])
            kb.op("dve", lambda: V.scalar_tensor_tensor(vt[tt][:], vt[tt][:], vst[:, tt, 3:4], lng[:], ALU.mult, ALU.mult),
                  reads=[vt[tt], vst, lng], writes=[vt[tt]])
            kb.op("dve", lambda: V.tensor_tensor(vl[tt][:], vt[tt][:], lnb[:], ALU.add), reads=[vt[tt], lnb], writes=[vl[tt]])
        for gi in range(8):
            ps = psr.next()
            for tt in range(4):
                kb.op("pe", lambda: nc.tensor.matmul(ps[:, tt * 128:(tt + 1) * 128], vl[tt][:, gi * 128:(gi + 1) * 128], wsm[:, gi, :],
                                                     start=True, stop=False), reads=[vl[tt], wsm], writes=[ps], inc=False)
                kb.op("pe", lambda: nc.tensor.matmul(ps[:, tt * 128:(tt + 1) * 128], onesf[0:1, :], bs[0:1, gi * 128:(gi + 1) * 128],
                                                     start=False, stop=True), reads=[onesf, bs], writes=[ps], inc=(tt == 3))
            kb.op("dve", lambda: V.tensor_tensor(ysgu[gi][:], ps[:, 0:TG], ug[gi][:], ALU.mult), reads=[ps, ug[gi]], writes=[ysgu[gi]])

        ybr = [ypool, yssd, yatt, ysgu]
        if dbg and g == 0:
            for b in range(4):
                for j in range(8):
                    kb.dma("act", o_dy[b, j * 128:(j + 1) * 128, :], ybr[b][j][:], reads=[ybr[b][j]])
        for dc in range(KD):
            acc = accr.next()
            for b in range(4):
                wbb = wbbr.next()
                kb.dma("pool", wbb[:], wbr_d[b, dc].rearrange("p (k c) -> p k c", k=8), writes=[wbb])
                psP = psr.next()
                kb.mm(psP, psP[:, 0:TG], [(wbb[:, j, :], ybr[b][j][:], [wbb, ybr[b][j]]) for j in range(8)])
                wg_ = load_wblock(32 + b * 16 + dc)
                psG = psr.next()
                kb.mm(psG, psG[:, 0:TG], hp(wg_, 0, 128))
                sg = tmpr.next()
                kb.op("act", lambda: A.activation(sg[:], psG[:, 0:TG], AF.Sigmoid), reads=[psG], writes=[sg])
                if b == 0:
                    kb.op("dve", lambda: V.tensor_tensor(acc[:], psP[:, 0:TG], sg[:], ALU.mult), reads=[psP, sg], writes=[acc])
                else:
                    kb.op("dve", lambda: V.tensor_tensor(sg[:], psP[:, 0:TG], sg[:], ALU.mult), reads=[psP, sg], writes=[sg])
                    if b < 3:
                        kb.op("dve", lambda: V.tensor_tensor(acc[:], acc[:], sg[:], ALU.add), reads=[acc, sg], writes=[acc])
                    else:
                        kb.op("dve", lambda: V.tensor_tensor(merged[dc][:], acc[:], sg[:], ALU.add), reads=[acc, sg], writes=[merged[dc]])
        if dbg and g == 0:
            for j in range(KD):
                kb.dma("act", o_dm[j * 128:(j + 1) * 128, :], merged[j][:], reads=[merged[j]])
        for dc in range(KD):
            wo = wbr.next()
            kb.dma("pool", wo[:], wout_d[dc].rearrange("p (k c) -> p k c", k=KD), writes=[wo])
            ps = psr.next()
            kb.mm(ps, ps[:, 0:TG], [(wo[:, k, :], merged[k][:], [wo, merged[k]]) for k in range(KD)])
            xt = xload(dc)
            kb.op("dve", lambda: V.scalar_tensor_tensor(xt[:], ps[:, 0:TG], modT[:, 32 + dc:33 + dc], xt[:], ALU.mult, ALU.add),
                  reads=[ps, modT, xt], writes=[xt])
            kb.dma("act", o_x[dc * 128:(dc + 1) * 128, tsl], xt[:], reads=[xt])
    kb.finish()
    return nc


def prep_C1(inp, layer, mod):
    w_in = inp["w_in"][layer]
    cols = np.concatenate([w_in[:, OFF["a_in"]:OFF["a_in"] + 1024], w_in[:, OFF["z"]:OFF["z"] + 1024],
                           w_in[:, OFF["uv"]:OFF["uv"] + 2048], w_in[:, OFF["gates"]:OFF["gates"] + 8192]], axis=1)
    wp = inp["w_pool"][layer]
    wpoolb = np.ascontiguousarray(wp.reshape(4, 2, 128, 256).transpose(0, 2, 1, 3).reshape(4, 128, 512))
    wbr = inp["w_branch"][layer]
    wbrb = np.ascontiguousarray(wbr.reshape(4, 8, 128, 16, 128).transpose(0, 3, 2, 1, 4).reshape(4, 16, 128, 1024))
    wout = inp["w_out"][layer]
    s = np.arange(128)[:, None]
    t = np.arange(128)[None, :]
    return dict(
        modT=np.ascontiguousarray(mod[:, 0:48]),
        winb=_blockify(np.ascontiguousarray(cols), NBC),
        wpoolb=wpoolb,
        pscale_pk=_pk(inp["pool_scale"][layer], 8),
        ssdnorm_pk=_pk(inp["ssd_norm"][layer], 8),
        lng_bc=np.ascontiguousarray(np.broadcast_to(inp["sgu_ln_gain"][layer][None, :], (128, 1024))).astype(np.float32),
        lnb_bc=np.ascontiguousarray(np.broadcast_to(inp["sgu_ln_bias"][layer][None, :], (128, 1024))).astype(np.float32),
        wsT=np.ascontiguousarray(inp["sgu_w_s"][layer].transpose(2, 0, 1)),
        Umask=(s <= t).astype(np.float32),
        bs_row=np.ascontiguousarray(inp["sgu_b_s"][layer].reshape(1, 1024)),
        wbrb=wbrb,
        woutb=_blockify(np.ascontiguousarray(wout), 16),
    )


def run_C1(inp, layer, xT, B_out, S, NTOK, mod, dbg=False):
    ncore = S // NTOK
    shared = prep_C1(inp, layer, mod)
    xTh = np.concatenate([np.zeros((D, 16), np.float32), xT], axis=1)
    maps = []
    for i in range(ncore):
        m = dict(shared)
        m["xTh"] = np.ascontiguousarray(xTh[:, i * NTOK:(i + 1) * NTOK + 16])
        m["yattT"] = np.ascontiguousarray(B_out["yattT"][:, i * NTOK:(i + 1) * NTOK])
        m["yssdT"] = np.ascontiguousarray(B_out["yssdT"][:, i * NTOK:(i + 1) * NTOK])
        m["haloflag"] = np.full((128, 1), 0.0 if i == 0 else 1.0, np.float32)
        pc = np.ones((128, 4, 16), np.float32)
        if i == 0:
            for wi, win in enumerate((2, 4, 8, 16)):
                tt = np.arange(16)
                pc[:, wi, :] = (win / np.minimum(tt + 1, win))[None, :]
        m["pcorr"] = pc
        maps.append(m)
    nc = build_C1(NTOK, dbg)
    res = run_bass_kernel_spmd(nc, maps, core_ids=list(range(ncore)))
    if dbg:
        return res.results
    return np.concatenate([r["xmidT"] for r in res.results], axis=1)


def build_C2(NTOK, NF, expert, SG=2):
    TG = 512
    if (NTOK // TG) % SG != 0:
        SG = 1
    NSG = NTOK // (TG * SG)
    NH = NF // 2
    nc = bass.Bass("TRN2", target_bir_lowering=False)
    xT = _din(nc, "xT", [D, NTOK])
    mod_d = _din(nc, "modT", [128, 48])
    w1_d = _din(nc, "w1b", [NF, 128, KD * 128])
    w3_d = _din(nc, "w3b", [NF, 128, KD * 128])
    w2_d = _din(nc, "w2b", [16, 128, NF * 128])
    if expert:
        wrow_d = _din(nc, "wrow", [1, NTOK])
    o_x = _dout(nc, "xoutT", [D, NTOK])

    kb = KB(nc)
    V = nc.vector
    A = nc.scalar
    G = nc.gpsimd
    psr = kb.ring(8, [128, 512], F32, "ps", psum=True)
    ones = kb.sb([128, 128], BF16, "ones")
    onesf = kb.sb([1, 128], F32, "onesf")
    epsb = kb.sb([128, 1], F32, "eps")
    kb.op("pool", lambda: G.memset(ones[:], 1.0), writes=[ones])
    kb.op("pool", lambda: G.memset(onesf[:], 1.0), writes=[onesf])
    kb.op("pool", lambda: G.memset(epsb[:], EPS), writes=[epsb])
    modT = _load_mod(kb, mod_d, 48)
    onepT = kb.sb([128, 16], F32, "onep")
    kb.op("dve", lambda: V.tensor_scalar_add(onepT[:], modT[:, 16:32], 1.0), reads=[modT], writes=[onepT])

    xkr = kb.ring(4, [128, TG], F32, "xk")
    hT = [[kb.sb([128, TG], BF16, "h%d_%d" % (s_, k)) for k in range(KD)] for s_ in range(SG)]
    rstd_x = kb.sb([128, TG], F32, "rstdx")
    sqr = kb.ring(2, [128, TG], BF16, "sq")
    tmpr = kb.ring(3, [128, TG], F32, "tmp")
    wbr = kb.ring(4 if expert else 8, [128, KD, 128], BF16, "wb")
    w2r = kb.ring(3 if expert else 4, [128, NH, 128], BF16, "w2")
    gt = [[kb.sb([128, TG], BF16, "g%d_%d" % (s_, f)) for f in range(NF)] for s_ in range(SG)]
    if expert:
        wrow = kb.sb([1, TG], F32, "wrow")
        wbc = [kb.sb([128, TG], F32, "wbc%d" % s_) for s_ in range(SG)]
    xTv = xT.rearrange("(k p) t -> p k t", p=128)

    for sg in range(NSG):
        tsls = [slice((sg * SG + s_) * TG, (sg * SG + s_ + 1) * TG) for s_ in range(SG)]

        def mk_xload(tsl):
            def xload(k):
                xt = xkr.next()
                kb.dma("sp", xt[:], xTv[:, k, tsl], writes=[xt])
                return xt
            return xload
        for s_ in range(SG):
            if expert:
                kb.dma("sp", wrow[:], wrow_d[:, tsls[s_]], writes=[wrow])
                ps = psr.next()
                kb.op("pe", lambda: nc.tensor.matmul(ps[:, 0:TG], onesf[0:1, :], wrow[0:1, :], start=True, stop=True),
                      reads=[onesf, wrow], writes=[ps])
                kb.op("act", lambda: A.copy(wbc[s_][:], ps[:, 0:TG]), reads=[ps], writes=[wbc[s_]])
            _emit_norm_mod2(kb, mk_xload(tsls[s_]), hT[s_], modT, onepT, ones, epsb, psr, sqr, tmpr, TG, rstd_x)
        for f in range(NF):
            w1 = wbr.next()
            kb.dma("pool", w1[:], w1_d[f].rearrange("p (k c) -> p k c", k=KD), writes=[w1])
            w3 = wbr.next()
            kb.dma("pool", w3[:], w3_d[f].rearrange("p (k c) -> p k c", k=KD), writes=[w3])
            for s_ in range(SG):
                p1 = psr.next()
                kb.mm(p1, p1[:, 0:TG], [(w1[:, k, :], hT[s_][k][:], [w1, hT[s_][k]]) for k in range(KD)])
                p3 = psr.next()
                kb.mm(p3, p3[:, 0:TG], [(w3[:, k, :], hT[s_][k][:], [w3, hT[s_][k]]) for k in range(KD)])
                s1 = tmpr.next()
                kb.op("act", lambda: A.activation(s1[:], p1[:, 0:TG], AF.Silu), reads=[p1], writes=[s1])
                if expert:
                    kb.op("dve", lambda: V.tensor_tensor(s1[:], s1[:], wbc[s_][:], ALU.mult), reads=[s1, wbc[s_]], writes=[s1])
                kb.op("dve", lambda: V.tensor_tensor(gt[s_][f][:], p3[:, 0:TG], s1[:], ALU.mult), reads=[p3, s1], writes=[gt[s_][f]])
        for dc in range(KD):
            w2h = []
            for hf in range(2):
                w2 = w2r.next()
                kb.dma("pool", w2[:], w2_d[dc][:, hf * NH * 128:(hf + 1) * NH * 128].rearrange("p (f c) -> p f c", f=NH), writes=[w2])
                w2h.append(w2)
            for s_ in range(SG):
                ps = psr.next()
                kb.mm(ps, ps[:, 0:TG], [(w2h[f // NH][:, f % NH, :], gt[s_][f][:], [w2h[f // NH], gt[s_][f]]) for f in range(NF)])
                xo = xkr.next()
                if expert:
                    kb.op("act", lambda: A.activation(xo[:], ps[:, 0:TG], AF.Identity, scale=modT[:, 32 + dc:33 + dc]),
                          reads=[ps, modT], writes=[xo])
                else:
                    kb.dma("sp", xo[:], xTv[:, dc, tsls[s_]], writes=[xo])
                    kb.op("dve", lambda: V.scalar_tensor_tensor(xo[:], ps[:, 0:TG], modT[:, 32 + dc:33 + dc], xo[:], ALU.mult, ALU.add),
                          reads=[ps, modT, xo], writes=[xo])
                kb.dma("act", o_x[dc * 128:(dc + 1) * 128, tsls[s_]], xo[:], reads=[xo])
    kb.finish()
    return nc


def _blockify_w2(w2, nf):
    w = w2.reshape(nf, 128, 16, 128)
    return np.ascontiguousarray(w.transpose(2, 1, 0, 3).reshape(16, 128, nf * 128))


def run_C2_dense(inp, layer, xT, S, NTOK, mod):
    ncore = S // NTOK
    idx = layer // 2
    nf = 5632 // 128
    shared = dict(modT=np.ascontiguousarray(mod[:, 48:96]),
                  w1b=_blockify(np.ascontiguousarray(inp["ffn_w1"][idx]), nf),
                  w3b=_blockify(np.ascontiguousarray(inp["ffn_w3"][idx]), nf),
                  w2b=_blockify_w2(inp["ffn_w2"][idx], nf))
    maps = []
    for i in range(ncore):
        m = dict(shared)
        m["xT"] = np.ascontiguousarray(xT[:, i * NTOK:(i + 1) * NTOK])
        maps.append(m)
    res = run_bass_kernel_spmd(build_C2(NTOK, nf, False), maps, core_ids=list(range(ncore)))
    return np.concatenate([r["xoutT"] for r in res.results], axis=1)


def build_R(NTOK):
    TG = 512
    NG = NTOK // TG
    nc = bass.Bass("TRN2", target_bir_lowering=False)
    xT = _din(nc, "xT", [D, NTOK])
    mod_d = _din(nc, "modT", [128, 48])
    wr_d = _din(nc, "wr_pk", [128, KD, 8])
    o_w = _dout(nc, "wt", [NTOK, 8])
    kb = KB(nc)
    V = nc.vector
    A = nc.scalar
    G = nc.gpsimd
    psr = kb.ring(4, [128, 512], F32, "ps", psum=True)
    ones = kb.sb([128, 128], BF16, "ones")
    epsb = kb.sb([128, 1], F32, "eps")
    kb.op("pool", lambda: G.memset(ones[:], 1.0), writes=[ones])
    kb.op("pool", lambda: G.memset(epsb[:], EPS), writes=[epsb])
    modT = _load_mod(kb, mod_d, 48)
    onepT = kb.sb([128, 16], F32, "onep")
    kb.op("dve", lambda: V.tensor_scalar_add(onepT[:], modT[:, 16:32], 1.0), reads=[modT], writes=[onepT])
    wr = kb.sb([128, KD, 8], F32, "wr")
    kb.dma("sp", wr[:], wr_d, writes=[wr])
    xk = [kb.sb([128, TG], F32, "x%d" % k) for k in range(KD)]
    h32 = [kb.sb([128, TG], F32, "h%d" % k) for k in range(KD)]
    sqr = kb.ring(3, [128, TG], BF16, "sq")
    rstd = kb.sb([128, TG], F32, "rstd")
    lg = kb.sb([128, 4, 8], F32, "lg")
    l2 = kb.sb([128, 4, 8], F32, "l2")
    mk1 = kb.sb([128, 4, 8], F32, "mk1")
    mk2 = kb.sb([128, 4, 8], F32, "mk2")
    wt = kb.sb([128, 4, 8], F32, "wt")
    mm_ = kb.sb([128, 4, 4], F32, "mm_")
    xTv = xT.rearrange("(k p) t -> p k t", p=128)
    for g in range(NG):
        tsl = slice(g * TG, (g + 1) * TG)
        for k in range(KD):
            kb.dma("sp", xk[k][:], xTv[:, k, tsl], writes=[xk[k]])
        ps = psr.next()
        for k in range(KD):
            sq = sqr.next()
            kb.op("act", lambda: A.activation(sq[:], xk[k][:], AF.Square), reads=[xk[k]], writes=[sq])
            kb.op("pe", lambda: nc.tensor.matmul(ps[:, 0:TG], ones[:], sq[:], start=(k == 0), stop=(k == KD - 1)),
                  reads=[ones, sq], writes=[ps])
        _rstd(kb, ps, rstd, D, epsb)
        for k in range(KD):
            kb.op("dve", lambda: V.tensor_tensor(h32[k][:], xk[k][:], rstd[:], ALU.mult), reads=[xk[k], rstd], writes=[h32[k]])
            kb.op("act", lambda: A.activation(h32[k][:], h32[k][:], AF.Identity, bias=modT[:, k:k + 1], scale=onepT[:, k:k + 1]),
                  reads=[h32[k], modT, onepT], writes=[h32[k]])
        ps = psr.next()
        for tt in range(4):
            kb.mm(ps, ps[:, tt * 8:(tt + 1) * 8],
                  [(h32[k][:, tt * 128:(tt + 1) * 128], wr[:, k, :], [h32[k], wr]) for k in range(KD)])
        kb.op("dve", lambda: V.tensor_copy(lg[:].rearrange("p a b -> p (a b)"), ps[:, 0:32]), reads=[ps], writes=[lg])
        for tt in range(4):
            kb.op("dve", lambda: V.reduce_max(mm_[:, tt, 0:1], lg[:, tt, :], axis=AX.X), reads=[lg], writes=[mm_])
            kb.op("dve", lambda: V.tensor_scalar(mk1[:, tt, :], lg[:, tt, :], mm_[:, tt, 0:1], None, ALU.is_equal),
                  reads=[lg, mm_], writes=[mk1])
            kb.op("dve", lambda: V.scalar_tensor_tensor(l2[:, tt, :], mk1[:, tt, :], -1e30, lg[:, tt, :], ALU.mult, ALU.add),
                  reads=[mk1, lg], writes=[l2])
            kb.op("dve", lambda: V.reduce_max(mm_[:, tt, 1:2], l2[:, tt, :], axis=AX.X), reads=[l2], writes=[mm_])
            kb.op("dve", lambda: V.tensor_scalar(mk2[:, tt, :], l2[:, tt, :], mm_[:, tt, 1:2], None, ALU.is_equal),
                  reads=[l2, mm_], writes=[mk2])
            kb.op("dve", lambda: V.tensor_tensor(mm_[:, tt, 2:3], mm_[:, tt, 1:2], mm_[:, tt, 0:1], ALU.subtract), reads=[mm_], writes=[mm_])
            kb.op("act", lambda: A.activation(mm_[:, tt, 2:3], mm_[:, tt, 2:3], AF.Exp), reads=[mm_], writes=[mm_])
            kb.op("dve", lambda: V.tensor_scalar_add(mm_[:, tt, 3:4], mm_[:, tt, 2:3], 1.0), reads=[mm_], writes=[mm_])
            kb.op("dve", lambda: V.reciprocal(mm_[:, tt, 3:4], mm_[:, tt, 3:4]), reads=[mm_], writes=[mm_])
            kb.op("dve", lambda: V.tensor_tensor(mm_[:, tt, 2:3], mm_[:, tt, 2:3], mm_[:, tt, 3:4], ALU.mult), reads=[mm_], writes=[mm_])
            kb.op("dve", lambda: V.tensor_scalar_mul(wt[:, tt, :], mk1[:, tt, :], mm_[:, tt, 3:4]), reads=[mk1, mm_], writes=[wt])
            kb.op("dve", lambda: V.scalar_tensor_tensor(wt[:, tt, :], mk2[:, tt, :], mm_[:, tt, 2:3], wt[:, tt, :], ALU.mult, ALU.add),
                  reads=[mk2, mm_, wt], writes=[wt])
        kb.dma("act", o_w[tsl, :].rearrange("(t p) e -> p t e", p=128), wt[:], reads=[wt])
    kb.finish()
    return nc


def run_R(inp, layer, xT, S, NTOK, mod):
    ncore = S // NTOK
    idx = layer // 2
    shared = dict(modT=np.ascontiguousarray(mod[:, 48:96]),
                  wr_pk=np.ascontiguousarray(inp["moe_router"][idx].reshape(KD, 128, 8).transpose(1, 0, 2)))
    maps = []
    for i in range(ncore):
        m = dict(shared)
        m["xT"] = np.ascontiguousarray(xT[:, i * NTOK:(i + 1) * NTOK])
        maps.append(m)
    res = run_bass_kernel_spmd(build_R(NTOK), maps, core_ids=list(range(ncore)))
    return np.concatenate([r["wt"] for r in res.results], axis=0)


def build_S(NTOK):
    TG = 512
    nc = bass.Bass("TRN2", target_bir_lowering=False)
    xT = _din(nc, "xT", [D, NTOK])
    y0 = _din(nc, "y0T", [D, NTOK])
    y1 = _din(nc, "y1T", [D, NTOK])
    o = _dout(nc, "xoutT", [D, NTOK])
    kb = KB(nc)
    V = nc.vector
    ar = kb.ring(3, [128, TG], F32, "a")
    br = kb.ring(3, [128, TG], F32, "b")
    cr = kb.ring(3, [128, TG], F32, "c")
    for k in range(KD):
        for g in range(NTOK // TG):
            rs = slice(k * 128, (k + 1) * 128)
            tsl = slice(g * TG, (g + 1) * TG)
            a, b, c = ar.next(), br.next(), cr.next()
            kb.dma("sp", a[:], xT[rs, tsl], writes=[a])
            kb.dma("sp", b[:], y0[rs, tsl], writes=[b])
            kb.dma("sp", c[:], y1[rs, tsl], writes=[c])
            kb.op("dve", lambda: V.tensor_tensor(b[:], b[:], c[:], ALU.add), reads=[b, c], writes=[b])
            kb.op("dve", lambda: V.tensor_tensor(a[:], a[:], b[:], ALU.add), reads=[a, b], writes=[a])
            kb.dma("act", o[rs, tsl], a[:], reads=[a])
    kb.finish()
    return nc


def run_moe(inp, layer, xT, S, NTOK, mod):
    idx = layer // 2
    nf = 7168 // 128
    wt = run_R(inp, layer, xT, S, NTOK, mod)
    sel = wt > 0
    lists = [np.nonzero(sel[:, e])[0] for e in range(8)]
    cap = max(1024, int(-(-max(len(l) for l in lists) // 1024) * 1024))
    maps = []
    for e in range(8):
        tl = lists[e]
        xg = np.zeros((D, cap), np.float32)
        xg[:, :len(tl)] = xT[:, tl]
        wrow = np.zeros((1, cap), np.float32)
        wrow[0, :len(tl)] = wt[tl, e]
        maps.append(dict(xT=xg, wrow=wrow, modT=np.ascontiguousarray(mod[:, 48:96]),
                         w1b=_blockify(np.ascontiguousarray(inp["moe_w1"][idx, e]), nf),
                         w3b=_blockify(np.ascontiguousarray(inp["moe_w3"][idx, e]), nf),
                         w2b=_blockify_w2(inp["moe_w2"][idx, e], nf)))
    res = run_bass_kernel_spmd(build_C2(cap, nf, True), maps, core_ids=list(range(8)))
    y = [np.zeros((D, S), np.float32), np.zeros((D, S), np.float32)]
    nsel = np.zeros(S, np.int64)
    for e in range(8):
        tl = lists[e]
        ye = res.results[e]["xoutT"][:, :len(tl)]
        slot = nsel[tl]
        for sidx in (0, 1):
            m = slot == sidx
            y[sidx][:, tl[m]] = ye[:, m]
        nsel[tl] += 1
    ncore = S // NTOK
    maps = []
    for i in range(ncore):
        sl = slice(i * NTOK, (i + 1) * NTOK)
        maps.append(dict(xT=np.ascontiguousarray(xT[:, sl]), y0T=np.ascontiguousarray(y[0][:, sl]), y1T=np.ascontiguousarray(y[1][:, sl])))
    res = run_bass_kernel_spmd(build_S(NTOK), maps, core_ids=list(range(ncore)))
    return np.concatenate([r["xoutT"] for r in res.results], axis=1)


def kernel(**inp):
    inp = {k: np.asarray(v) for k, v in inp.items()}
    S = inp["x"].shape[1]
    NTOK = S // 8
    xT = np.ascontiguousarray(inp["x"][0].T)
    mods = run_M(inp)
    for layer in range(2):
        A_out = run_A(inp, layer, xT, S, NTOK, mods[layer])
        B_out = run_B(inp, layer, A_out, S)
        del A_out
        xmid = run_C1(inp, layer, xT, B_out, S, NTOK, mods[layer])
        del B_out
        if layer % 2 == 0:
            xT = run_C2_dense(inp, layer, xmid, S, NTOK, mods[layer])
        else:
            xT = run_moe(inp, layer, xmid, S, NTOK, mods[layer])
    return np.ascontiguousarray(xT.T)[None].astype(np.float32)
```

```python
import math
import numpy as np
import ml_dtypes
import concourse.bass as bass
import concourse.mybir as mybir
from concourse.bass_utils import run_bass_kernel_spmd

F32 = mybir.dt.float32
BF16 = mybir.dt.bfloat16
I32 = mybir.dt.int32
AF = mybir.ActivationFunctionType
ALU = mybir.AluOpType
AX = mybir.AxisListType

D = 2048
KD = 16
EPS = 1e-6
OFF = dict(a_in=0, z=1024, xbc=2048, dt=3584, cq=3600, ckv=4368, kr=4880, uv=4944, gates=6992)


class Tl:
    __slots__ = ("t", "name", "last_w", "readers", "root")

    def __init__(self, t, name):
        self.t = t
        self.name = name
        self.last_w = None
        self.readers = {}
        self.root = self

    def __getitem__(self, idx):
        return self.t[idx]


class View:
    __slots__ = ("t", "name", "root")

    def __init__(self, parent, ap, name):
        self.t = ap
        self.name = name
        self.root = parent.root

    def __getitem__(self, idx):
        return self.t[idx]


class Ring:
    def __init__(self, tiles):
        self.tiles = tiles
        self.i = 0

    def next(self):
        t = self.tiles[self.i]
        self.i = (self.i + 1) % len(self.tiles)
        return t


class KB:
    NDMA = 8

    def __init__(self, nc):
        self.nc = nc
        self.E = {"pe": nc.tensor, "act": nc.scalar, "dve": nc.vector,
                  "pool": nc.gpsimd, "sp": nc.sync}
        self.sems = {}
        self.cnt = {}
        for k in self.E:
            self.sems[k] = nc.alloc_semaphore("c_" + k)
            self.cnt[k] = 0
        self.dma_rr = {}
        for q in ("sp", "pool", "act"):
            self.dma_rr[q] = 0
            for i in range(self.NDMA):
                key = ("d", q, i)
                self.sems[key] = nc.alloc_semaphore("d_%s_%d" % (q, i))
                self.cnt[key] = 0
        self.seen = {k: {} for k in self.E}
        self.ntile = 0
        self.pending_out = []

    def sb(self, shape, dt, name="t"):
        self.ntile += 1
        return Tl(self.nc.alloc_sbuf_tensor("%s_%d" % (name, self.ntile), list(shape), dt), name)

    def ps(self, shape, dt=F32, name="p"):
        self.ntile += 1
        return Tl(self.nc.alloc_psum_tensor("%s_%d" % (name, self.ntile), list(shape), dt), name)

    def ring(self, n, shape, dt, name="r", psum=False):
        f = self.ps if psum else self.sb
        return Ring([f(shape, dt, name) for _ in range(n)])

    def _wait(self, e, deps):
        need = {}
        for d in deps:
            if d is None:
                continue
            k, v = d
            if k == e and e == "pe":
                continue
            if need.get(k, 0) < v:
                need[k] = v
        seen = self.seen[e]
        for k, v in need.items():
            if seen.get(k, 0) >= v:
                continue
            self.E[e].wait_ge(self.sems[k], v)
            seen[k] = v

    def _deps(self, reads, writes):
        deps = []
        for r in reads:
            deps.append(r.root.last_w)
        for w in writes:
            deps.append(w.root.last_w)
            deps.extend(w.root.readers.items())
        return deps

    def _commit(self, tok, reads, writes):
        writes = [w.root for w in writes]
        reads = [r.root for r in reads]
        for w in writes:
            w.last_w = tok
            w.readers = {}
        k, v = tok
        for r in reads:
            if r in writes:
                continue
            if r.readers.get(k, 0) < v:
                r.readers[k] = v

    def op(self, e, fn, reads=(), writes=(), inc=True):
        self._wait(e, self._deps(reads, writes))
        ins = fn()
        if inc:
            self.cnt[e] += 1
            ins.then_inc(self.sems[e], 1)
            tok = (e, self.cnt[e])
        else:
            tok = (e, self.cnt[e] + 1)
        self._commit(tok, reads, writes)
        return ins

    def dma(self, q, out, in_, reads=(), writes=(), **kw):
        i = self.dma_rr[q]
        self.dma_rr[q] = (i + 1) % self.NDMA
        key = ("d", q, i)
        deps = self._deps(reads, writes)
        deps.append((key, self.cnt[key]))
        self._wait(q, deps)
        ins = self.E[q].dma_start(out=out, in_=in_, **kw)
        self.cnt[key] += 16
        ins.then_inc(self.sems[key], 16)
        tok = (key, self.cnt[key])
        self._commit(tok, reads, writes)
        if not writes:
            self.pending_out.append(tok)
        return ins

    def finish(self):
        last = {}
        for k, v in self.pending_out:
            last[k] = max(last.get(k, 0), v)
        self._wait("sp", list(last.items()))
        self._wait("sp", [(k, self.cnt[k]) for k in ("pe", "act", "dve", "pool") if self.cnt[k]])

    def mm(self, ps, out_ap, pairs):
        nc = self.nc
        n = len(pairs)
        for i, (l, r, rd) in enumerate(pairs):
            self.op("pe", lambda: nc.tensor.matmul(out_ap, l, r, start=(i == 0), stop=(i == n - 1)),
                    reads=rd, writes=[ps], inc=(i == n - 1))


def _din(nc, name, shape, dt=F32):
    return nc.dram_tensor(name, list(shape), dt, kind="ExternalInput").ap()


def _dout(nc, name, shape, dt=F32):
    return nc.dram_tensor(name, list(shape), dt, kind="ExternalOutput").ap()


def _rstd(kb, ps_ss, out, n, epsb):
    nc = kb.nc
    kb.op("act", lambda: nc.scalar.activation(out[:], ps_ss[:], AF.Sqrt, bias=epsb[:, 0:1], scale=1.0 / n),
          reads=[ps_ss, epsb], writes=[out])
    kb.op("dve", lambda: nc.vector.reciprocal(out[:], out[:]), reads=[out], writes=[out])


def _emit_mod(kb, c_pk, wada, bada, ncol, psr):
    nc = kb.nc
    nj = ncol // 128
    cs = kb.sb([128, 16], F32, "cs")
    cact = kb.sb([128, 16], F32, "cact")
    bsb = kb.sb([128, nj], F32, "bada")
    modT = kb.sb([128, nj], F32, "modT")
    kb.dma("sp", cs[:], c_pk, writes=[cs])
    kb.dma("sp", bsb[:], bada, writes=[bsb])
    kb.op("act", lambda: nc.scalar.activation(cact[:], cs[:], AF.Silu), reads=[cs], writes=[cact])
    war = kb.ring(2, [128, 16, 128], F32, "wada")
    ps = psr.next()
    for j in range(nj):
        wa = war.next()
        kb.dma("sp", wa[:], wada[j].rearrange("p (k c) -> p k c", k=16), writes=[wa])
        kb.mm(ps, ps[:, j:j + 1], [(wa[:, k, :], cact[:, k:k + 1], [wa, cact]) for k in range(16)])
    kb.op("dve", lambda: nc.vector.tensor_tensor(modT[:], ps[:, 0:nj], bsb[:], ALU.add),
          reads=[ps, bsb], writes=[modT])
    return modT


def _load_mod(kb, mod_d, nj):
    t = kb.sb([128, nj], F32, "modT")
    kb.dma("sp", t[:], mod_d, writes=[t])
    return t


def build_M():
    nc = bass.Bass("TRN2", target_bir_lowering=False)
    c_pk = _din(nc, "c_pk", [128, 16])
    wada = _din(nc, "wada", [24, 128, KD * 128])
    bada = _din(nc, "bada_pk", [128, 24])
    o = _dout(nc, "modT", [128, 24])
    kb = KB(nc)
    psr = kb.ring(2, [128, 512], F32, "ps", psum=True)
    modT = _emit_mod(kb, c_pk, wada, bada, 24 * 128, psr)
    kb.dma("sp", o, modT[:], reads=[modT])
    kb.finish()
    return nc


def run_M(inp):
    maps = []
    for i in range(8):
        w = np.concatenate([inp["w_ada"][l][:, i * 1536:(i + 1) * 1536] for l in range(2)], axis=1)
        b = np.concatenate([inp["b_ada"][l][i * 1536:(i + 1) * 1536] for l in range(2)])
        maps.append(dict(c_pk=_pk(inp["c"][0], 16), wada=_blockify(np.ascontiguousarray(w), 24), bada_pk=_pk(b, 24)))
    res = run_bass_kernel_spmd(build_M(), maps, core_ids=list(range(8)))
    out = []
    for l in range(2):
        out.append(np.ascontiguousarray(np.concatenate([r["modT"][:, l * 12:(l + 1) * 12] for r in res.results], axis=1)))
    return out


def _emit_norm_mod(kb, xk, hT, shiftT, onepT, ones, epsb, psr, sqr, tmpr, TG, rstd_t):
    nc = kb.nc
    ps = psr.next()
    for k in range(KD):
        sq = sqr.next()
        kb.op("act", lambda: nc.scalar.activation(sq[:], xk[k][:], AF.Square), reads=[xk[k]], writes=[sq])
        kb.op("pe", lambda: nc.tensor.matmul(ps[:, 0:TG], ones[:], sq[:], start=(k == 0), stop=(k == KD - 1)),
              reads=[ones, sq], writes=[ps], inc=True)
    rstd = rstd_t
    _rstd(kb, ps, rstd, D, epsb)
    for k in range(KD):
        tmp = tmpr.next()
        kb.op("dve", lambda: nc.vector.tensor_tensor(tmp[:], xk[k][:], rstd[:], ALU.mult),
              reads=[xk[k], rstd], writes=[tmp])
        kb.op("act", lambda: nc.scalar.activation(hT[k][:], tmp[:], AF.Identity,
                                                  bias=shiftT[:, k:k + 1], scale=onepT[:, k:k + 1]),
              reads=[tmp, shiftT, onepT], writes=[hT[k]])


PI_LO = 3.1415925
C1_2PI = 6.28125
C2_2PI = 2.0 * math.pi - 6.28125


def _emit_rope_tables(kb, posi, ang, angk, angi, sin2, cos2, rc_s):
    nc = kb.nc
    V = nc.vector
    kb.op("dve", lambda: V.tensor_copy(ang[:], posi[:]), reads=[posi], writes=[ang])
    kb.op("dve", lambda: V.tensor_scalar_mul(ang[:], ang[:], rc_s[:, 0:1]), reads=[ang, rc_s], writes=[ang])
    kb.op("dve", lambda: V.tensor_scalar_mul(angk[:], ang[:], 1.0 / (2.0 * math.pi)), reads=[ang], writes=[angk])
    kb.op("dve", lambda: V.tensor_copy(angi[:], angk[:]), reads=[angk], writes=[angi])
    kb.op("dve", lambda: V.tensor_copy(angk[:], angi[:]), reads=[angi], writes=[angk])
    kb.op("dve", lambda: V.scalar_tensor_tensor(ang[:], angk[:], -C1_2PI, ang[:], ALU.mult, ALU.add),
          reads=[angk, ang], writes=[ang])
    kb.op("dve", lambda: V.scalar_tensor_tensor(ang[:], angk[:], -C2_2PI, ang[:], ALU.mult, ALU.add),
          reads=[angk, ang], writes=[ang])

    def wrap(t):
        kb.op("dve", lambda: V.tensor_scalar(angk[:], t[:], math.pi, -2.0 * math.pi, ALU.is_gt, ALU.mult),
              reads=[t], writes=[angk])
        kb.op("dve", lambda: V.tensor_tensor(t[:], t[:], angk[:], ALU.add), reads=[t, angk], writes=[t])
        kb.op("dve", lambda: V.tensor_scalar(angk[:], t[:], -math.pi, 2.0 * math.pi, ALU.is_lt, ALU.mult),
              reads=[t], writes=[angk])
        kb.op("dve", lambda: V.tensor_tensor(t[:], t[:], angk[:], ALU.add), reads=[t, angk], writes=[t])
        kb.op("dve", lambda: V.tensor_scalar(t[:], t[:], PI_LO, -PI_LO, ALU.min, ALU.max), reads=[t], writes=[t])
    wrap(ang)
    kb.op("dve", lambda: V.tensor_scalar_add(cos2[:], ang[:], 0.5 * math.pi), reads=[ang], writes=[cos2])
    wrap(cos2)
    kb.op("act", lambda: nc.scalar.activation(sin2[:], ang[:], AF.Sin, scale=rc_s[:, 1:2]),
          reads=[ang, rc_s], writes=[sin2])
    kb.op("act", lambda: nc.scalar.activation(cos2[:], cos2[:], AF.Sin), reads=[cos2], writes=[cos2])

def _emit_norm_mod2(kb, xload, hT, shiftT, onepT, ones, epsb, psr, sqr, tmpr, TG, rstd_t, after=None):
    nc = kb.nc
    ps = psr.next()
    for k in range(KD):
        xt = xload(k)
        sq = sqr.next()
        kb.op("act", lambda: nc.scalar.activation(sq[:], xt[:], AF.Square), reads=[xt], writes=[sq])
        kb.op("pe", lambda: nc.tensor.matmul(ps[:, 0:TG], ones[:], sq[:], start=(k == 0), stop=(k == KD - 1)),
              reads=[ones, sq], writes=[ps], inc=True)
    _rstd(kb, ps, rstd_t, D, epsb)
    for k in range(KD):
        xt = xload(k)
        tmp = tmpr.next()
        kb.op("dve", lambda: nc.vector.tensor_tensor(tmp[:], xt[:], rstd_t[:], ALU.mult),
              reads=[xt, rstd_t], writes=[tmp])
        kb.op("act", lambda: nc.scalar.activation(hT[k][:], tmp[:], AF.Identity,
                                                  bias=shiftT[:, k:k + 1], scale=onepT[:, k:k + 1]),
              reads=[tmp, shiftT, onepT], writes=[hT[k]])
        if after is not None:
            after(k, tmp)


NBA = 24


def build_A(NTOK, dbg=False):
    TG = 512
    NG = NTOK // TG
    nc = bass.Bass("TRN2", target_bir_lowering=False)
    xT = _din(nc, "xT", [D, NTOK])
    mod_d = _din(nc, "modT", [128, 32])
    winb = _din(nc, "winb", [NBA, 128, KD * 128])
    dtb = _din(nc, "dtb_bc", [128, 16])
    qn = _din(nc, "qnorm_pk", [128, 6])
    kvn = _din(nc, "kvnorm_pk", [128, 4])
    wuq = _din(nc, "wuq", [768, 2048])
    wukk = _din(nc, "wukv_k", [512, 1024])
    wukv = _din(nc, "wukv_v", [512, 1024])
    qg = _din(nc, "qgain", [128, 3])
    kg = _din(nc, "kgain", [128, 3])
    pos = _din(nc, "pos_bc", [64, NTOK], I32)
    rc = _din(nc, "ropec", [64, 4])
    o_xbc = _dout(nc, "xbcT", [1536, NTOK])
    o_dt = _dout(nc, "dt", [NTOK, 16])
    o_q = _dout(nc, "qT", [8, 192, NTOK], BF16)
    o_k = _dout(nc, "kT", [8, 192, NTOK], BF16)
    o_v = _dout(nc, "v", [NTOK, 1024], BF16)
    if dbg:
        o_dh = _dout(nc, "dbg_h", [D, TG], BF16)
        o_dm = _dout(nc, "dbg_mod", [128, 32])

    kb = KB(nc)
    psr = kb.ring(8, [128, 512], F32, "ps", psum=True)
    ones = kb.sb([128, 128], BF16, "ones")
    epsb = kb.sb([128, 1], F32, "eps")
    kb.op("pool", lambda: nc.gpsimd.memset(ones[:], 1.0), writes=[ones])
    kb.op("pool", lambda: nc.gpsimd.memset(epsb[:], EPS), writes=[epsb])

    modT = _load_mod(kb, mod_d, 32)
    onepT = kb.sb([128, 16], F32, "onep")
    kb.op("dve", lambda: nc.vector.tensor_scalar_add(onepT[:], modT[:, 16:32], 1.0), reads=[modT], writes=[onepT])

    def load_small(ap, shape, dt=F32, name="c"):
        t = kb.sb(shape, dt, name)
        kb.dma("sp", t[:], ap, writes=[t])
        return t
    dtb_s = load_small(dtb, [128, 16])
    qn_s = load_small(qn, [128, 6])
    kvn_s = load_small(kvn, [128, 4])
    qg_s = load_small(qg, [128, 3])
    kg_s = load_small(kg, [128, 3])
    rc_s = load_small(rc, [64, 4])
    wuq_s = kb.sb([128, 6, 2048], BF16, "wuq")
    wukk_s = kb.sb([128, 4, 1024], BF16, "wukk")
    wukv_s = kb.sb([128, 4, 1024], BF16, "wukv")
    wuq_v = wuq.rearrange("(k p) c -> p k c", p=128)
    for j in range(6):
        kb.dma("pool", wuq_s[:, j, :], wuq_v[:, j, :], writes=[wuq_s])
    kb.dma("pool", wukk_s[:], wukk.rearrange("(k p) c -> p k c", p=128), writes=[wukk_s])
    kb.dma("pool", wukv_s[:], wukv.rearrange("(k p) c -> p k c", p=128), writes=[wukv_s])

    xk = [kb.sb([128, TG], F32, "x%d" % k) for k in range(KD)]
    hT = [kb.sb([128, TG], BF16, "h%d" % k) for k in range(KD)]
    sqr = kb.ring(3, [128, TG], BF16, "sq")
    tmpr = kb.ring(6, [128, TG], F32, "tmp")
    rstd_x = kb.sb([128, TG], F32, "rstdx")
    wbr = kb.ring(7, [128, KD, 128], BF16, "wb")
    stg = kb.ring(3, [128, TG], F32, "stg")
    stgb = kb.ring(4, [128, TG], BF16, "stgb")
    cq_s = [kb.sb([128, TG], F32, "cq%d" % j) for j in range(6)]
    cqn = [kb.sb([128, TG], BF16, "cqn%d" % j) for j in range(6)]
    ckv_s = [kb.sb([128, TG], F32, "ckv%d" % j) for j in range(4)]
    ckvn = [kb.sb([128, TG], BF16, "ckvn%d" % j) for j in range(4)]
    kr_s = kb.sb([64, TG], F32, "kr")
    krs_s = kb.sb([64, TG], F32, "krs")
    krsq = kb.sb([64, TG], BF16, "krsq")
    posi = kb.sb([64, TG], I32, "posi")
    ang = kb.sb([64, TG], F32, "ang")
    angk = kb.sb([64, TG], F32, "angk")
    angi = kb.sb([64, TG], I32, "angi")
    cos2 = kb.sb([64, TG], F32, "cos2")
    sin2 = kb.sb([64, TG], F32, "sin2")
    dts = kb.sb([128, 4, 16], F32, "dts")
    xTv = xT.rearrange("(k p) t -> p k t", p=128)

    def load_wblock(b):
        wb = wbr.next()
        kb.dma("pool", wb[:], winb[b].rearrange("p (k c) -> p k c", k=KD), writes=[wb])
        return wb

    for g in range(NG):
        t0 = g * TG
        tsl = slice(t0, t0 + TG)
        for k in range(KD):
            kb.dma("sp", xk[k][:], xTv[:, k, tsl], writes=[xk[k]])
        kb.dma("sp", posi[:], pos[:, tsl], writes=[posi])
        _emit_norm_mod(kb, xk, hT, modT, onepT, ones, epsb, psr, sqr, tmpr, TG, rstd_x)
        _emit_rope_tables(kb, posi, ang, angk, angi, sin2, cos2, rc_s)
        if dbg and g == 0:
            for k in range(KD):
                kb.dma("act", o_dh[k * 128:(k + 1) * 128, :], hT[k][:], reads=[hT[k]])
            kb.dma("act", o_dm, modT[:], reads=[modT])

        def hp(wb, c0, c1):
            return [(wb[:, k, c0:c1], hT[k][:], [wb, hT[k]]) for k in range(KD)]

        for b in range(12):
            wb = load_wblock(b)
            ps = psr.next()
            kb.mm(ps, ps[:, 0:TG], hp(wb, 0, 128))
            st = stg.next()
            if b % 2 == 0:
                kb.op("act", lambda: nc.scalar.copy(st[:], ps[:, 0:TG]), reads=[ps], writes=[st])
            else:
                kb.op("dve", lambda: nc.vector.tensor_copy(st[:], ps[:, 0:TG]), reads=[ps], writes=[st])
            kb.dma("act", o_xbc[b * 128:(b + 1) * 128, tsl], st[:], reads=[st])

        def lat(b0, nb, dst, dstn, gains, nfeat):
            pss = psr.next()
            for j in range(nb):
                wb = load_wblock(b0 + j)
                ps = psr.next()
                kb.mm(ps, ps[:, 0:TG], hp(wb, 0, 128))
                kb.op("act", lambda: nc.scalar.copy(dst[j][:], ps[:, 0:TG]), reads=[ps], writes=[dst[j]])
                sq = sqr.next()
                kb.op("act", lambda: nc.scalar.activation(sq[:], ps[:, 0:TG], AF.Square), reads=[ps], writes=[sq])
                kb.op("pe", lambda: nc.tensor.matmul(pss[:, 0:TG], ones[:], sq[:], start=(j == 0), stop=(j == nb - 1)),
                      reads=[ones, sq], writes=[pss])
            r = tmpr.next()
            _rstd(kb, pss, r, nfeat, epsb)
            for j in range(nb):
                kb.op("dve", lambda: nc.vector.scalar_tensor_tensor(dstn[j][:], dst[j][:], gains[:, j:j + 1], r[:],
                                                                    ALU.mult, ALU.mult),
                      reads=[dst[j], gains, r], writes=[dstn[j]])
        lat(12, 6, cq_s, cqn, qn_s, 768)
        lat(18, 4, ckv_s, ckvn, kvn_s, 512)

        wb = load_wblock(22)
        ps = psr.next()
        kb.mm(ps, ps[0:64, 0:TG], hp(wb, 0, 64))
        kb.op("act", lambda: nc.scalar.copy(kr_s[:], ps[0:64, 0:TG]), reads=[ps], writes=[kr_s])
        kb.op("act", lambda: nc.scalar.activation(krsq[:], ps[0:64, 0:TG], AF.Square), reads=[ps], writes=[krsq])
        ps = psr.next()
        kb.mm(ps, ps[0:64, 0:TG], hp(wb, 64, 128))
        kb.op("act", lambda: nc.scalar.copy(krs_s[:], ps[0:64, 0:TG]), reads=[ps], writes=[krs_s])

        wb = load_wblock(23)
        ps = psr.next()
        for tt in range(TG // 128):
            kb.mm(ps, ps[:, tt * 16:(tt + 1) * 16],
                  [(hT[k][:, tt * 128:(tt + 1) * 128], wb[:, k, 0:16], [wb, hT[k]]) for k in range(KD)])
        for tt in range(TG // 128):
            kb.op("dve", lambda: nc.vector.tensor_tensor(dts[:, tt, :], ps[:, tt * 16:(tt + 1) * 16], dtb_s[:], ALU.add),
                  reads=[ps, dtb_s], writes=[dts])
        kb.op("act", lambda: nc.scalar.activation(dts[:], dts[:], AF.Exp), reads=[dts], writes=[dts])
        kb.op("act", lambda: nc.scalar.activation(dts[:], dts[:], AF.Ln, bias=1.0, scale=1.0), reads=[dts], writes=[dts])
        kb.dma("act", o_dt[tsl, :].rearrange("(t p) h -> p t h", p=128), dts[:], reads=[dts])

        def head(src_n_pairs, rope_src, gains, dst, h):
            psn = psr.next()
            kb.mm(psn, psn[:, 0:TG], src_n_pairs)
            sqn = sqr.next()
            kb.op("act", lambda: nc.scalar.activation(sqn[:], psn[:, 0:TG], AF.Square), reads=[psn], writes=[sqn])
            if rope_src is None:
                psr_ = psr.next()
                kb.mm(psr_, psr_[0:64, 0:TG], [(wuq_s[:, j, h * 256 + 128:h * 256 + 192], cqn[j][:], [wuq_s, cqn[j]]) for j in range(6)])
                pss_ = psr.next()
                kb.mm(pss_, pss_[0:64, 0:TG], [(wuq_s[:, j, h * 256 + 192:h * 256 + 256], cqn[j][:], [wuq_s, cqn[j]]) for j in range(6)])
                sqr_t = sqr.next()
                kb.op("act", lambda: nc.scalar.activation(sqr_t[0:64, :], psr_[0:64, 0:TG], AF.Square), reads=[psr_], writes=[sqr_t])
                r_ap, s_ap, r_t, s_t = psr_[0:64, 0:TG], pss_[0:64, 0:TG], psr_, pss_
            else:
                sqr_t = krsq
                r_ap, s_ap, r_t, s_t = kr_s[:], krs_s[:], kr_s, krs_s
            pss = psr.next()
            kb.op("pe", lambda: nc.tensor.matmul(pss[:, 0:TG], ones[:], sqn[:], start=True, stop=False),
                  reads=[ones, sqn], writes=[pss], inc=False)
            kb.op("pe", lambda: nc.tensor.matmul(pss[:, 0:TG], ones[0:64, :], sqr_t[0:64, :], start=False, stop=True),
                  reads=[ones, sqr_t], writes=[pss])
            r = tmpr.next()
            _rstd(kb, pss, r, 192, epsb)
            on = stgb.next()
            kb.op("dve", lambda: nc.vector.scalar_tensor_tensor(on[:], psn[:, 0:TG], gains[:, 0:1], r[:], ALU.mult, ALU.mult),
                  reads=[psn, gains, r], writes=[on])
            kb.dma("act", dst[h, 0:128, tsl], on[:], reads=[on])
            t1 = tmpr.next()
            t2 = tmpr.next()
            kb.op("dve", lambda: nc.vector.scalar_tensor_tensor(t1[0:64, :], r_ap, gains[0:64, 1:2], r[0:64, :], ALU.mult, ALU.mult),
                  reads=[r_t, gains, r], writes=[t1])
            kb.op("dve", lambda: nc.vector.tensor_tensor(t1[0:64, :], t1[0:64, :], cos2[:], ALU.mult),
                  reads=[t1, cos2], writes=[t1])
            kb.op("dve", lambda: nc.vector.scalar_tensor_tensor(t2[0:64, :], s_ap, gains[0:64, 2:3], r[0:64, :], ALU.mult, ALU.mult),
                  reads=[s_t, gains, r], writes=[t2])
            kb.op("dve", lambda: nc.vector.tensor_tensor(t2[0:64, :], t2[0:64, :], sin2[:], ALU.mult),
                  reads=[t2, sin2], writes=[t2])
            orp = stgb.next()
            kb.op("dve", lambda: nc.vector.tensor_tensor(orp[0:64, :], t1[0:64, :], t2[0:64, :], ALU.add),
                  reads=[t1, t2], writes=[orp])
            kb.dma("act", dst[h, 128:192, tsl], orp[0:64, :], reads=[orp])

        for h in range(8):
            head([(wuq_s[:, j, h * 256:h * 256 + 128], cqn[j][:], [wuq_s, cqn[j]]) for j in range(6)],
                 None, qg_s, o_q, h)
        for h in range(8):
            head([(wukk_s[:, j, h * 128:(h + 1) * 128], ckvn[j][:], [wukk_s, ckvn[j]]) for j in range(4)],
                 True, kg_s, o_k, h)
        for tt in range(TG // 128):
            for hf in range(2):
                ps = psr.next()
                kb.mm(ps, ps[:, 0:512], [(ckvn[j][:, tt * 128:(tt + 1) * 128], wukv_s[:, j, hf * 512:(hf + 1) * 512],
                                          [ckvn[j], wukv_s]) for j in range(4)])
                vb = stgb.next()
                kb.op("act", lambda: nc.scalar.copy(vb[:, 0:512], ps[:, 0:512]), reads=[ps], writes=[vb])
                kb.dma("act", o_v[t0 + tt * 128:t0 + (tt + 1) * 128, hf * 512:(hf + 1) * 512], vb[:, 0:512], reads=[vb])
    kb.finish()
    return nc


def _pk(v, n):
    return np.ascontiguousarray(np.asarray(v, np.float32).reshape(n, 128).T)


def _blockify(w, nb):
    w = w.reshape(KD, 128, nb, 128)
    return np.ascontiguousarray(w.transpose(2, 1, 0, 3).reshape(nb, 128, KD * 128))


def prep_A(inp, layer, S, mod):
    w_in = inp["w_in"][layer]
    cols = np.zeros((D, NBA * 128), np.float32)
    cols[:, 0:1536] = w_in[:, OFF["xbc"]:OFF["xbc"] + 1536]
    cols[:, 1536:2304] = w_in[:, OFF["cq"]:OFF["cq"] + 768]
    cols[:, 2304:2816] = w_in[:, OFF["ckv"]:OFF["ckv"] + 512]
    kr = w_in[:, OFF["kr"]:OFF["kr"] + 64]
    cols[:, 2816:2880] = kr
    cols[:, 2880:2912] = kr[:, 32:64]
    cols[:, 2912:2944] = kr[:, 0:32]
    cols[:, 2944:2960] = w_in[:, OFF["dt"]:OFF["dt"] + 16]
    wuq = inp["mla_w_uq"][layer].reshape(768, 8, 192)
    wuq2 = np.concatenate([wuq, wuq[:, :, 160:192], wuq[:, :, 128:160]], axis=2).reshape(768, 2048)
    wukv = inp["mla_w_ukv"][layer].reshape(512, 8, 256)

    def gain3(g):
        o = np.zeros((128, 3), np.float32)
        o[:, 0] = g[0:128]
        o[0:64, 1] = g[128:192]
        o[0:32, 2] = g[160:192]
        o[32:64, 2] = g[128:160]
        return o
    half = 32
    invf = (np.float32(10000.0) ** (-np.arange(half, dtype=np.float32) * np.float32(2.0) / np.float32(64))).astype(np.float32)
    rc = np.zeros((64, 4), np.float32)
    rc[:, 0] = np.concatenate([invf, invf])
    sgn = np.concatenate([-np.ones(32), np.ones(32)]).astype(np.float32)
    rc[:, 1] = sgn
    rc[:, 2] = -sgn * np.float32(math.pi)
    rc[:, 3] = -np.float32(math.pi)
    return dict(
        modT=np.ascontiguousarray(mod[:, 0:32]),
        winb=_blockify(cols, NBA),
        dtb_bc=np.ascontiguousarray(np.broadcast_to(inp["ssd_dt_bias"][layer][None, :], (128, 16))).astype(np.float32),
        qnorm_pk=_pk(inp["mla_q_norm"][layer], 6),
        kvnorm_pk=_pk(inp["mla_kv_norm"][layer], 4),
        wuq=np.ascontiguousarray(wuq2),
        wukv_k=np.ascontiguousarray(wukv[:, :, 0:128].reshape(512, 1024)),
        wukv_v=np.ascontiguousarray(wukv[:, :, 128:256].reshape(512, 1024)),
        qgain=gain3(inp["mla_q_gain"][layer]),
        kgain=gain3(inp["mla_k_gain"][layer]),
        ropec=rc,
    )


def run_A(inp, layer, xT, S, NTOK, mod, dbg=False):
    ncore = S // NTOK
    shared = prep_A(inp, layer, S, mod)
    pos = np.asarray(inp["positions"][0, :S]).astype(np.int32)
    maps = []
    for i in range(ncore):
        m = dict(shared)
        m["xT"] = np.ascontiguousarray(xT[:, i * NTOK:(i + 1) * NTOK])
        m["pos_bc"] = np.ascontiguousarray(np.broadcast_to(pos[None, i * NTOK:(i + 1) * NTOK], (64, NTOK)))
        maps.append(m)
    nc = build_A(NTOK, dbg)
    res = run_bass_kernel_spmd(nc, maps, core_ids=list(range(ncore)))
    R = res.results
    if dbg:
        return R
    out = dict(
        xbcT=np.concatenate([r["xbcT"] for r in R], axis=1),
        dt=np.concatenate([r["dt"] for r in R], axis=0),
        qT=np.concatenate([r["qT"] for r in R], axis=2),
        kT=np.concatenate([r["kT"] for r in R], axis=2),
        v=np.concatenate([r["v"] for r in R], axis=0),
    )
    return out


def build_B(S, do_att=True, do_ssd=True, stage=9):
    QG = 512
    NQG = S // QG
    NKB = S // 128
    NCH = S // 128
    nc = bass.Bass("TRN2", target_bir_lowering=False)
    qn_d = _din(nc, "qn", [128, S], BF16)
    qr_d = _din(nc, "qr", [64, S], BF16)
    kn_d = _din(nc, "kn", [128, S], BF16)
    kr_d = _din(nc, "kr", [64, S], BF16)
    v_d = _din(nc, "vb", [128, NKB, 128], BF16)
    mask_d = _din(nc, "masks", [128, 4, QG], BF16)
    slab_d = _din(nc, "slab", [384, S])
    convw_d = _din(nc, "convw", [128, 3, 4])
    convb_d = _din(nc, "convb", [128, 3])
    dt_d = _din(nc, "dth", [128, NCH, 2])
    alog_d = _din(nc, "alog_bc", [128, 2])
    dsk_d = _din(nc, "dskip_bc", [128, 2])
    U_d = _din(nc, "U", [128, 128])
    negm_d = _din(nc, "negmask", [128, 128])
    id_d = _din(nc, "ident", [128, 128])
    o_att = _dout(nc, "yatt", [128, S], BF16)
    o_ssd = _dout(nc, "yssd", [128, S], F32)

    kb = KB(nc)
    V = nc.vector
    A = nc.scalar
    ones = kb.sb([128, 128], BF16, "ones")
    onesf = kb.sb([128, 128], F32, "onesf")
    kb.op("pool", lambda: nc.gpsimd.memset(ones[:], 1.0), writes=[ones])
    kb.op("pool", lambda: nc.gpsimd.memset(onesf[:], 1.0), writes=[onesf])

    def load(ap, shape, dt, name, q="sp"):
        t = kb.sb(shape, dt, name)
        kb.dma(q, t[:], ap, writes=[t])
        return t

    if do_att:
        kn = load(kn_d, [128, S], BF16, "kn")
        kr = load(kr_d, [64, S], BF16, "kr")
        vb = load(v_d, [128, NKB, 128], BF16, "vb")
        masks = load(mask_d, [128, 4, QG], BF16, "masks")
        qnr = kb.ring(2, [128, QG], BF16, "qn")
        qrr = kb.ring(2, [64, QG], BF16, "qr")
        ptr = kb.ring(4, [128, QG], BF16, "pt")
        ps_s = kb.ring(2, [128, QG], F32, "pss", psum=True)
        ps_o = kb.ring(1, [128, QG], F32, "pso", psum=True)
        ps_d = kb.ring(1, [128, QG], F32, "psd", psum=True)
        rec = kb.sb([128, QG], F32, "rec")
        yst = kb.ring(2, [128, QG], BF16, "yst")
        scale = 192.0 ** -0.5
    if do_ssd:
        bX = kb.ps([128, 512], F32, "bankX")
        bY = kb.ps([128, 512], F32, "bankY")
        bZ = [kb.ps([128, 512], F32, "bankZ%d" % h) for h in range(2)]
        p_xt = View(bX, bX[:, 0:128], "p_xt")
        p_bt = View(bX, bX[:, 128:256], "p_bt")
        p_g = View(bX, bX[:, 256:384], "p_g")
        p_acs = View(bX, bX[:, 384:386], "p_acs")
        p_abc = [View(bY, bY[:, h * 128:(h + 1) * 128], "p_abc%d" % h) for h in range(2)]
        p_y = [View(bZ[h], bZ[h][:, 0:128], "p_y%d" % h) for h in range(2)]
        p_s = [View(bZ[h], bZ[h][:, 128:192], "p_s%d" % h) for h in range(2)]
        convw = load(convw_d, [128, 3, 4], F32, "convw")
        convb = load(convb_d, [128, 3], F32, "convb")
        dth = load(dt_d, [128, NCH, 2], F32, "dth")
        alog = load(alog_d, [128, 2], F32, "alog")
        dsk = load(dsk_d, [128, 2], F32, "dsk")
        U = load(U_d, [128, 128], F32, "U")
        negm = load(negm_d, [128, 128], F32, "negm")
        identf = load(id_d, [128, 128], F32, "identf")
        ident = kb.sb([128, 128], BF16, "ident")
        kb.op("dve", lambda: V.tensor_copy(ident[:], identf[:]), reads=[identf], writes=[ident])
        aneg = kb.sb([128, 2], F32, "aneg")
        kb.op("act", lambda: A.activation(aneg[:], alog[:], AF.Exp), reads=[alog], writes=[aneg])
        kb.op("dve", lambda: V.tensor_scalar_mul(aneg[:], aneg[:], -1.0), reads=[aneg], writes=[aneg])
        dI = [kb.sb([128, 128], BF16, "dI%d" % h) for h in range(2)]
        for h in range(2):
            kb.op("dve", lambda: V.tensor_scalar_mul(dI[h][:], identf[:], dsk[:, h:h + 1]), reads=[identf, dsk], writes=[dI[h]])
        CW = 512
        slabr = [kb.ring(2, [128, CW + 3], F32, "slab%d" % j) for j in range(3)]
        accr = kb.ring(2, [128, CW], F32, "cacc")
        xcT = [kb.ring(2, [128, CW], BF16, "xcT%d" % j) for j in range(3)]
        HT = [kb.sb([128, 64], F32, "HT%d" % h) for h in range(2)]
        Hbf = [kb.sb([128, 64], BF16, "Hbf%d" % h) for h in range(2)]
        for h in range(2):
            kb.op("pool", lambda: nc.gpsimd.memset(HT[h][:], 0.0), writes=[HT[h]])
            kb.op("pool", lambda: nc.gpsimd.memset(Hbf[h][:], 0.0), writes=[Hbf[h]])
        xtokr = kb.ring(2, [128, 128], BF16, "xtok")
        btokr = kb.ring(2, [128, 128], BF16, "btok")
        da = kb.ring(2, [128, 2], F32, "da")
        acs = kb.ring(2, [128, 2], F32, "acs")
        darep = kb.ring(2, [128, 128], F32, "darep")
        argr = kb.ring(2, [128, 128], F32, "arg")
        LTr = kb.ring(2, [128, 128], F32, "LT")
        MTr = kb.ring(2, [128, 128], BF16, "MT")
        Er = kb.ring(2, [128, 128], F32, "E")
        CPr = kb.ring(2, [128, 128], BF16, "CP")
        xdtr = kb.ring(2, [128, 64], BF16, "xdt")
        xdtdr = kb.ring(2, [128, 64], BF16, "xdtd")
        cdecr = kb.ring(2, [128, 1], F32, "cdec")
        stmpr = kb.ring(2, [128, 64], F32, "stmp")
        ystg = [kb.ring(2, [64, CW], F32, "ystg%d" % h) for h in range(2)]

    def att_group(g, gen):
        q0 = g * QG
        qn = qnr.next()
        qr = qrr.next()
        kb.dma("sp", qn[:], qn_d[:, q0:q0 + QG], writes=[qn])
        kb.dma("sp", qr[:], qr_d[:, q0:q0 + QG], writes=[qr])
        po = ps_o.next()
        pd = ps_d.next()
        nkb = (g + 1) * 4

        def qk(i):
            ksl = slice(i * 128, (i + 1) * 128)
            ps = ps_s.next()
            kb.op("pe", lambda: nc.tensor.matmul(ps[:], kn[:, ksl], qn[:], start=True, stop=False),
                  reads=[kn, qn], writes=[ps], inc=False)
            kb.op("pe", lambda: nc.tensor.matmul(ps[:], kr[:, ksl], qr[:], start=False, stop=True),
                  reads=[kr, qr], writes=[ps])
            return ps
        cur = qk(0)
        for i in range(nkb):
            nxt = qk(i + 1) if i + 1 < nkb else None
            ps = cur
            pt = ptr.next()
            kb.op("act", lambda: A.activation(pt[:], ps[:], AF.Exp, scale=scale), reads=[ps], writes=[pt])
            d = i - g * 4
            if d >= 0:
                kb.op("dve", lambda: V.tensor_tensor(pt[:], pt[:], masks[:, d, :], ALU.mult), reads=[pt, masks], writes=[pt])
            kb.op("pe", lambda: nc.tensor.matmul(po[:], vb[:, i, :], pt[:], start=(i == 0), stop=(i == nkb - 1)),
                  reads=[vb, pt], writes=[po], inc=False)
            kb.op("pe", lambda: nc.tensor.matmul(pd[:], ones[:], pt[:], start=(i == 0), stop=(i == nkb - 1)),
                  reads=[ones, pt], writes=[pd])
            if gen is not None:
                next(gen, None)
            cur = nxt
        kb.op("dve", lambda: V.reciprocal(rec[:], pd[:]), reads=[pd], writes=[rec])
        ys = yst.next()
        kb.op("dve", lambda: V.tensor_tensor(ys[:], po[:], rec[:], ALU.mult), reads=[po, rec], writes=[ys])
        kb.dma("act", o_att[:, q0:q0 + QG], ys[:], reads=[ys])

    def ssd_piece(pc):
        t0 = pc * CW
        cur = []
        for j in range(3):
            sl = slabr[j].next()
            if pc == 0:
                kb.op("pool", lambda: nc.gpsimd.memset(sl[:, 0:3], 0.0), writes=[sl])
                kb.dma("sp", sl[:, 3:CW + 3], slab_d[j * 128:(j + 1) * 128, 0:CW], writes=[sl])
            else:
                kb.dma("sp", sl[:], slab_d[j * 128:(j + 1) * 128, t0 - 3:t0 + CW], writes=[sl])
            acc = accr.next()
            kb.op("dve", lambda: V.tensor_scalar_mul(acc[:], sl[:, 0:CW], convw[:, j, 0:1]), reads=[sl, convw], writes=[acc])
            for k in range(1, 4):
                kb.op("dve", lambda: V.scalar_tensor_tensor(acc[:], sl[:, k:k + CW], convw[:, j, k:k + 1], acc[:], ALU.mult, ALU.add),
                      reads=[sl, convw, acc], writes=[acc])
            o = xcT[j].next()
            kb.op("act", lambda: A.activation(o[:], acc[:], AF.Silu, bias=convb[:, j:j + 1], scale=1.0),
                  reads=[acc, convb], writes=[o])
            cur.append(o)
            yield
        xT_, BT_, CT_ = cur
        if stage < 2:
            return
        yst2 = [ystg[h].next() for h in range(2)]
        for cc in range(CW // 128):
            c = pc * (CW // 128) + cc
            csl = slice(cc * 128, (cc + 1) * 128)
            kb.op("pe", lambda: nc.tensor.matmul(p_xt[:], xT_[:, csl], ident[:], start=True, stop=True),
                  reads=[xT_, ident], writes=[p_xt])
            kb.op("pe", lambda: nc.tensor.matmul(p_bt[:], BT_[:, csl], ident[:], start=True, stop=True),
                  reads=[BT_, ident], writes=[p_bt])
            kb.op("pe", lambda: nc.tensor.matmul(p_g[:], BT_[:, csl], CT_[:, csl], start=True, stop=True),
                  reads=[BT_, CT_], writes=[p_g])
            yield
            xtok = xtokr.next()
            btok = btokr.next()
            kb.op("act", lambda: A.copy(xtok[:], p_xt[:]), reads=[p_xt], writes=[xtok])
            kb.op("act", lambda: A.copy(btok[:], p_bt[:]), reads=[p_bt], writes=[btok])
            if stage < 3:
                continue
            da_t = da.next()
            kb.op("dve", lambda: V.tensor_tensor(da_t[:], dth[:, c, :], aneg[:], ALU.mult), reads=[dth, aneg], writes=[da_t])
            kb.op("pe", lambda: nc.tensor.matmul(p_acs[:], U[:], da_t[:], start=True, stop=True),
                  reads=[U, da_t], writes=[p_acs])
            dr = []
            for h in range(2):
                drt = darep.next()
                kb.op("act", lambda: A.activation(drt[:], onesf[:], AF.Identity, scale=da_t[:, h:h + 1]),
                      reads=[onesf, da_t], writes=[drt])
                dr.append(drt)
            for h in range(2):
                kb.op("pe", lambda: nc.tensor.matmul(p_abc[h][:], dr[h][:], U[:], start=True, stop=True),
                      reads=[dr[h], U], writes=[p_abc[h]])
            yield
            if stage < 4:
                continue
            acs_t = acs.next()
            kb.op("dve", lambda: V.tensor_copy(acs_t[:], p_acs[:]), reads=[p_acs], writes=[acs_t])
            for h in range(2):
                Abc = p_abc[h][:]
                psa = p_abc[h]
                arg = argr.next()
                kb.op("dve", lambda: V.scalar_tensor_tensor(arg[:], Abc, acs_t[:, h:h + 1], negm[:], ALU.subtract, ALU.add),
                      reads=[psa, acs_t, negm], writes=[arg])
                yield
                LT = LTr.next()
                kb.op("act", lambda: A.activation(LT[:], arg[:], AF.Exp), reads=[arg], writes=[LT])
                MT = MTr.next()
                kb.op("dve", lambda: V.tensor_tensor(MT[:], p_g[:], LT[:], ALU.mult), reads=[p_g, LT], writes=[MT])
                if stage < 5:
                    continue
                E = Er.next()
                kb.op("act", lambda: A.activation(E[:], Abc, AF.Exp), reads=[psa], writes=[E])
                CP = CPr.next()
                kb.op("pool", lambda: nc.gpsimd.tensor_tensor(CP[:], CT_[:, csl], E[:], ALU.mult), reads=[CT_, E], writes=[CP])
                yield
                xdt = xdtr.next()
                kb.op("dve", lambda: V.tensor_scalar_mul(xdt[:], xtok[:, h * 64:(h + 1) * 64], dth[:, c, h:h + 1]),
                      reads=[xtok, dth], writes=[xdt])
                xdtd = xdtdr.next()
                kb.op("dve", lambda: V.tensor_scalar_mul(xdtd[:], xdt[:], LT[:, 127:128]), reads=[xdt, LT], writes=[xdtd])
                cdec = cdecr.next()
                kb.op("act", lambda: A.copy(cdec[:], E[:, 127:128]), reads=[E], writes=[cdec])
                yield
                if stage < 6:
                    continue
                psy = p_y[h]
                pss_ = p_s[h]
                kb.op("pe", lambda: nc.tensor.matmul(psy[0:64, 0:128], xdt[:], MT[:], start=True, stop=False),
                      reads=[xdt, MT], writes=[psy], inc=False)
                kb.op("pe", lambda: nc.tensor.matmul(psy[0:64, 0:128], Hbf[h][:], CP[:], start=False, stop=False),
                      reads=[Hbf[h], CP], writes=[psy], inc=False)
                kb.op("pe", lambda: nc.tensor.matmul(psy[0:64, 0:128], xtok[:, h * 64:(h + 1) * 64], dI[h][:], start=False, stop=True),
                      reads=[xtok, dI[h]], writes=[psy], inc=False)
                kb.op("pe", lambda: nc.tensor.matmul(pss_[:], btok[:], xdtd[:], start=True, stop=True),
                      reads=[btok, xdtd], writes=[psy, pss_])
                yield
                if stage < 7:
                    continue
                kb.op("act", lambda: A.copy(yst2[h][:, csl], psy[0:64, 0:128]), reads=[psy], writes=[yst2[h]])
                if stage < 8:
                    continue
                stmp = stmpr.next()
                kb.op("act", lambda: A.copy(stmp[:], pss_[:]), reads=[pss_], writes=[stmp])
                kb.op("dve", lambda: V.scalar_tensor_tensor(HT[h][:], HT[h][:], cdec[:, 0:1], stmp[:], ALU.mult, ALU.add),
                      reads=[HT[h], cdec, stmp], writes=[HT[h]])
                kb.op("pool", lambda: nc.gpsimd.tensor_copy(Hbf[h][:], HT[h][:]), reads=[HT[h]], writes=[Hbf[h]])
        if stage < 9:
            return
        for h in range(2):
            kb.dma("act", o_ssd[h * 64:(h + 1) * 64, t0:t0 + CW], yst2[h][:], reads=[yst2[h]])

    def ssd_all():
        for pc in range(S // CW):
            yield from ssd_piece(pc)

    gen = ssd_all() if do_ssd else None
    if do_att:
        for g in range(NQG):
            att_group(g, gen)
    if gen is not None:
        for _ in gen:
            pass
    kb.finish()
    return nc


def consts_B():
    k = np.arange(128)[:, None]
    q = np.arange(512)[None, :]
    masks = np.stack([(k + d * 128 <= q) for d in range(4)], axis=1).astype(np.float32).astype(ml_dtypes.bfloat16)
    s = np.arange(128)[:, None]
    l = np.arange(128)[None, :]
    U = (s <= l).astype(np.float32)
    negm = np.where(s <= l, 0.0, -30000.0).astype(np.float32)
    return dict(masks=np.ascontiguousarray(masks), U=U, negmask=negm, ident=np.eye(128, dtype=np.float32))


def run_B(inp, layer, A_out, S, do_att=True, do_ssd=True, stage=9):
    cst = consts_B()
    qT, kT, v = A_out["qT"], A_out["kT"], A_out["v"]
    xbcT, dt = A_out["xbcT"], A_out["dt"]
    cw = inp["ssd_conv_w"][layer]
    cb = inp["ssd_conv_b"][layer]
    NCH = S // 128
    maps = []
    for i in range(8):
        g = i // 4
        rows = np.concatenate([np.arange(i * 128, (i + 1) * 128), 1024 + g * 128 + np.arange(128),
                               1280 + g * 128 + np.arange(128)])
        m = dict(cst)
        m["qn"] = np.ascontiguousarray(qT[i, 0:128])
        m["qr"] = np.ascontiguousarray(qT[i, 128:192])
        m["kn"] = np.ascontiguousarray(kT[i, 0:128])
        m["kr"] = np.ascontiguousarray(kT[i, 128:192])
        m["vb"] = np.ascontiguousarray(v[:, i * 128:(i + 1) * 128].reshape(S // 128, 128, 128).transpose(1, 0, 2))
        m["slab"] = np.ascontiguousarray(xbcT[rows])
        m["convw"] = np.ascontiguousarray(cw[:, rows].T.reshape(3, 128, 4).transpose(1, 0, 2))
        m["convb"] = np.ascontiguousarray(cb[rows].reshape(3, 128).T)
        m["dth"] = np.ascontiguousarray(dt[:, 2 * i:2 * i + 2].reshape(NCH, 128, 2).transpose(1, 0, 2))
        m["alog_bc"] = np.ascontiguousarray(np.broadcast_to(inp["ssd_a_log"][layer][None, 2 * i:2 * i + 2], (128, 2))).astype(np.float32)
        m["dskip_bc"] = np.ascontiguousarray(np.broadcast_to(inp["ssd_d"][layer][None, 2 * i:2 * i + 2], (128, 2))).astype(np.float32)
        maps.append(m)
    nc = build_B(S, do_att, do_ssd, stage)
    res = run_bass_kernel_spmd(nc, maps, core_ids=list(range(8)))
    R = res.results
    return dict(yattT=np.concatenate([r["yatt"] for r in R], axis=0),
                yssdT=np.concatenate([r["yssd"] for r in R], axis=0))


NBC = 96


def build_C1(NTOK, dbg=False):
    TG = 512
    NG = NTOK // TG
    nc = bass.Bass("TRN2", target_bir_lowering=False)
    xTh = _din(nc, "xTh", [D, NTOK + 16])
    yatt_d = _din(nc, "yattT", [1024, NTOK], BF16)
    yssd_d = _din(nc, "yssdT", [1024, NTOK])
    mod_d = _din(nc, "modT", [128, 48])
    winb = _din(nc, "winb", [NBC, 128, KD * 128])
    wpool_d = _din(nc, "wpoolb", [4, 128, 2 * 256])
    pscale_d = _din(nc, "pscale_pk", [128, 8])
    hflag_d = _din(nc, "haloflag", [128, 1])
    pcorr_d = _din(nc, "pcorr", [128, 4, 16])
    snorm_d = _din(nc, "ssdnorm_pk", [128, 8])
    lng_d = _din(nc, "lng_bc", [128, 1024])
    lnb_d = _din(nc, "lnb_bc", [128, 1024])
    wsT_d = _din(nc, "wsT", [128, 8, 128])
    um_d = _din(nc, "Umask", [128, 128])
    bs_d = _din(nc, "bs_row", [1, 1024])
    wbr_d = _din(nc, "wbrb", [4, 16, 128, 8 * 128])
    wout_d = _din(nc, "woutb", [16, 128, KD * 128])
    o_x = _dout(nc, "xmidT", [D, NTOK])
    if dbg:
        o_dy = _dout(nc, "dbg_y", [4, 1024, 512], BF16)
        o_dm = _dout(nc, "dbg_m", [2048, 512], BF16)

    kb = KB(nc)
    V = nc.vector
    A = nc.scalar
    G = nc.gpsimd
    psr = kb.ring(7, [128, 512], F32, "ps", psum=True)
    ps_stat = kb.ps([128, 512], F32, "ps_stat")
    ones = kb.sb([128, 128], BF16, "ones")
    onesf = kb.sb([1, 128], F32, "onesf")
    epsb = kb.sb([128, 1], F32, "eps")
    kb.op("pool", lambda: G.memset(ones[:], 1.0), writes=[ones])
    kb.op("pool", lambda: G.memset(onesf[:], 1.0), writes=[onesf])
    kb.op("pool", lambda: G.memset(epsb[:], EPS), writes=[epsb])
    modT = _load_mod(kb, mod_d, 48)
    onepT = kb.sb([128, 16], F32, "onep")
    kb.op("dve", lambda: V.tensor_scalar_add(onepT[:], modT[:, 16:32], 1.0), reads=[modT], writes=[onepT])

    def load(ap, shape, dt=F32, name="c", q="sp"):
        t = kb.sb(shape, dt, name)
        kb.dma(q, t[:], ap, writes=[t])
        return t
    pscale = load(pscale_d, [128, 8])
    hflag = load(hflag_d, [128, 1])
    pcorr = load(pcorr_d, [128, 4, 16])
    snorm = load(snorm_d, [128, 8])
    lng = load(lng_d, [128, 1024])
    lnb = load(lnb_d, [128, 1024])
    wsTf = load(wsT_d, [128, 8, 128])
    um = load(um_d, [128, 128])
    bs = load(bs_d, [1, 1024])
    wsm = kb.sb([128, 8, 128], BF16, "wsm")
    for gi in range(8):
        kb.op("dve", lambda: V.tensor_tensor(wsm[:, gi, :], wsTf[:, gi, :], um[:], ALU.mult), reads=[wsTf, um], writes=[wsm])
    wpool_s = kb.sb([128, 4, 512], BF16, "wpool")
    for wg in range(4):
        kb.dma("pool", wpool_s[:, wg, :], wpool_d[wg], writes=[wpool_s])

    xkr = kb.ring(4, [128, TG], F32, "xk")
    hT = [kb.sb([128, TG], BF16, "h%d" % k) for k in range(KD)]
    xh = kb.sb([128, KD, 16], F32, "xh")
    hTh = kb.sb([128, KD, 16], BF16, "hTh")
    sqh = kb.sb([128, KD, 16], BF16, "sqh")
    rstd_h = kb.sb([128, 16], F32, "rstdh")
    tmph = kb.sb([128, 16], F32, "tmph")
    rstd_x = kb.sb([128, TG], F32, "rstdx")
    sqr = kb.ring(3, [128, TG], BF16, "sq")
    tmpr = kb.ring(4, [128, TG], F32, "tmp")
    wbr = kb.ring(6, [128, KD, 128], BF16, "wb")
    wbbr = kb.ring(6, [128, 8, 128], BF16, "wbb")
    Ar = kb.ring(2, [128, TG + 16], F32, "A")
    Sr = kb.ring(2, [128, TG + 16], F32, "S")
    plr = kb.ring(4, [128, TG], BF16, "pl")
    ypool = [kb.sb([128, TG], BF16, "ypool%d" % j) for j in range(8)]
    yssd = [kb.sb([128, TG], BF16, "yssd%d" % j) for j in range(8)]
    yssdf = kb.ring(2, [128, TG], F32, "yssdf")
    yatt = [kb.sb([128, TG], BF16, "yatt%d" % j) for j in range(8)]
    ug = [kb.sb([128, TG], BF16, "ug%d" % j) for j in range(8)]
    ysgu = ug
    vt = [kb.sb([128, 1024], F32, "vt%d" % t) for t in range(TG // 128)]
    vl = [kb.sb([128, 1024], BF16, "vl%d" % t) for t in range(TG // 128)]
    vsum = kb.sb([128, 4, 8], F32, "vsum")
    vst = kb.sb([128, 4, 4], F32, "vst")
    junk = kb.sb([128, 1024], BF16, "junk")
    merged = [kb.sb([128, TG], BF16, "mrg%d" % j) for j in range(KD)]
    accr = kb.ring(2, [128, TG], F32, "acc")
    xTv = xTh.rearrange("(k p) t -> p k t", p=128)

    def load_wblock(b):
        wb = wbr.next()
        kb.dma("pool", wb[:], winb[b].rearrange("p (k c) -> p k c", k=KD), writes=[wb])
        return wb

    def hp(wb, c0, c1):
        return [(wb[:, k, c0:c1], hT[k][:], [wb, hT[k]]) for k in range(KD)]

    for g in range(NG):
        t0 = g * TG
        tsl = slice(t0, t0 + TG)
        kb.dma("sp", xh[:], xTv[:, :, t0:t0 + 16], writes=[xh])
        def xload(k):
            xt = xkr.next()
            kb.dma("sp", xt[:], xTv[:, k, t0 + 16:t0 + 16 + TG], writes=[xt])
            return xt
        for j in range(8):
            kb.dma("sp", yatt[j][:], yatt_d[j * 128:(j + 1) * 128, tsl], writes=[yatt[j]])
        _emit_norm_mod2(kb, xload, hT, modT, onepT, ones, epsb, psr, sqr, tmpr, TG, rstd_x)
        kb.op("act", lambda: A.activation(sqh[:], xh[:], AF.Square), reads=[xh], writes=[sqh])
        ps = psr.next()
        kb.mm(ps, ps[:, 0:16], [(ones[:], sqh[:, k, :], [ones, sqh]) for k in range(KD)])
        kb.op("act", lambda: A.activation(rstd_h[:], ps[:, 0:16], AF.Sqrt, bias=epsb[:, 0:1], scale=1.0 / D),
              reads=[ps, epsb], writes=[rstd_h])
        kb.op("dve", lambda: V.reciprocal(rstd_h[:], rstd_h[:]), reads=[rstd_h], writes=[rstd_h])
        for k in range(KD):
            kb.op("dve", lambda: V.scalar_tensor_tensor(tmph[:], xh[:, k, :], onepT[:, k:k + 1], rstd_h[:], ALU.mult, ALU.mult),
                  reads=[xh, onepT, rstd_h], writes=[tmph])
            kb.op("dve", lambda: V.tensor_scalar_add(hTh[:, k, :], tmph[:], modT[:, k:k + 1]), reads=[tmph, modT], writes=[hTh])

        plc = {}
        for blk in range(8):
            plc[blk] = plr.next()
            wb = load_wblock(blk)
            ps = psr.next()
            kb.mm(ps, ps[:, 0:TG], hp(wb, 0, 128))
            ps2 = psr.next()
            kb.mm(ps2, ps2[:, 0:16], [(wb[:, k, :], hTh[:, k, :], [wb, hTh]) for k in range(KD)])
            At = Ar.next()
            kb.op("act", lambda: A.copy(At[:, 16:16 + TG], ps[:, 0:TG]), reads=[ps], writes=[At])
            if g == 0:
                kb.op("dve", lambda: V.tensor_scalar_mul(At[:, 0:16], ps2[:, 0:16], hflag[:, 0:1]), reads=[ps2, hflag], writes=[At])
            else:
                kb.op("dve", lambda: V.tensor_copy(At[:, 0:16], ps2[:, 0:16]), reads=[ps2], writes=[At])
            wi = blk // 2
            W = TG + 16
            cur = At
            sh = 1
            for step in range(wi + 1):
                St = Sr.next()
                eng = "dve"
                E_ = V if eng == "dve" else G
                kb.op(eng, lambda: E_.tensor_tensor(St[:, sh:W], cur[:, sh:W], cur[:, 0:W - sh], ALU.add),
                      reads=[cur], writes=[St])
                cur = St
                sh *= 2
            win = float(2 ** (wi + 1))
            kb.op("dve", lambda: V.scalar_tensor_tensor(plc[blk][:], cur[:, 16:W], 1.0 / win, At[:, 16:W], ALU.mult, ALU.subtract),
                  reads=[cur, At], writes=[plc[blk]])
            if g == 0:
                kb.op("dve", lambda: V.tensor_tensor(cur[:, 16:32], cur[:, 16:32], pcorr[:, wi, :], ALU.mult),
                      reads=[cur, pcorr], writes=[cur])
                kb.op("dve", lambda: V.scalar_tensor_tensor(plc[blk][:, 0:16], cur[:, 16:32], 1.0 / win, At[:, 16:32], ALU.mult, ALU.subtract),
                      reads=[cur, At], writes=[plc[blk]])
            if blk % 2 == 0:
                continue
            wg = blk // 2
            for dh in range(2):
                ps = psr.next()
                kb.mm(ps, ps[:, 0:TG], [(wpool_s[:, wg, kc * 256 + dh * 128:kc * 256 + (dh + 1) * 128], plc[wg * 2 + kc][:],
                                         [wpool_s, plc[wg * 2 + kc]]) for kc in range(2)])
                j = wg * 2 + dh
                kb.op("act", lambda: A.activation(ypool[j][:], ps[:, 0:TG], AF.Identity, scale=pscale[:, j:j + 1]),
                      reads=[ps, pscale], writes=[ypool[j]])

        pss = ps_stat
        for blk in range(8):
            yf = yssdf.next()
            kb.dma("sp", yf[:], yssd_d[blk * 128:(blk + 1) * 128, tsl], writes=[yf])
            wb = load_wblock(8 + blk)
            ps = psr.next()
            kb.mm(ps, ps[:, 0:TG], hp(wb, 0, 128))
            sz = tmpr.next()
            kb.op("act", lambda: A.activation(sz[:], ps[:, 0:TG], AF.Silu), reads=[ps], writes=[sz])
            kb.op("dve", lambda: V.tensor_tensor(sz[:], sz[:], yf[:], ALU.mult), reads=[sz, yf], writes=[sz])
            kb.op("dve", lambda: V.tensor_copy(yssd[blk][:], sz[:]), reads=[sz], writes=[yssd[blk]])
            sq = sqr.next()
            kb.op("act", lambda: A.activation(sq[:], sz[:], AF.Square), reads=[sz], writes=[sq])
            kb.op("pe", lambda: nc.tensor.matmul(pss[:, 0:TG], ones[:], sq[:], start=(blk == 0), stop=(blk == 7)),
                  reads=[ones, sq], writes=[pss])
        r = tmpr.next()
        _rstd(kb, pss, r, 1024, epsb)
        for blk in range(8):
            kb.op("dve", lambda: V.scalar_tensor_tensor(yssd[blk][:], yssd[blk][:], snorm[:, blk:blk + 1], r[:], ALU.mult, ALU.mult),
                  reads=[yssd[blk], snorm, r], writes=[yssd[blk]])

        for blk in range(8):
            wb = load_wblock(16 + blk)
            ps = psr.next()
            kb.mm(ps, ps[:, 0:TG], hp(wb, 0, 128))
            kb.op("act", lambda: A.activation(ug[blk][:], ps[:, 0:TG], AF.Gelu), reads=[ps], writes=[ug[blk]])
        for blk in range(8):
            wb = load_wblock(24 + blk)
            ps = psr.next()
            for tt in range(4):
                kb.mm(ps, ps[:, tt * 128:(tt + 1) * 128],
                      [(hT[k][:, tt * 128:(tt + 1) * 128], wb[:, k, :], [wb, hT[k]]) for k in range(KD)])
            for tt in range(4):
                kb.op("act", lambda: A.activation(vt[tt][:, blk * 128:(blk + 1) * 128], ps[:, tt * 128:(tt + 1) * 128], AF.Gelu,
                                                  accum_out=vsum[:, tt, blk:blk + 1]),
                      reads=[ps], writes=[vt[tt], vsum])
        for tt in range(4):
            kb.op("dve", lambda: V.reduce_sum(vst[:, tt, 0:1], vsum[:, tt, :], axis=AX.X), reads=[vsum], writes=[vst])
            kb.op("dve", lambda: V.tensor_scalar_mul(vst[:, tt, 0:1], vst[:, tt, 0:1], 1.0 / 1024), reads=[vst], writes=[vst])
            kb.op("dve", lambda: V.tensor_scalar_sub(vt[tt][:], vt[tt][:], vst[:, tt, 0:1]), reads=[vt[tt], vst], writes=[vt[tt]])
            kb.op("act", lambda: A.activation(junk[:], vt[tt][:], AF.Square, accum_out=vst[:, tt, 1:2]),
                  reads=[vt[tt]], writes=[junk, vst])
            kb.op("act", lambda: A.activation(vst[:, tt, 2:3], vst[:, tt, 1:2], AF.Sqrt, bias=epsb[:, 0:1], scale=1.0 / 1024),
                  reads=[vst, epsb], writes=[vst])
            kb.op("dve", lambda: V.reciprocal(vst[:, tt, 3:4], vst[:, tt, 2:3]), reads=[vst], writes=[vst])
            kb.op("dve", lambda: V.scalar_tensor_tensor(vt[tt][:], vt[tt][:], vst[:, tt, 3:4], lng[:], ALU.mult, ALU.mult),
                  reads=[vt[tt], vst, lng], writes=[vt[tt]])
            kb.op("dve", lambda: V.tensor_tensor(vl[tt][:], vt[tt][:], lnb[:], ALU.add), reads=[vt[tt], lnb], writes=[vl[tt]])
        for gi in range(8):
            ps = psr.next()
            for tt in range(4):
                kb.op("pe", lambda: nc.tensor.matmul(ps[:, tt * 128:(tt + 1) * 128], vl[tt][:, gi * 128:(gi + 1) * 128], wsm[:, gi, :],
                                                     start=True, stop=False), reads=[vl[tt], wsm], writes=[ps], inc=False)
                kb.op("pe", lambda: nc.tensor.matmul(ps[:, tt * 128:(tt + 1) * 128], onesf[0:1, :], bs[0:1, gi * 128:(gi + 1) * 128],
                                                     start=False, stop=True), reads=[onesf, bs], writes=[ps], inc=(tt == 3))
            kb.op("dve", lambda: V.tensor_tensor(ysgu[gi][:], ps[:, 0:TG], ug[gi][:], ALU.mult), reads=[ps, ug[gi]], writes=[ysgu[gi]])

        ybr = [ypool, yssd, yatt, ysgu]
        if dbg and g == 0:
            for b in range(4):
                for j in range(8):
                    kb.dma("act", o_dy[b, j * 128:(j + 1) * 128, :], ybr[b][j][:], reads=[ybr[b][j]])
        for dc in range(KD):
            acc = accr.next()
            for b in range(4):
                wbb = wbbr.next()
                kb.dma("pool", wbb[:], wbr_d[b, dc].rearrange("p (k c) -> p k c", k=8), writes=[wbb])
                psP = psr.next()
                kb.mm(psP, psP[:, 0:TG], [(wbb[:, j, :], ybr[b][j][:], [wbb, ybr[b][j]]) for j in range(8)])
                wg_ = load_wblock(32 + b * 16 + dc)
                psG = psr.next()
                kb.mm(psG, psG[:, 0:TG], hp(wg_, 0, 128))
                sg = tmpr.next()
                kb.op("act", lambda: A.activation(sg[:], psG[:, 0:TG], AF.Sigmoid), reads=[psG], writes=[sg])
                if b == 0:
                    kb.op("dve", lambda: V.tensor_tensor(acc[:], psP[:, 0:TG], sg[:], ALU.mult), reads=[psP, sg], writes=[acc])
                else:
                    kb.op("dve", lambda: V.tensor_tensor(sg[:], psP[:, 0:TG], sg[:], ALU.mult), reads=[psP, sg], writes=[sg])
                    if b < 3:
                        kb.op("dve", lambda: V.tensor_tensor(acc[:], acc[:], sg[:], ALU.add), reads=[acc, sg], writes=[acc])
                    else:
                        kb.op("dve", lambda: V.tensor_tensor(merged[dc][:], acc[:], sg[:], ALU.add), reads=[acc, sg], writes=[merged[dc]])
        if dbg and g == 0:
            for j in range(KD):
                kb.dma("act", o_dm[j * 128:(j + 1) * 128, :], merged[j][:], reads=[merged[j]])
        for dc in range(KD):
            wo = wbr.next()
            kb.dma("pool", wo[:], wout_d[dc].rearrange("p (k c) -> p k c", k=KD), writes=[wo])
            ps = psr.next()
            kb.mm(ps, ps[:, 0:TG], [(wo[:, k, :], merged[k][:], [wo, merged[k]]) for k in range(KD)])
            xt = xload(dc)
            kb.op("dve", lambda: V.scalar_tensor_tensor(xt[:], ps[:, 0:TG], modT[:, 32 + dc:33 + dc], xt[:], ALU.mult, ALU.add),
                  reads=[ps, modT, xt], writes=[xt])
            kb.dma("act", o_x[dc * 128:(dc + 1) * 128, tsl], xt[:], reads=[xt])
    kb.finish()
    return nc


def prep_C1(inp, layer, mod):
    w_in = inp["w_in"][layer]
    cols = np.concatenate([w_in[:, OFF["a_in"]:OFF["a_in"] + 1024], w_in[:, OFF["z"]:OFF["z"] + 1024],
                           w_in[:, OFF["uv"]:OFF["uv"] + 2048], w_in[:, OFF["gates"]:OFF["gates"] + 8192]], axis=1)
    wp = inp["w_pool"][layer]
    wpoolb = np.ascontiguousarray(wp.reshape(4, 2, 128, 256).transpose(0, 2, 1, 3).reshape(4, 128, 512))
    wbr = inp["w_branch"][layer]
    wbrb = np.ascontiguousarray(wbr.reshape(4, 8, 128, 16, 128).transpose(0, 3, 2, 1, 4).reshape(4, 16, 128, 1024))
    wout = inp["w_out"][layer]
    s = np.arange(128)[:, None]
    t = np.arange(128)[None, :]
    return dict(
        modT=np.ascontiguousarray(mod[:, 0:48]),
        winb=_blockify(np.ascontiguousarray(cols), NBC),
        wpoolb=wpoolb,
        pscale_pk=_pk(inp["pool_scale"][layer], 8),
        ssdnorm_pk=_pk(inp["ssd_norm"][layer], 8),
        lng_bc=np.ascontiguousarray(np.broadcast_to(inp["sgu_ln_gain"][layer][None, :], (128, 1024))).astype(np.float32),
        lnb_bc=np.ascontiguousarray(np.broadcast_to(inp["sgu_ln_bias"][layer][None, :], (128, 1024))).astype(np.float32),
        wsT=np.ascontiguousarray(inp["sgu_w_s"][layer].transpose(2, 0, 1)),
        Umask=(s <= t).astype(np.float32),
        bs_row=np.ascontiguousarray(inp["sgu_b_s"][layer].reshape(1, 1024)),
        wbrb=wbrb,
        woutb=_blockify(np.ascontiguousarray(wout), 16),
    )


def run_C1(inp, layer, xT, B_out, S, NTOK, mod, dbg=False):
    ncore = S // NTOK
    shared = prep_C1(inp, layer, mod)
    xTh = np.concatenate([np.zeros((D, 16), np.float32), xT], axis=1)
    maps = []
    for i in range(ncore):
        m = dict(shared)
        m["xTh"] = np.ascontiguousarray(xTh[:, i * NTOK:(i + 1) * NTOK + 16])
        m["yattT"] = np.ascontiguousarray(B_out["yattT"][:, i * NTOK:(i + 1) * NTOK])
        m["yssdT"] = np.ascontiguousarray(B_out["yssdT"][:, i * NTOK:(i + 1) * NTOK])
        m["haloflag"] = np.full((128, 1), 0.0 if i == 0 else 1.0, np.float32)
        pc = np.ones((128, 4, 16), np.float32)
        if i == 0:
            for wi, win in enumerate((2, 4, 8, 16)):
                tt = np.arange(16)
                pc[:, wi, :] = (win / np.minimum(tt + 1, win))[None, :]
        m["pcorr"] = pc
        maps.append(m)
    nc = build_C1(NTOK, dbg)
    res = run_bass_kernel_spmd(nc, maps, core_ids=list(range(ncore)))
    if dbg:
        return res.results
    return np.concatenate([r["xmidT"] for r in res.results], axis=1)


def build_C2(NTOK, NF, expert, SG=2):
    TG = 512
    NGRP = NTOK // TG
    NSG = -(-NGRP // SG)
    NH = NF // 2
    nc = bass.Bass("TRN2", target_bir_lowering=False)
    xT = _din(nc, "xT", [D, NTOK])
    mod_d = _din(nc, "modT", [128, 48])
    w1_d = _din(nc, "w1b", [NF, 128, KD * 128])
    w3_d = _din(nc, "w3b", [NF, 128, KD * 128])
    w2_d = _din(nc, "w2b", [16, 128, NF * 128])
    if expert:
        wrow_d = _din(nc, "wrow", [1, NTOK])
    o_x = _dout(nc, "xoutT", [D, NTOK])

    kb = KB(nc)
    V = nc.vector
    A = nc.scalar
    G = nc.gpsimd
    psr = kb.ring(8, [128, 512], F32, "ps", psum=True)
    ones = kb.sb([128, 128], BF16, "ones")
    onesf = kb.sb([1, 128], F32, "onesf")
    epsb = kb.sb([128, 1], F32, "eps")
    kb.op("pool", lambda: G.memset(ones[:], 1.0), writes=[ones])
    kb.op("pool", lambda: G.memset(onesf[:], 1.0), writes=[onesf])
    kb.op("pool", lambda: G.memset(epsb[:], EPS), writes=[epsb])
    modT = _load_mod(kb, mod_d, 48)
    onepT = kb.sb([128, 16], F32, "onep")
    kb.op("dve", lambda: V.tensor_scalar_add(onepT[:], modT[:, 16:32], 1.0), reads=[modT], writes=[onepT])

    xkr = kb.ring(4, [128, TG], F32, "xk")
    hT = [[kb.sb([128, TG], BF16, "h%d_%d" % (s_, k)) for k in range(KD)] for s_ in range(SG)]
    rstd_x = kb.sb([128, TG], F32, "rstdx")
    sqr = kb.ring(2, [128, TG], BF16, "sq")
    tmpr = kb.ring(3, [128, TG], F32, "tmp")
    wbr = kb.ring(4 if expert else 8, [128, KD, 128], BF16, "wb")
    w2r = kb.ring(3 if expert else 4, [128, NH, 128], BF16, "w2")
    gt = [[kb.sb([128, TG], BF16, "g%d_%d" % (s_, f)) for f in range(NF)] for s_ in range(SG)]
    if expert:
        wrow = kb.sb([1, TG], F32, "wrow")
        wbc = [kb.sb([128, TG], F32, "wbc%d" % s_) for s_ in range(SG)]
    xTv = xT.rearrange("(k p) t -> p k t", p=128)

    for sg in range(NSG):
        nsg = min(SG, NGRP - sg * SG)
        tsls = [slice((sg * SG + s_) * TG, (sg * SG + s_ + 1) * TG) for s_ in range(nsg)]

        def mk_xload(tsl):
            def xload(k):
                xt = xkr.next()
                kb.dma("sp", xt[:], xTv[:, k, tsl], writes=[xt])
                return xt
            return xload
        for s_ in range(nsg):
            if expert:
                kb.dma("sp", wrow[:], wrow_d[:, tsls[s_]], writes=[wrow])
                ps = psr.next()
                kb.op("pe", lambda: nc.tensor.matmul(ps[:, 0:TG], onesf[0:1, :], wrow[0:1, :], start=True, stop=True),
                      reads=[onesf, wrow], writes=[ps])
                kb.op("act", lambda: A.copy(wbc[s_][:], ps[:, 0:TG]), reads=[ps], writes=[wbc[s_]])
            _emit_norm_mod2(kb, mk_xload(tsls[s_]), hT[s_], modT, onepT, ones, epsb, psr, sqr, tmpr, TG, rstd_x)
        for f in range(NF):
            w1 = wbr.next()
            kb.dma("pool", w1[:], w1_d[f].rearrange("p (k c) -> p k c", k=KD), writes=[w1])
            w3 = wbr.next()
            kb.dma("pool", w3[:], w3_d[f].rearrange("p (k c) -> p k c", k=KD), writes=[w3])
            for s_ in range(nsg):
                p1 = psr.next()
                kb.mm(p1, p1[:, 0:TG], [(w1[:, k, :], hT[s_][k][:], [w1, hT[s_][k]]) for k in range(KD)])
                p3 = psr.next()
                kb.mm(p3, p3[:, 0:TG], [(w3[:, k, :], hT[s_][k][:], [w3, hT[s_][k]]) for k in range(KD)])
                s1 = tmpr.next()
                kb.op("act", lambda: A.activation(s1[:], p1[:, 0:TG], AF.Silu), reads=[p1], writes=[s1])
                if expert:
                    kb.op("dve", lambda: V.tensor_tensor(s1[:], s1[:], wbc[s_][:], ALU.mult), reads=[s1, wbc[s_]], writes=[s1])
                kb.op("dve", lambda: V.tensor_tensor(gt[s_][f][:], p3[:, 0:TG], s1[:], ALU.mult), reads=[p3, s1], writes=[gt[s_][f]])
        for dc in range(KD):
            w2h = []
            for hf in range(2):
                w2 = w2r.next()
                kb.dma("pool", w2[:], w2_d[dc][:, hf * NH * 128:(hf + 1) * NH * 128].rearrange("p (f c) -> p f c", f=NH), writes=[w2])
                w2h.append(w2)
            pso = [psr.next() for s_ in range(nsg)]
            for hf in range(2):
                for s_ in range(nsg):
                    for fi in range(NH):
                        f = hf * NH + fi
                        kb.op("pe", lambda: nc.tensor.matmul(pso[s_][:, 0:TG], w2h[hf][:, fi, :], gt[s_][f][:],
                                                             start=(f == 0), stop=(f == NF - 1)),
                              reads=[w2h[hf], gt[s_][f]], writes=[pso[s_]], inc=(fi == NH - 1))
            for s_ in range(nsg):
                ps = pso[s_]
                xo = xkr.next()
                if expert:
                    kb.op("act", lambda: A.activation(xo[:], ps[:, 0:TG], AF.Identity, scale=modT[:, 32 + dc:33 + dc]),
                          reads=[ps, modT], writes=[xo])
                else:
                    kb.dma("sp", xo[:], xTv[:, dc, tsls[s_]], writes=[xo])
                    kb.op("dve", lambda: V.scalar_tensor_tensor(xo[:], ps[:, 0:TG], modT[:, 32 + dc:33 + dc], xo[:], ALU.mult, ALU.add),
                          reads=[ps, modT, xo], writes=[xo])
                kb.dma("act", o_x[dc * 128:(dc + 1) * 128, tsls[s_]], xo[:], reads=[xo])
    kb.finish()
    return nc


def _blockify_w2(w2, nf):
    w = w2.reshape(nf, 128, 16, 128)
    return np.ascontiguousarray(w.transpose(2, 1, 0, 3).reshape(16, 128, nf * 128))


def run_C2_dense(inp, layer, xT, S, NTOK, mod):
    ncore = S // NTOK
    idx = layer // 2
    nf = 5632 // 128
    shared = dict(modT=np.ascontiguousarray(mod[:, 48:96]),
                  w1b=_blockify(np.ascontiguousarray(inp["ffn_w1"][idx]), nf),
                  w3b=_blockify(np.ascontiguousarray(inp["ffn_w3"][idx]), nf),
                  w2b=_blockify_w2(inp["ffn_w2"][idx], nf))
    maps = []
    for i in range(ncore):
        m = dict(shared)
        m["xT"] = np.ascontiguousarray(xT[:, i * NTOK:(i + 1) * NTOK])
        maps.append(m)
    res = run_bass_kernel_spmd(build_C2(NTOK, nf, False), maps, core_ids=list(range(ncore)))
    return np.concatenate([r["xoutT"] for r in res.results], axis=1)


def build_R(NTOK):
    TG = 512
    NG = NTOK // TG
    nc = bass.Bass("TRN2", target_bir_lowering=False)
    xT = _din(nc, "xT", [D, NTOK])
    mod_d = _din(nc, "modT", [128, 48])
    wr_d = _din(nc, "wr_pk", [128, KD, 8])
    o_w = _dout(nc, "wt", [NTOK, 8])
    kb = KB(nc)
    V = nc.vector
    A = nc.scalar
    G = nc.gpsimd
    psr = kb.ring(4, [128, 512], F32, "ps", psum=True)
    ones = kb.sb([128, 128], BF16, "ones")
    epsb = kb.sb([128, 1], F32, "eps")
    kb.op("pool", lambda: G.memset(ones[:], 1.0), writes=[ones])
    kb.op("pool", lambda: G.memset(epsb[:], EPS), writes=[epsb])
    modT = _load_mod(kb, mod_d, 48)
    onepT = kb.sb([128, 16], F32, "onep")
    kb.op("dve", lambda: V.tensor_scalar_add(onepT[:], modT[:, 16:32], 1.0), reads=[modT], writes=[onepT])
    wr = kb.sb([128, KD, 8], F32, "wr")
    kb.dma("sp", wr[:], wr_d, writes=[wr])
    xk = [kb.sb([128, TG], F32, "x%d" % k) for k in range(KD)]
    h32 = [kb.sb([128, TG], F32, "h%d" % k) for k in range(KD)]
    sqr = kb.ring(3, [128, TG], BF16, "sq")
    rstd = kb.sb([128, TG], F32, "rstd")
    lg = kb.sb([128, 4, 8], F32, "lg")
    l2 = kb.sb([128, 4, 8], F32, "l2")
    mk1 = kb.sb([128, 4, 8], F32, "mk1")
    mk2 = kb.sb([128, 4, 8], F32, "mk2")
    wt = kb.sb([128, 4, 8], F32, "wt")
    mm_ = kb.sb([128, 4, 4], F32, "mm_")
    xTv = xT.rearrange("(k p) t -> p k t", p=128)
    for g in range(NG):
        tsl = slice(g * TG, (g + 1) * TG)
        for k in range(KD):
            kb.dma("sp", xk[k][:], xTv[:, k, tsl], writes=[xk[k]])
        ps = psr.next()
        for k in range(KD):
            sq = sqr.next()
            kb.op("act", lambda: A.activation(sq[:], xk[k][:], AF.Square), reads=[xk[k]], writes=[sq])
            kb.op("pe", lambda: nc.tensor.matmul(ps[:, 0:TG], ones[:], sq[:], start=(k == 0), stop=(k == KD - 1)),
                  reads=[ones, sq], writes=[ps])
        _rstd(kb, ps, rstd, D, epsb)
        for k in range(KD):
            kb.op("dve", lambda: V.tensor_tensor(h32[k][:], xk[k][:], rstd[:], ALU.mult), reads=[xk[k], rstd], writes=[h32[k]])
            kb.op("act", lambda: A.activation(h32[k][:], h32[k][:], AF.Identity, bias=modT[:, k:k + 1], scale=onepT[:, k:k + 1]),
                  reads=[h32[k], modT, onepT], writes=[h32[k]])
        ps = psr.next()
        for tt in range(4):
            kb.mm(ps, ps[:, tt * 8:(tt + 1) * 8],
                  [(h32[k][:, tt * 128:(tt + 1) * 128], wr[:, k, :], [h32[k], wr]) for k in range(KD)])
        kb.op("dve", lambda: V.tensor_copy(lg[:].rearrange("p a b -> p (a b)"), ps[:, 0:32]), reads=[ps], writes=[lg])
        for tt in range(4):
            kb.op("dve", lambda: V.reduce_max(mm_[:, tt, 0:1], lg[:, tt, :], axis=AX.X), reads=[lg], writes=[mm_])
            kb.op("dve", lambda: V.tensor_scalar(mk1[:, tt, :], lg[:, tt, :], mm_[:, tt, 0:1], None, ALU.is_equal),
                  reads=[lg, mm_], writes=[mk1])
            kb.op("dve", lambda: V.scalar_tensor_tensor(l2[:, tt, :], mk1[:, tt, :], -1e30, lg[:, tt, :], ALU.mult, ALU.add),
                  reads=[mk1, lg], writes=[l2])
            kb.op("dve", lambda: V.reduce_max(mm_[:, tt, 1:2], l2[:, tt, :], axis=AX.X), reads=[l2], writes=[mm_])
            kb.op("dve", lambda: V.tensor_scalar(mk2[:, tt, :], l2[:, tt, :], mm_[:, tt, 1:2], None, ALU.is_equal),
                  reads=[l2, mm_], writes=[mk2])
            kb.op("dve", lambda: V.tensor_tensor(mm_[:, tt, 2:3], mm_[:, tt, 1:2], mm_[:, tt, 0:1], ALU.subtract), reads=[mm_], writes=[mm_])
            kb.op("act", lambda: A.activation(mm_[:, tt, 2:3], mm_[:, tt, 2:3], AF.Exp), reads=[mm_], writes=[mm_])
            kb.op("dve", lambda: V.tensor_scalar_add(mm_[:, tt, 3:4], mm_[:, tt, 2:3], 1.0), reads=[mm_], writes=[mm_])
            kb.op("dve", lambda: V.reciprocal(mm_[:, tt, 3:4], mm_[:, tt, 3:4]), reads=[mm_], writes=[mm_])
            kb.op("dve", lambda: V.tensor_tensor(mm_[:, tt, 2:3], mm_[:, tt, 2:3], mm_[:, tt, 3:4], ALU.mult), reads=[mm_], writes=[mm_])
            kb.op("dve", lambda: V.tensor_scalar_mul(wt[:, tt, :], mk1[:, tt, :], mm_[:, tt, 3:4]), reads=[mk1, mm_], writes=[wt])
            kb.op("dve", lambda: V.scalar_tensor_tensor(wt[:, tt, :], mk2[:, tt, :], mm_[:, tt, 2:3], wt[:, tt, :], ALU.mult, ALU.add),
                  reads=[mk2, mm_, wt], writes=[wt])
        kb.dma("act", o_w[tsl, :].rearrange("(t p) e -> p t e", p=128), wt[:], reads=[wt])
    kb.finish()
    return nc


def run_R(inp, layer, xT, S, NTOK, mod):
    ncore = S // NTOK
    idx = layer // 2
    shared = dict(modT=np.ascontiguousarray(mod[:, 48:96]),
                  wr_pk=np.ascontiguousarray(inp["moe_router"][idx].reshape(KD, 128, 8).transpose(1, 0, 2)))
    maps = []
    for i in range(ncore):
        m = dict(shared)
        m["xT"] = np.ascontiguousarray(xT[:, i * NTOK:(i + 1) * NTOK])
        maps.append(m)
    res = run_bass_kernel_spmd(build_R(NTOK), maps, core_ids=list(range(ncore)))
    return np.concatenate([r["wt"] for r in res.results], axis=0)


def build_S(NTOK):
    TG = 512
    nc = bass.Bass("TRN2", target_bir_lowering=False)
    xT = _din(nc, "xT", [D, NTOK])
    y0 = _din(nc, "y0T", [D, NTOK])
    y1 = _din(nc, "y1T", [D, NTOK])
    o = _dout(nc, "xoutT", [D, NTOK])
    kb = KB(nc)
    V = nc.vector
    ar = kb.ring(3, [128, TG], F32, "a")
    br = kb.ring(3, [128, TG], F32, "b")
    cr = kb.ring(3, [128, TG], F32, "c")
    for k in range(KD):
        for g in range(NTOK // TG):
            rs = slice(k * 128, (k + 1) * 128)
            tsl = slice(g * TG, (g + 1) * TG)
            a, b, c = ar.next(), br.next(), cr.next()
            kb.dma("sp", a[:], xT[rs, tsl], writes=[a])
            kb.dma("sp", b[:], y0[rs, tsl], writes=[b])
            kb.dma("sp", c[:], y1[rs, tsl], writes=[c])
            kb.op("dve", lambda: V.tensor_tensor(b[:], b[:], c[:], ALU.add), reads=[b, c], writes=[b])
            kb.op("dve", lambda: V.tensor_tensor(a[:], a[:], b[:], ALU.add), reads=[a, b], writes=[a])
            kb.dma("act", o[rs, tsl], a[:], reads=[a])
    kb.finish()
    return nc


def run_moe(inp, layer, xT, S, NTOK, mod):
    idx = layer // 2
    nf = 7168 // 128
    wt = run_R(inp, layer, xT, S, NTOK, mod)
    sel = wt > 0
    lists = [np.nonzero(sel[:, e])[0] for e in range(8)]
    cap = max(512, int(-(-max(len(l) for l in lists) // 512) * 512))
    print('[moe] tokens per expert', [len(l) for l in lists], 'cap', cap, flush=True)
    maps = []
    for e in range(8):
        tl = lists[e]
        xg = np.zeros((D, cap), np.float32)
        xg[:, :len(tl)] = xT[:, tl]
        wrow = np.zeros((1, cap), np.float32)
        wrow[0, :len(tl)] = wt[tl, e]
        maps.append(dict(xT=xg, wrow=wrow, modT=np.ascontiguousarray(mod[:, 48:96]),
                         w1b=_blockify(np.ascontiguousarray(inp["moe_w1"][idx, e]), nf),
                         w3b=_blockify(np.ascontiguousarray(inp["moe_w3"][idx, e]), nf),
                         w2b=_blockify_w2(inp["moe_w2"][idx, e], nf)))
    res = run_bass_kernel_spmd(build_C2(cap, nf, True), maps, core_ids=list(range(8)))
    y = [np.zeros((D, S), np.float32), np.zeros((D, S), np.float32)]
    nsel = np.zeros(S, np.int64)
    for e in range(8):
        tl = lists[e]
        ye = res.results[e]["xoutT"][:, :len(tl)]
        slot = nsel[tl]
        for sidx in (0, 1):
            m = slot == sidx
            y[sidx][:, tl[m]] = ye[:, m]
        nsel[tl] += 1
    ncore = S // NTOK
    maps = []
    for i in range(ncore):
        sl = slice(i * NTOK, (i + 1) * NTOK)
        maps.append(dict(xT=np.ascontiguousarray(xT[:, sl]), y0T=np.ascontiguousarray(y[0][:, sl]), y1T=np.ascontiguousarray(y[1][:, sl])))
    res = run_bass_kernel_spmd(build_S(NTOK), maps, core_ids=list(range(ncore)))
    return np.concatenate([r["xoutT"] for r in res.results], axis=1)


def kernel(**inp):
    inp = {k: np.asarray(v) for k, v in inp.items()}
    S = inp["x"].shape[1]
    NTOK = S // 8
    xT = np.ascontiguousarray(inp["x"][0].T)
    mods = run_M(inp)
    for layer in range(2):
        A_out = run_A(inp, layer, xT, S, NTOK, mods[layer])
        B_out = run_B(inp, layer, A_out, S)
        del A_out
        xmid = run_C1(inp, layer, xT, B_out, S, NTOK, mods[layer])
        del B_out
        if layer % 2 == 0:
            xT = run_C2_dense(inp, layer, xmid, S, NTOK, mods[layer])
        else:
            xT = run_moe(inp, layer, xmid, S, NTOK, mods[layer])
    return np.ascontiguousarray(xT.T)[None].astype(np.float32)
```
